# Optimizing a Trainium2 kernel written in Bass

```python
import math
import jax, jax.numpy as jnp
from jax import lax
import numpy as np

D_MODEL = 1024
BATCH = 8
SEQ = 4096
DEPTH = 4

PLE_DIM = 256
DIFF_HEADS = 8
DIFF_HEAD_DIM = 64
DIFF_V_DIM = 2 * DIFF_HEAD_DIM
MLA_HEADS = 8
MLA_Q_RANK = 384
MLA_KV_RANK = 256
MLA_NOPE_DIM = 64
MLA_ROPE_DIM = 32
MLA_V_DIM = 64
ROPE_THETA = 10000.0
REL_BUCKETS = 32
REL_MAX_DIST = 128
DENSE_FF = 2816
N_EXPERTS = 8
TOP_K = 2
EXPERT_FF = 3584
EXPERT_BLOCK = 128
Q_BLOCK = 128
EPS = 1e-5
DEEPNORM_ALPHA = (2.0 * DEPTH) ** 0.25
DEEPNORM_BETA = (8.0 * DEPTH) ** -0.25
N_DENSE = (DEPTH + 1) // 2
N_MOE = DEPTH // 2

DIFF_Q_COLS = DIFF_HEADS * 2 * DIFF_HEAD_DIM
DIFF_K_COLS = DIFF_HEADS * 2 * DIFF_HEAD_DIM
DIFF_V_COLS = DIFF_HEADS * DIFF_V_DIM
GATE_COLS = 2 * D_MODEL
IN_SIZES = (DIFF_Q_COLS, DIFF_K_COLS, DIFF_V_COLS, MLA_Q_RANK, MLA_KV_RANK, MLA_ROPE_DIM, GATE_COLS)
IN_SPLIT_IDX = [sum(IN_SIZES[:j + 1]) for j in range(len(IN_SIZES) - 1)]
IN_COLS = sum(IN_SIZES)

kernel_name = "hybrid_diffattn_mla_gated_moe_deepnorm"


def _layernorm(x, g, b):
    xf = x.astype(jnp.float32)
    mu = jnp.mean(xf, axis=-1, keepdims=True)
    var = jnp.mean(jnp.square(xf - mu), axis=-1, keepdims=True)
    y = (xf - mu) * lax.rsqrt(var + EPS) * g.astype(jnp.float32) + b.astype(jnp.float32)
    return y.astype(x.dtype)


def _rmsnorm(x, g):
    xf = x.astype(jnp.float32)
    y = xf * lax.rsqrt(jnp.mean(jnp.square(xf), axis=-1, keepdims=True) + EPS) * g.astype(jnp.float32)
    return y.astype(x.dtype)


def _rope(x, pos):
    half = x.shape[-1] // 2
    inv_freq = ROPE_THETA ** (-jnp.arange(half, dtype=jnp.float32) / half)
    ang = pos.astype(jnp.float32)[:, :, None] * inv_freq
    c = jnp.cos(ang)[:, :, None, :]
    s = jnp.sin(ang)[:, :, None, :]
    xf = x.astype(jnp.float32)
    x1, x2 = xf[..., :half], xf[..., half:]
    return jnp.concatenate([x1 * c - x2 * s, x1 * s + x2 * c], axis=-1).astype(x.dtype)


def _t5_bucket(dist):
    n = jnp.maximum(dist, 0)
    max_exact = REL_BUCKETS // 2
    large = max_exact + (jnp.log(jnp.maximum(n, 1).astype(jnp.float32) / max_exact)
                         / math.log(REL_MAX_DIST / max_exact) * (REL_BUCKETS - max_exact)).astype(jnp.int32)
    large = jnp.minimum(large, REL_BUCKETS - 1)
    return jnp.where(n < max_exact, n, large)


def _t5_bias(table, q_pos, k_pos):
    bucket = _t5_bucket(q_pos[:, :, None] - k_pos[:, None, :])
    return jnp.transpose(table[bucket].astype(jnp.float32), (0, 3, 1, 2))


def _causal_mask(q0, kend):
    return jnp.arange(kend)[None, :] <= (q0 + jnp.arange(Q_BLOCK))[:, None]


def _diff_attention(q, k, v, pos, rel_table, lam, lam_init, subln_g):
    B, S, H = q.shape[0], q.shape[1], q.shape[2]
    scale = DIFF_HEAD_DIM ** -0.5
    outs = []
    for q0 in range(0, S, Q_BLOCK):
        kend = q0 + Q_BLOCK
        s = jnp.einsum('bqhmd,bkhmd->bhmqk', q[:, q0:kend], k[:, :kend]).astype(jnp.float32) * scale
        s = s + _t5_bias(rel_table, pos[:, q0:kend], pos[:, :kend])[:, :, None]
        s = jnp.where(_causal_mask(q0, kend), s, -jnp.inf)
        a = jax.nn.softmax(s, axis=-1)
        w = a[:, :, 0] - lam * a[:, :, 1]
        outs.append(jnp.einsum('bhqk,bkhe->bqhe', w.astype(v.dtype), v[:, :kend]))
    o = jnp.concatenate(outs, axis=1)
    o = _rmsnorm(o, subln_g) * (1.0 - lam_init)
    return o.reshape(B, S, H * DIFF_V_DIM)


def _mla(c_q, c_kv, k_r, pos, q_norm_g, w_uq, kv_norm_g, w_ukv):
    B, S = c_q.shape[0], c_q.shape[1]
    q = (_rmsnorm(c_q, q_norm_g) @ w_uq).reshape(B, S, MLA_HEADS, MLA_NOPE_DIM + MLA_ROPE_DIM)
    q_nope, q_rope = q[..., :MLA_NOPE_DIM], _rope(q[..., MLA_NOPE_DIM:], pos)
    kv = (_rmsnorm(c_kv, kv_norm_g) @ w_ukv).reshape(B, S, MLA_HEADS, MLA_NOPE_DIM + MLA_V_DIM)
    k_nope, v = kv[..., :MLA_NOPE_DIM], kv[..., MLA_NOPE_DIM:]
    k_rope = _rope(k_r[:, :, None, :], pos)[:, :, 0]
    scale = (MLA_NOPE_DIM + MLA_ROPE_DIM) ** -0.5
    outs = []
    for q0 in range(0, S, Q_BLOCK):
        kend = q0 + Q_BLOCK
        s = (jnp.einsum('bqhd,bkhd->bhqk', q_nope[:, q0:kend], k_nope[:, :kend])
             + jnp.einsum('bqhr,bkr->bhqk', q_rope[:, q0:kend], k_rope[:, :kend])).astype(jnp.float32) * scale
        s = jnp.where(_causal_mask(q0, kend), s, -jnp.inf)
        a = jax.nn.softmax(s, axis=-1)
        outs.append(jnp.einsum('bhqk,bkhd->bqhd', a.astype(v.dtype), v[:, :kend]))
    return jnp.concatenate(outs, axis=1).reshape(B, S, MLA_HEADS * MLA_V_DIM)


def _mixer(h, pos, rel_table, w_in, b_gate, lq1, lk1, lq2, lk2, lam_init, diff_subln_g,
           mla_q_norm_g, w_uq, mla_kv_norm_g, w_ukv, w_branch_diff, w_branch_mla, w_out):
    B, S, _ = h.shape
    z = h @ w_in
    dq, dk, dv, cq, ckv, kr, gl = jnp.split(z, IN_SPLIT_IDX, axis=-1)
    lam = (jnp.exp(jnp.sum(lq1.astype(jnp.float32) * lk1.astype(jnp.float32)))
           - jnp.exp(jnp.sum(lq2.astype(jnp.float32) * lk2.astype(jnp.float32))) + lam_init)
    o_a = _diff_attention(dq.reshape(B, S, DIFF_HEADS, 2, DIFF_HEAD_DIM),
                          dk.reshape(B, S, DIFF_HEADS, 2, DIFF_HEAD_DIM),
                          dv.reshape(B, S, DIFF_HEADS, DIFF_V_DIM),
                          pos, rel_table, lam, lam_init, diff_subln_g)
    o_b = _mla(cq, ckv, kr, pos, mla_q_norm_g, w_uq, mla_kv_norm_g, w_ukv)
    g = jax.nn.sigmoid(gl + b_gate)
    g_a, g_b = g[..., :D_MODEL], g[..., D_MODEL:]
    m = g_a * (o_a @ w_branch_diff) + g_b * (o_b @ w_branch_mla)
    return m @ w_out


def _swiglu(x, w1, w3, w2):
    return (jax.nn.silu(x @ w1) * (x @ w3)) @ w2


def _moe(x, w_router, w1, w3, w2):
    B, S, D = x.shape
    xt = x.reshape(-1, D)
    T = xt.shape[0]
    M = T * TOP_K
    logits = (xt @ w_router).astype(jnp.float32)
    top_v, top_e = lax.top_k(logits, TOP_K)
    gates = jax.nn.softmax(top_v, axis=-1)
    flat_e = top_e.reshape(-1)
    flat_tok = jnp.repeat(jnp.arange(T, dtype=jnp.int32), TOP_K)
    order = jnp.argsort(flat_e, stable=True)
    se, stok, sg = flat_e[order], flat_tok[order], gates.reshape(-1)[order]
    counts = jnp.bincount(flat_e, length=N_EXPERTS)
    padded = ((counts + EXPERT_BLOCK - 1) // EXPERT_BLOCK) * EXPERT_BLOCK
    start = jnp.cumsum(counts) - counts
    pstart = jnp.cumsum(padded) - padded
    dest = pstart[se] + jnp.arange(M, dtype=jnp.int32) - start[se]
    n_blocks = -(-M // EXPERT_BLOCK) + N_EXPERTS
    P = n_blocks * EXPERT_BLOCK
    buf = jnp.zeros((P, D), x.dtype).at[dest].set(xt[stok])
    block_e = jnp.minimum(jnp.searchsorted(jnp.cumsum(padded), jnp.arange(n_blocks) * EXPERT_BLOCK,
                                           side='right'), N_EXPERTS - 1)

    def run(args):
        xb, e = args
        return _swiglu(xb, w1[e], w3[e], w2[e])

    yb = lax.map(run, (buf.reshape(n_blocks, EXPERT_BLOCK, D), block_e)).reshape(P, D)
    y = jnp.zeros((T, D), x.dtype).at[stok].add(yb[dest] * sg[:, None].astype(x.dtype))
    return y.reshape(B, S, D)


def setup_inputs(seed: int = 0) -> dict:
    key = jax.random.key(seed)
    ks = jax.random.split(key, 32)
    f32 = jnp.float32

    def nrm(k, shape, scale):
        return jax.random.normal(k, shape, f32) * scale

    beta = DEEPNORM_BETA
    return {
        "x": nrm(ks[0], (BATCH, SEQ, D_MODEL), 1.0),
        "p": nrm(ks[1], (DEPTH, BATCH, SEQ, PLE_DIM), 1.0),
        "positions": jnp.broadcast_to(jnp.arange(SEQ, dtype=jnp.int32), (BATCH, SEQ)),
        "rel_bias_table": nrm(ks[2], (REL_BUCKETS, DIFF_HEADS), 0.5),
        "w_in": nrm(ks[3], (DEPTH, D_MODEL, IN_COLS), D_MODEL ** -0.5),
        "b_gate": nrm(ks[4], (DEPTH, GATE_COLS), 0.02),
        "lambda_q1": nrm(ks[5], (DEPTH, DIFF_HEAD_DIM), 0.1),
        "lambda_k1": nrm(ks[6], (DEPTH, DIFF_HEAD_DIM), 0.1),
        "lambda_q2": nrm(ks[7], (DEPTH, DIFF_HEAD_DIM), 0.1),
        "lambda_k2": nrm(ks[8], (DEPTH, DIFF_HEAD_DIM), 0.1),
        "diff_subln_g": 1.0 + nrm(ks[9], (DEPTH, DIFF_V_DIM), 0.02),
        "mla_q_norm_g": 1.0 + nrm(ks[10], (DEPTH, MLA_Q_RANK), 0.02),
        "w_uq": nrm(ks[11], (DEPTH, MLA_Q_RANK, MLA_HEADS * (MLA_NOPE_DIM + MLA_ROPE_DIM)), MLA_Q_RANK ** -0.5),
        "mla_kv_norm_g": 1.0 + nrm(ks[12], (DEPTH, MLA_KV_RANK), 0.02),
        "w_ukv": nrm(ks[13], (DEPTH, MLA_KV_RANK, MLA_HEADS * (MLA_NOPE_DIM + MLA_V_DIM)), MLA_KV_RANK ** -0.5),
        "w_branch_diff": nrm(ks[14], (DEPTH, DIFF_HEADS * DIFF_V_DIM, D_MODEL), (DIFF_HEADS * DIFF_V_DIM) ** -0.5),
        "w_branch_mla": nrm(ks[15], (DEPTH, MLA_HEADS * MLA_V_DIM, D_MODEL), (MLA_HEADS * MLA_V_DIM) ** -0.5),
        "w_out": nrm(ks[16], (DEPTH, D_MODEL, D_MODEL), beta * D_MODEL ** -0.5),
        "ln_mix_g": 1.0 + nrm(ks[17], (DEPTH, D_MODEL), 0.02),
        "ln_mix_b": nrm(ks[18], (DEPTH, D_MODEL), 0.02),
        "dense_w1": nrm(ks[19], (N_DENSE, D_MODEL, DENSE_FF), D_MODEL ** -0.5),
        "dense_w3": nrm(ks[20], (N_DENSE, D_MODEL, DENSE_FF), D_MODEL ** -0.5),
        "dense_w2": nrm(ks[21], (N_DENSE, DENSE_FF, D_MODEL), beta * DENSE_FF ** -0.5),
        "router_w": nrm(ks[22], (N_MOE, D_MODEL, N_EXPERTS), D_MODEL ** -0.5),
        "expert_w1": nrm(ks[23], (N_MOE, N_EXPERTS, D_MODEL, EXPERT_FF), D_MODEL ** -0.5),
        "expert_w3": nrm(ks[24], (N_MOE, N_EXPERTS, D_MODEL, EXPERT_FF), D_MODEL ** -0.5),
        "expert_w2": nrm(ks[25], (N_MOE, N_EXPERTS, EXPERT_FF, D_MODEL), beta * EXPERT_FF ** -0.5),
        "w_ple_gate": nrm(ks[26], (DEPTH, D_MODEL, D_MODEL), D_MODEL ** -0.5),
        "w_ple_proj": nrm(ks[27], (DEPTH, PLE_DIM, D_MODEL), beta * PLE_DIM ** -0.5),
        "ln_ffn_g": 1.0 + nrm(ks[28], (DEPTH, D_MODEL), 0.02),
        "ln_ffn_b": nrm(ks[29], (DEPTH, D_MODEL), 0.02),
    }


def reference(x, p, positions, rel_bias_table, w_in, b_gate, lambda_q1, lambda_k1, lambda_q2, lambda_k2,
              diff_subln_g, mla_q_norm_g, w_uq, mla_kv_norm_g, w_ukv, w_branch_diff, w_branch_mla, w_out,
              ln_mix_g, ln_mix_b, dense_w1, dense_w3, dense_w2, router_w, expert_w1, expert_w3, expert_w2,
              w_ple_gate, w_ple_proj, ln_ffn_g, ln_ffn_b):
    for i in range(DEPTH):
        lam_init = 0.8 - 0.6 * math.exp(-0.3 * i)
        y = _mixer(x, positions, rel_bias_table, w_in[i], b_gate[i], lambda_q1[i], lambda_k1[i],
                   lambda_q2[i], lambda_k2[i], lam_init, diff_subln_g[i], mla_q_norm_g[i], w_uq[i],
                   mla_kv_norm_g[i], w_ukv[i], w_branch_diff[i], w_branch_mla[i], w_out[i])
        x = _layernorm(DEEPNORM_ALPHA * x + y, ln_mix_g[i], ln_mix_b[i])
        j = i // 2
        if i % 2 == 0:
            f = _swiglu(x, dense_w1[j], dense_w3[j], dense_w2[j])
        else:
            f = _moe(x, router_w[j], expert_w1[j], expert_w3[j], expert_w2[j])
        e = jax.nn.sigmoid(x @ w_ple_gate[i]) * (p[i] @ w_ple_proj[i])
        x = _layernorm(DEEPNORM_ALPHA * x + f + e, ln_ffn_g[i], ln_ffn_b[i])
    return x
```

```python
import os
import math
from contextlib import ExitStack
import numpy as np
import concourse.bass as bass
import concourse.mybir as mybir
from concourse.bass_utils import run_bass_kernel_spmd

F32 = mybir.dt.float32
BF16 = mybir.dt.bfloat16
I32 = mybir.dt.int32
AF = mybir.ActivationFunctionType
ALU = mybir.AluOpType
AX = mybir.AxisListType

ENGS = ("pe", "act", "dve", "pool", "sp")
SEM_MAX = 20000
NSLOT = 84


class Op:
    __slots__ = ("eng", "fn", "deps", "dma", "slot", "idx", "sig", "ordn", "barrier")

    def __init__(self, eng, fn, dma):
        self.eng, self.fn, self.dma = eng, fn, dma
        self.deps = []
        self.sig = False
        self.ordn = None
        self.slot = None
        self.barrier = False


class Sched:
    def __init__(self):
        self.ops = {e: [] for e in ENGS}
        self.state = {}
        self.slot_of = {}
        self.slot_cnt = [0] * NSLOT
        self.slot_last = [None] * NSLOT
        self.nslot_used = 0
        self.max_slots = 0

    def add(self, eng, fn, r=(), w=(), dma=None):
        op = Op(eng, fn, dma)
        w = list(w) + [b for b in r if b.startswith('ps')]
        r = [b for b in r if not b.startswith('ps')]
        deps = []
        for b in r:
            st = self.state.setdefault(b, [None, []])
            if st[0] is not None:
                deps.append(st[0])
        for b in w:
            st = self.state.setdefault(b, [None, []])
            if st[0] is not None:
                deps.append(st[0])
            deps.extend(st[1])
        seen = set()
        for d in deps:
            if id(d) in seen:
                continue
            seen.add(id(d))
            if d.dma is None and op.dma is None and d.eng == "pe" and eng == "pe":
                continue
            op.deps.append(d)
            if d.dma is None:
                d.sig = True
        if dma is not None:
            if dma not in self.slot_of:
                assert self.nslot_used < NSLOT, "out of dma semaphore slots"
                self.slot_of[dma] = self.nslot_used
                self.nslot_used += 1
                self.max_slots = max(self.max_slots, self.nslot_used)
            sl = self.slot_of[dma]
            self.slot_cnt[sl] += 1
            assert self.slot_cnt[sl] * 16 < 32000, "dma sem count too large: %s" % dma
            op.slot = sl
            op.ordn = self.slot_cnt[sl]
            self.slot_last[sl] = op
        op.idx = len(self.ops[eng])
        self.ops[eng].append(op)
        for b in r:
            self.state[b][1].append(op)
        for b in w:
            self.state[b] = [op, []]
        return op

    def barrier(self):
        used = self.nslot_used
        c = Op("sp", None, None)
        c.barrier = True
        for e in ENGS:
            if e == "sp":
                continue
            for op in reversed(self.ops[e]):
                if op.dma is None:
                    if not op.barrier:
                        c.deps.append(op)
                        op.sig = True
                    break
        for sl in range(used):
            if self.slot_last[sl] is not None:
                c.deps.append(self.slot_last[sl])
        c.slot = used
        c.sig = True
        c.idx = len(self.ops["sp"])
        self.ops["sp"].append(c)
        for e in ENGS:
            if e == "sp":
                continue
            d = Op(e, None, None)
            d.barrier = True
            d.deps.append(c)
            d.idx = len(self.ops[e])
            self.ops[e].append(d)
        self.state = {}
        self.slot_of = {}
        self.slot_cnt = [0] * NSLOT
        self.slot_last = [None] * NSLOT
        self.nslot_used = 0

    def emit(self, nc):
        from contextlib import ExitStack
        self.barrier()
        nsig = {}
        for e in ENGS:
            n = 0
            for op in self.ops[e]:
                if op.dma is None and op.sig:
                    op.ordn = n
                    n += 1
            nsig[e] = n
        with ExitStack() as es:
            esem = {}
            for e in ENGS:
                k = max(1, (nsig[e] + SEM_MAX - 1) // SEM_MAX)
                esem[e] = [es.enter_context(nc.semaphore("s_%s_%d" % (e, i))) for i in range(k)]
            dsem = [es.enter_context(nc.semaphore("d_%d" % i)) for i in range(self.max_slots)]
            block = es.enter_context(nc.Block())

            def target(d):
                if d.dma is not None:
                    return dsem[d.slot], 16 * d.ordn
                return esem[d.eng][d.ordn // SEM_MAX], d.ordn % SEM_MAX + 1

            def run(e, eng):
                waited = {}
                for op in self.ops[e]:
                    for d in op.deps:
                        s, v = target(d)
                        if waited.get(s.name, 0) >= v:
                            continue
                        waited[s.name] = v
                        eng.wait_ge(s, v)
                    if op.barrier:
                        if e == "sp":
                            for sl in range(op.slot):
                                eng.sem_clear(dsem[sl])
                            ins = eng.nop()
                        else:
                            ins = eng.nop()
                        for k in list(waited.keys()):
                            if k.startswith("d_"):
                                del waited[k]
                    else:
                        ins = op.fn(eng)
                    if op.dma is not None:
                        ins.then_inc(dsem[op.slot], 16)
                    elif op.sig:
                        ins.then_inc(esem[e][op.ordn // SEM_MAX], 1)

            @block.tensor
            def _(eng):
                run("pe", eng)

            @block.scalar
            def _(eng):
                run("act", eng)

            @block.vector
            def _(eng):
                run("dve", eng)

            @block.gpsimd
            def _(eng):
                run("pool", eng)

            @block.sync
            def _(eng):
                run("sp", eng)


D = 1024
NQ = 384
NKV = 256
COLQ, COLK, COLV, COLCQ, COLCKV, COLKR, COLG = 0, 1024, 2048, 3072, 3456, 3712, 3744
INC = 5792
DENSE_FF = 2816
EXP_FF = 3584
NEG = -30000.0
DEPTH_TOTAL = 4
ALPHA = (2.0 * DEPTH_TOTAL) ** 0.25
EPS = 1e-5


def t5_thresholds():
    n = np.arange(0, 4096, dtype=np.int32)
    nf = np.maximum(n, 1).astype(np.float32)
    large = 16 + ((np.log(nf / np.float32(16)) / np.float32(math.log(128 / 16))) * np.float32(16)).astype(np.int32)
    large = np.minimum(large, 31)
    bucket = np.where(n < 16, n, large)
    th = []
    for j in range(1, 32):
        th.append(int(np.argmax(bucket >= j)))
    return th


def build(S, L, TG=1024, dbg=(), upto=None):
    nc = bass.Bass("TRN2", target_bir_lowering=False)
    NT = S // 512
    NB = S // 128
    NG = S // TG
    ND = (L + 1) // 2
    NM = L // 2
    es = ExitStack()
    S_ = Sched()
    add = S_.add

    def din(name, shape, dt=F32):
        return nc.dram_tensor(name, list(shape), dt, kind="ExternalInput").ap()

    def dscr(name, shape, dt):
        kind = "ExternalOutput" if name in dbg else "Internal"
        return nc.dram_tensor(name, list(shape), dt, kind=kind).ap()

    x_in = din("x", [S, D])
    p_in = din("p", [L, S, 256])
    pos_in = din("pos", [1, S], I32)
    tab_in = din("tab", [1, 256])
    w_in = din("w_in", [L, D, INC])
    bg_in = din("bg", [L, 128, 16])
    lam_in = din("lam", [L, 1, 256])
    subg_in = din("subg", [L, 128, 1])
    gq_in = din("gq", [L, 128, 3])
    wuq_in = din("wuq", [L, NQ, 768])
    gkv_in = din("gkv", [L, 128, 2])
    wukv_in = din("wukv", [L, NKV, 1024])
    wbd_in = din("wbd", [L, 1024, D])
    wbm_in = din("wbm", [L, 512, D])
    wout_in = din("wout", [L, D, D])
    lnm_in = din("lnm", [L, 2, D])
    dw1_in = din("dw1", [ND, D, DENSE_FF])
    dw3_in = din("dw3", [ND, D, DENSE_FF])
    dw2_in = din("dw2", [ND, DENSE_FF, D])
    rw_in = din("rw", [max(NM, 1), 1, 8 * D])
    ew1_in = din("ew1", [max(NM, 1), 8, D, EXP_FF])
    ew3_in = din("ew3", [max(NM, 1), 8, D, EXP_FF])
    ew2_in = din("ew2", [max(NM, 1), 8, EXP_FF, D])
    wpg_in = din("wpg", [L, D, D])
    wpp_in = din("wpp", [L, 256, D])
    lnf_in = din("lnf", [L, 2, D])
    cst_in = din("cst", [128, 4])
    out = nc.dram_tensor("out", [S, D], F32, kind="ExternalOutput").ap()

    XT = dscr("XT", [8, 128, S], BF16)
    QD = dscr("QD", [8, 128, S], BF16)
    KD = dscr("KD", [8, 128, S], BF16)
    VD = dscr("VD", [S, 1024], BF16)
    GT = dscr("GT", [16, 128, S], BF16)
    QMN = dscr("QMN", [4, 128, S], BF16)
    KMN = dscr("KMN", [4, 128, S], BF16)
    QR = dscr("QR", [2, 128, S], BF16)
    KR = dscr("KR", [2, 16, S], BF16)
    VM = dscr("VM", [S, 512], BF16)
    OAT = dscr("OAT", [8, 128, S], BF16)
    OBT = dscr("OBT", [8, 64, S], BF16)
    X1 = dscr("X1", [S, D], F32)
    X1T = dscr("X1T", [8, 128, S], BF16)
    EE = dscr("EE", [S, D], F32)
    XR = dscr("XR", [S, D], F32)
    CS = dscr("CS", [2, 128, S], F32)
    BST = dscr("BST", [9, 128, 1024], F32)

    sbc = [0]

    def sb(stack, name, shape, dt):
        sbc[0] += 1
        return stack.enter_context(nc.sbuf_tensor("sb_%s_%d" % (name, sbc[0]), list(shape), dt))

    ident = sb(es, "ident", [128, 128], BF16)
    ones_bf = sb(es, "ones_bf", [128, 128], BF16)
    ones_f = sb(es, "ones_f", [128, 128], F32)
    cst = sb(es, "cst", [128, 4], F32)
    ccol = sb(es, "ccol", [128, 8], F32)
    tabb = sb(es, "tabb", [128, 256], F32)
    gates = sb(es, "gates", [128, NB, 8], F32)
    PSB = []
    PSTT = []
    psn = ["ps%d" % i for i in range(8)]
    psc = [0]

    def alloc_psum(ph, nf, nbf):
        psc[0] += 1
        PSB[:] = [ph.enter_context(nc.psum_tensor("psb%d_%d" % (i, psc[0]), [128, 512], F32)) for i in range(nf)]
        PSTT[:] = [ph.enter_context(nc.psum_tensor("pst%d_%d" % (i, psc[0]), [128, 1024], BF16)) for i in range(nbf)]
    ring = [0]

    def nps():
        i = ring[0] % len(PSB)
        ring[0] += 1
        return PSB[i], psn[i]

    def mm(out_ap, pairs, r, w):
        pairs = list(pairs)

        def fn(e):
            ins = None
            n = len(pairs)
            for i, (l, rh) in enumerate(pairs):
                ins = e.matmul(out_ap, lhsT=l, rhs=rh, start=(i == 0), stop=(i == n - 1))
            return ins
        add("pe", fn, r=r, w=w)

    def mm1(out_ap, l, rh, start, stop, r, w):
        add("pe", lambda e: e.matmul(out_ap, lhsT=l, rhs=rh, start=start, stop=stop), r=r, w=w)

    def dma(eng, out_ap, in_ap, r, w, key):
        add(eng, lambda e: e.dma_start(out=out_ap, in_=in_ap), r=r, w=w, dma=key)

    def act(out_ap, in_ap, func, r, w, bias=None, scale=None):
        kw = {}
        if bias is not None:
            kw["bias"] = bias
        if scale is not None:
            kw["scale"] = scale
        add("act", lambda e: e.activation(out=out_ap, in_=in_ap, func=func, **kw), r=r, w=w)

    def tt(eng, out_ap, a, b, op, r, w):
        add(eng, lambda e: e.tensor_tensor(out=out_ap, in0=a, in1=b, op=op), r=r, w=w)

    def ts(eng, out_ap, a, s1, s2, op0, op1, r, w):
        if op1 is None:
            add(eng, lambda e: e.tensor_scalar(out=out_ap, in0=a, scalar1=s1, scalar2=None, op0=op0), r=r, w=w)
        else:
            add(eng, lambda e: e.tensor_scalar(out=out_ap, in0=a, scalar1=s1, scalar2=s2, op0=op0, op1=op1), r=r, w=w)

    def stt(eng, out_ap, a, sc, b, op0, op1, r, w):
        add(eng, lambda e: e.scalar_tensor_tensor(out=out_ap, in0=a, scalar=sc, in1=b, op0=op0, op1=op1), r=r, w=w)

    def cp(eng, out_ap, in_ap, r, w):
        if eng == "act":
            add("act", lambda e: e.copy(out=out_ap, in_=in_ap), r=r, w=w)
        else:
            add(eng, lambda e: e.tensor_copy(out=out_ap, in_=in_ap), r=r, w=w)

    STGW = 2048
    stg_state = {"tiles": None, "n": 0}
    cast_rot = ("pool", "act", "pool", "dve")

    def wload(dst_ap, src_ap, width, wname, parts=128):
        tiles = stg_state["tiles"]
        k = stg_state["n"]
        stg_state["n"] += 1
        i = k % len(tiles)
        st_ = tiles[i]
        dma("sp", st_[0:parts, 0:width], src_ap, r=[], w=["stg%d" % i], key="stg%d" % i)
        cp(cast_rot[k % 4], dst_ap, st_[0:parts, 0:width], r=["stg%d" % i], w=[wname])

    def rstd_from(out_ap, in_ap, scale, r, w):
        act(out_ap, in_ap, AF.Ln, r=r + ["ccol"], w=w, bias=ccol[:, 0:1], scale=scale)
        act(out_ap, out_ap, AF.Exp, r=w, w=w, scale=-0.5)

    add("pool", lambda e: e.memset(ident[:], 0.0), w=["ident"])
    add("pool", lambda e: e.affine_select(out=ident[:], in_=ident[:], compare_op=ALU.not_equal, fill=1.0,
                                          base=0, pattern=[[-1, 128]], channel_multiplier=1),
        r=["ident"], w=["ident"])
    add("dve", lambda e: e.memset(ones_bf[:], 1.0), w=["ones_bf"])
    add("dve", lambda e: e.memset(ones_f[:], 1.0), w=["ones_f"])
    add("dve", lambda e: e.memset(ccol[:, 0:1], EPS), w=["ccol"])
    add("dve", lambda e: e.memset(ccol[:, 1:2], -math.pi), r=["ccol"], w=["ccol"])
    add("dve", lambda e: e.memset(ccol[:, 2:3], 1.0), r=["ccol"], w=["ccol"])
    add("dve", lambda e: e.memset(ccol[:, 3:4], 0.0), r=["ccol"], w=["ccol"])
    dma("sp", cst[:], cst_in, r=[], w=["cst"], key="cst")
    dma("sp", tabb[:], tab_in.partition_broadcast(128), r=[], w=["tabb"], key="tabb")

    with ExitStack() as ph:
        posi = sb(ph, "posi", [128, S], I32)
        posf = sb(ph, "posf", [128, S], F32)
        ang = sb(ph, "ang", [128, S], F32)
        tmpc = sb(ph, "tmpc", [128, S], F32)
        dma("sp", posi[:], pos_in.partition_broadcast(128), r=[], w=["posi"], key="posi")
        cp("dve", posf[:], posi[:], r=["posi"], w=["posf"])
        ts("dve", ang[:], posf[:], cst[:, 0:1], None, ALU.mult, None, r=["posf", "cst"], w=["ang"])
        C1 = 6.28125
        C2 = 2.0 * math.pi - C1
        PI_IN = 3.1415925
        indc = sb(ph, "indc", [128, S], F32)
        ts("dve", tmpc[:], ang[:], 1.0 / (2.0 * math.pi), None, ALU.mult, None, r=["ang"], w=["tmpc"])
        cp("dve", posi[:], tmpc[:], r=["tmpc", "posf"], w=["posi"])
        cp("dve", posf[:], posi[:], r=["posi", "ang"], w=["posf"])
        stt("dve", tmpc[:], posf[:], -C1, ang[:], ALU.mult, ALU.add, r=["posf", "ang"], w=["tmpc"])
        stt("dve", tmpc[:], posf[:], -C2, tmpc[:], ALU.mult, ALU.add, r=["posf", "tmpc"], w=["tmpc"])

        def wrap(tn, t):
            ts("dve", indc[:], t[:], math.pi, None, ALU.is_gt, None, r=[tn], w=["indc"])
            stt("dve", t[:], indc[:], -2.0 * math.pi, t[:], ALU.mult, ALU.add, r=["indc", tn], w=[tn])
            ts("dve", indc[:], t[:], -math.pi, None, ALU.is_lt, None, r=[tn], w=["indc"])
            stt("dve", t[:], indc[:], 2.0 * math.pi, t[:], ALU.mult, ALU.add, r=["indc", tn], w=[tn])
            ts("dve", t[:], t[:], PI_IN, -PI_IN, ALU.min, ALU.max, r=[tn], w=[tn])
        wrap("tmpc", tmpc)
        ts("dve", ang[:], tmpc[:], 0.5 * math.pi, None, ALU.add, None, r=["tmpc"], w=["ang"])
        wrap("ang", ang)
        act(ang[:], ang[:], AF.Sin, r=["ang"], w=["ang"])
        dma("sp", CS[0], ang[:], r=["ang"], w=[], key="ang")
        act(tmpc[:], tmpc[:], AF.Sin, r=["tmpc"], w=["tmpc"])
        dma("sp", CS[1], tmpc[:], r=["tmpc"], w=[], key="tmpc")
    S_.barrier()
    with ExitStack() as ph:
        nmi = sb(ph, "nmi", [128, 1024], I32)
        nmat = sb(ph, "nmat", [128, 1024], F32)
        ind = sb(ph, "ind", [128, 1024], F32)
        mstrip = sb(ph, "mstrip", [128, 1024], F32)
        dtab = sb(ph, "dtab", [128, 256], F32)
        bst = [sb(ph, "bst%d" % h, [128, 1024], F32) for h in range(8)]
        add("pool", lambda e: e.iota(nmi[:], pattern=[[1, 1024]], base=-384, channel_multiplier=-1), w=["nmi"])
        cp("dve", nmat[:], nmi[:], r=["nmi"], w=["nmat"])
        tt("dve", dtab[:, 8:256], tabb[:, 8:256], tabb[:, 0:248], ALU.subtract, r=["tabb"], w=["dtab"])
        ts("dve", ind[:], nmat[:], 0.0, None, ALU.is_ge, None, r=["nmat"], w=["ind"])
        ts("dve", mstrip[:], ind[:], 1.0, -NEG, ALU.subtract, ALU.mult, r=["ind"], w=["mstrip"])
        for h in range(8):
            ts("pool", bst[h][:], mstrip[:], tabb[:, h:h + 1], None, ALU.add, None, r=["mstrip", "tabb"], w=["bst%d" % h])
        th = t5_thresholds()
        for j in range(1, 32):
            ts("dve", ind[:], nmat[:], float(th[j - 1]), None, ALU.is_ge, None, r=["nmat"], w=["ind"])
            for h in range(8):
                eng = "dve"
                stt(eng, bst[h][:], ind[:], dtab[:, j * 8 + h:j * 8 + h + 1], bst[h][:], ALU.mult, ALU.add,
                    r=["ind", "dtab", "bst%d" % h], w=["bst%d" % h])
        for h in range(8):
            dma("sp", BST[h], bst[h][:], r=["bst%d" % h], w=[], key="bst%d" % h)
        dma("sp", BST[8], mstrip[:], r=["mstrip"], w=[], key="mstrip")
    S_.barrier()
    if upto == 'C':
        S_.emit(nc)
        es.close()
        return nc

    for li in range(L):
        lam_init = 0.8 - 0.6 * math.exp(-0.3 * li)
        x_src = x_in if li == 0 else XR
        x_dst = out if li == L - 1 else XR

        with ExitStack() as ph:
            alloc_psum(ph, 6, 2)
            w1 = sb(ph, "p1w", [128, 8, 3072], BF16)
            xs = [sb(ph, "p1xs%d" % i, [128, 4, 1024], F32) for i in range(2)]
            xb = sb(ph, "p1xb", [128, 4, 1024], BF16)
            xT = [sb(ph, "p1xT%d" % i, [128, 8, 512], BF16) for i in range(2)]
            qd = [sb(ph, "p1qd%d" % i, [128, 8, 512], BF16) for i in range(2)]
            kd = [sb(ph, "p1kd%d" % i, [128, 8, 512], BF16) for i in range(2)]
            vd = [sb(ph, "p1vd%d" % i, [128, 4, 1024], BF16) for i in range(2)]
            stg_state["tiles"] = [sb(ph, "p1stg%d" % i, [128, STGW], F32) for i in range(3)]
            for kb in range(8):
                for c3 in range(0, 3072, STGW):
                    wd = min(STGW, 3072 - c3)
                    wload(w1[:, kb, c3:c3 + wd], w_in[li, kb * 128:(kb + 1) * 128, c3:c3 + wd], wd, "p1w%d_%d" % (kb, c3))
            wn = ["p1w%d_%d" % (kb, c3) for kb in range(8) for c3 in range(0, 3072, STGW)]

            def ldx(t):
                i = t % 2
                dma("sp", xs[i][:], x_src[t * 512:(t + 1) * 512, :].rearrange("(j p) d -> p j d", p=128),
                    r=[], w=["p1xs%d" % i], key="p1xs%d" % i)
            ldx(0)
            for t in range(NT):
                i = t % 2
                if t + 1 < NT:
                    ldx(t + 1)
                import os
                cut = int(os.environ.get("P1CUT", "99"))
                if cut < 1:
                    continue
                for j in range(4):
                    cp("dve" if j % 2 == 0 else "pool", xb[:, j, :], xs[i][:, j, :], r=["p1xs%d" % i], w=["p1xb%d" % j])
                if cut < 2:
                    continue
                for kb in range(8):
                    def tr(e, kb=kb, PSTT=tuple(PSTT)):
                        ins = None
                        for j in range(4):
                            ins = e.transpose(out=PSTT[kb % 2][:, j * 128:(j + 1) * 128],
                                              in_=xb[:, j, kb * 128:(kb + 1) * 128], identity=ident[:])
                        return ins
                    sub_ = os.environ.get("P1SUB", "")
                    if sub_ == "one" and kb > 0:
                        continue
                    add("pe", tr, r=["p1xb%d" % j for j in range(4)] + ["ident"], w=["pst%d" % (kb % 2)])
                    if sub_ == "tr":
                        continue
                    cp("act" if kb % 2 else "dve", xT[i][:, kb, :], PSTT[kb % 2][:, 0:512],
                       r=["pst%d" % (kb % 2)], w=["p1xT%d_%d" % (i, kb)])
                xTn = ["p1xT%d_%d" % (i, kb) for kb in range(8)]
                if cut < 3:
                    continue
                dma("sp", XT[:, :, t * 512:(t + 1) * 512].rearrange("k p s -> p k s"), xT[i][:], r=xTn, w=[], key="p1xT%d" % i)
                if cut < 4:
                    continue
                for (col, dst, dstn, DR, sc) in ((COLQ, qd, "p1qd", QD, 0.125), (COLK, kd, "p1kd", KD, 1.0)):
                    for h in range(8):
                        pt, pn = nps()
                        mm(pt[:], [(w1[:, kb, col + h * 128:col + (h + 1) * 128], xT[i][:, kb, :]) for kb in range(8)],
                           r=wn + xTn, w=[pn])
                        if h % 2 == 0:
                            act(dst[i][:, h, :], pt[:], AF.Copy, r=[pn], w=["%s%d_%d" % (dstn, i, h)], scale=sc)
                        else:
                            ts("dve", dst[i][:, h, :], pt[:], sc, None, ALU.mult, None, r=[pn], w=["%s%d_%d" % (dstn, i, h)])
                    dma("sp", DR[:, :, t * 512:(t + 1) * 512].rearrange("h p s -> p h s"), dst[i][:],
                        r=["%s%d_%d" % (dstn, i, h) for h in range(8)], w=[], key="%s%d" % (dstn, i))
                if cut < 5:
                    continue
                for j in range(4):
                    for hf in range(2):
                        pt, pn = nps()
                        mm(pt[:], [(xT[i][:, kb, j * 128:(j + 1) * 128], w1[:, kb, COLV + hf * 512:COLV + (hf + 1) * 512]) for kb in range(8)],
                           r=wn + xTn, w=[pn])
                        cp("act" if hf else "dve", vd[i][:, j, hf * 512:(hf + 1) * 512], pt[:], r=[pn], w=["p1vd%d_%d_%d" % (i, j, hf)])
                dma("sp", VD[t * 512:(t + 1) * 512, :].rearrange("(j p) e -> p j e", p=128), vd[i][:],
                    r=["p1vd%d_%d_%d" % (i, j, hf) for j in range(4) for hf in range(2)], w=[], key="p1vd%d" % i)
        S_.barrier()
        if upto == 'P1':
            S_.emit(nc)
            es.close()
            return nc

        with ExitStack() as ph:
            alloc_psum(ph, 8, 0)
            w2 = sb(ph, "p2w", [128, 8, INC - 3072], BF16)
            W2O = 3072
            wq_st = sb(ph, "p2wqs", [128, 3, 768], F32)
            wkv_st = sb(ph, "p2wkvs", [128, 2, 1024], F32)
            wq = sb(ph, "p2wq", [128, 3, 768], BF16)
            wkv = sb(ph, "p2wkv", [128, 2, 1024], BF16)
            gq = sb(ph, "p2gq", [128, 3], F32)
            gkv = sb(ph, "p2gkv", [128, 2], F32)
            bg = sb(ph, "p2bg", [128, 16], F32)
            xT = [sb(ph, "p2xT%d" % i, [128, 8, 512], BF16) for i in range(2)]
            cs = [sb(ph, "p2cs%d" % i, [128, 2, 512], F32) for i in range(2)]
            gsb = sb(ph, "p2g", [128, 16, 512], BF16)
            cqb = sb(ph, "p2cqb", [128, 3, 512], BF16)
            sqq = sb(ph, "p2sqq", [128, 3, 512], BF16)
            ckb = sb(ph, "p2ckb", [128, 2, 512], BF16)
            sqk = sb(ph, "p2sqk", [128, 2, 512], BF16)
            rq = sb(ph, "p2rq", [128, 512], F32)
            rk = sb(ph, "p2rk", [128, 512], F32)
            rkc = sb(ph, "p2rkc", [128, 4], F32)
            qn = sb(ph, "p2qn", [128, 4, 512], BF16)
            kn = sb(ph, "p2kn", [128, 4, 512], BF16)
            vm = sb(ph, "p2vm", [128, 4, 512], BF16)
            x1s = sb(ph, "p2x1s", [128, 512], F32)
            x2s = sb(ph, "p2x2s", [128, 512], F32)
            ta = sb(ph, "p2ta", [128, 512], F32)
            tb = sb(ph, "p2tb", [128, 512], F32)
            qr = sb(ph, "p2qr", [128, 2, 512], BF16)
            kr = sb(ph, "p2kr", [16, 2, 512], BF16)
            stg_state["tiles"] = [sb(ph, "p2stg%d" % i, [128, STGW], F32) for i in range(2)]
            W2W = INC - 3072
            for kb in range(8):
                for c3 in range(0, W2W, STGW):
                    wd = min(STGW, W2W - c3)
                    wload(w2[:, kb, c3:c3 + wd], w_in[li, kb * 128:(kb + 1) * 128, 3072 + c3:3072 + c3 + wd], wd, "p2w%d_%d" % (kb, c3))
            wn = ["p2w%d_%d" % (kb, c3) for kb in range(8) for c3 in range(0, W2W, STGW)]
            dma("sp", wq_st[:], wuq_in[li].rearrange("(k p) c -> p k c", p=128), r=[], w=["p2wqs"], key="p2wqs")
            dma("sp", wkv_st[:], wukv_in[li].rearrange("(k p) c -> p k c", p=128), r=[], w=["p2wkvs"], key="p2wkvs")
            dma("sp", gq[:], gq_in[li], r=[], w=["p2gq"], key="p2gq")
            dma("sp", gkv[:], gkv_in[li], r=[], w=["p2gkv"], key="p2gkv")
            dma("sp", bg[:], bg_in[li], r=[], w=["p2bg"], key="p2bg")
            for k in range(3):
                ts("dve", wq[:, k, :], wq_st[:, k, :], gq[:, k:k + 1], None, ALU.mult, None, r=["p2wqs", "p2gq"], w=["p2wq"])
            for k in range(2):
                ts("dve", wkv[:, k, :], wkv_st[:, k, :], gkv[:, k:k + 1], None, ALU.mult, None, r=["p2wkvs", "p2gkv"], w=["p2wkv"])

            def ldt(t):
                i = t % 2
                dma("sp", xT[i][:], XT[:, :, t * 512:(t + 1) * 512].rearrange("k p s -> p k s"), r=[], w=["p2xT%d" % i], key="p2xT%d" % i)
                dma("sp", cs[i][:], CS[:, :, t * 512:(t + 1) * 512].rearrange("c p s -> p c s"), r=[], w=["p2cs%d" % i], key="p2cs%d" % i)
            ldt(0)
            MSC = 96.0 ** -0.5
            for t in range(NT):
                i = t % 2
                if t + 1 < NT:
                    ldt(t + 1)
                xn = ["p2xT%d" % i]
                tsl = slice(t * 512, (t + 1) * 512)
                for gb in range(16):
                    pt, pn = nps()
                    c0 = COLG - W2O + gb * 128
                    mm(pt[:], [(w2[:, kb, c0:c0 + 128], xT[i][:, kb, :]) for kb in range(8)], r=wn + xn, w=[pn])
                    act(gsb[:, gb, :], pt[:], AF.Sigmoid, r=[pn, "p2bg"], w=["p2g%d" % gb], bias=bg[:, gb:gb + 1], scale=1.0)
                dma("sp", GT[:, :, tsl].rearrange("g p s -> p g s"), gsb[:], r=["p2g%d" % gb for gb in range(16)], w=[], key="p2g")
                for b in range(3):
                    pt, pn = nps()
                    c0 = COLCQ - W2O + b * 128
                    mm(pt[:], [(w2[:, kb, c0:c0 + 128], xT[i][:, kb, :]) for kb in range(8)], r=wn + xn, w=[pn])
                    cp("dve", cqb[:, b, :], pt[:], r=[pn], w=["p2cqb%d" % b])
                    act(sqq[:, b, :], pt[:], AF.Square, r=[pn], w=["p2sqq%d" % b])
                pt, pn = nps()
                mm(pt[:], [(ones_bf[:], sqq[:, b, :]) for b in range(3)], r=["ones_bf"] + ["p2sqq%d" % b for b in range(3)], w=[pn])
                rstd_from(rq[:], pt[:], 1.0 / NQ, r=[pn], w=["p2rq"])
                ts("dve", rq[:], rq[:], MSC, None, ALU.mult, None, r=["p2rq"], w=["p2rq"])
                cqn = ["p2cqb%d" % b for b in range(3)]
                for pr in range(4):
                    pt, pn = nps()
                    mm(pt[:], [(wq[:, k, pr * 128:(pr + 1) * 128], cqb[:, k, :]) for k in range(3)], r=["p2wq"] + cqn, w=[pn])
                    tt("dve", qn[:, pr, :], pt[:], rq[:], ALU.mult, r=[pn, "p2rq"], w=["p2qn%d" % pr])
                dma("sp", QMN[:, :, tsl].rearrange("j p s -> p j s"), qn[:], r=["p2qn%d" % pr for pr in range(4)], w=[], key="p2qn")
                pt1, pn1 = nps()
                mm(pt1[:], [(wq[:, k, 512:640], cqb[:, k, :]) for k in range(3)], r=["p2wq"] + cqn, w=[pn1])
                pt2, pn2 = nps()
                mm(pt2[:], [(wq[:, k, 640:768], cqb[:, k, :]) for k in range(3)], r=["p2wq"] + cqn, w=[pn2])
                tt("dve", x1s[:], pt1[:], rq[:], ALU.mult, r=[pn1, "p2rq"], w=["p2x1s"])
                tt("dve", x2s[:], pt2[:], rq[:], ALU.mult, r=[pn2, "p2rq"], w=["p2x2s"])
                csn = "p2cs%d" % i
                tt("dve", ta[:], x1s[:], cs[i][:, 0, :], ALU.mult, r=["p2x1s", csn], w=["p2ta"])
                tt("pool", tb[:], x2s[:], cs[i][:, 1, :], ALU.mult, r=["p2x2s", csn], w=["p2tb"])
                tt("dve", qr[:, 0, :], ta[:], tb[:], ALU.subtract, r=["p2ta", "p2tb"], w=["p2qr0"])
                tt("dve", ta[:], x1s[:], cs[i][:, 1, :], ALU.mult, r=["p2x1s", csn], w=["p2ta"])
                tt("pool", tb[:], x2s[:], cs[i][:, 0, :], ALU.mult, r=["p2x2s", csn], w=["p2tb"])
                tt("dve", qr[:, 1, :], ta[:], tb[:], ALU.add, r=["p2ta", "p2tb"], w=["p2qr1"])
                dma("sp", QR[:, :, tsl].rearrange("c p s -> p c s"), qr[:], r=["p2qr0", "p2qr1"], w=[], key="p2qr")
                for b in range(2):
                    pt, pn = nps()
                    c0 = COLCKV - W2O + b * 128
                    mm(pt[:], [(w2[:, kb, c0:c0 + 128], xT[i][:, kb, :]) for kb in range(8)], r=wn + xn, w=[pn])
                    cp("dve", ckb[:, b, :], pt[:], r=[pn], w=["p2ckb%d" % b])
                    act(sqk[:, b, :], pt[:], AF.Square, r=[pn], w=["p2sqk%d" % b])
                sqn = ["p2sqk%d" % b for b in range(2)]
                pt, pn = nps()
                mm(pt[:], [(ones_bf[:], sqk[:, b, :]) for b in range(2)], r=["ones_bf"] + sqn, w=[pn])
                rstd_from(rk[:], pt[:], 1.0 / NKV, r=[pn], w=["p2rk"])
                ptc, pnc = nps()
                for j in range(4):
                    mm(ptc[:, j:j + 1], [(sqk[:, b, j * 128:(j + 1) * 128], ones_bf[:, 0:1]) for b in range(2)], r=["ones_bf"] + sqn, w=[pnc])
                rstd_from(rkc[:], ptc[:, 0:4], 1.0 / NKV, r=[pnc], w=["p2rkc"])
                ckn = ["p2ckb%d" % b for b in range(2)]
                for pr in range(4):
                    pt, pn = nps()
                    mm(pt[:], [(wkv[:, k, pr * 128:(pr + 1) * 128], ckb[:, k, :]) for k in range(2)], r=["p2wkv"] + ckn, w=[pn])
                    tt("dve", kn[:, pr, :], pt[:], rk[:], ALU.mult, r=[pn, "p2rk"], w=["p2kn%d" % pr])
                dma("sp", KMN[:, :, tsl].rearrange("j p s -> p j s"), kn[:], r=["p2kn%d" % pr for pr in range(4)], w=[], key="p2kn")
                for j in range(4):
                    pt, pn = nps()
                    mm(pt[:], [(ckb[:, k, j * 128:(j + 1) * 128], wkv[:, k, 512:1024]) for k in range(2)], r=["p2wkv"] + ckn, w=[pn])
                    act(vm[:, j, :], pt[:], AF.Copy, r=[pn, "p2rkc"], w=["p2vm%d" % j], scale=rkc[:, j:j + 1])
                dma("sp", VM[tsl, :].rearrange("(j p) e -> p j e", p=128), vm[:], r=["p2vm%d" % j for j in range(4)], w=[], key="p2vm")
                pt1, pn1 = nps()
                c0 = COLKR - W2O
                mm(pt1[0:16, :], [(w2[:, kb, c0:c0 + 16], xT[i][:, kb, :]) for kb in range(8)], r=wn + xn, w=[pn1])
                pt2, pn2 = nps()
                mm(pt2[0:16, :], [(w2[:, kb, c0 + 16:c0 + 32], xT[i][:, kb, :]) for kb in range(8)], r=wn + xn, w=[pn2])
                tt("dve", ta[0:16, :], pt1[0:16, :], cs[i][0:16, 0, :], ALU.mult, r=[pn1, csn], w=["p2ta"])
                tt("dve", tb[0:16, :], pt2[0:16, :], cs[i][0:16, 1, :], ALU.mult, r=[pn2, csn], w=["p2tb"])
                tt("dve", kr[:, 0, :], ta[0:16, :], tb[0:16, :], ALU.subtract, r=["p2ta", "p2tb"], w=["p2kr0"])
                tt("dve", ta[0:16, :], pt1[0:16, :], cs[i][0:16, 1, :], ALU.mult, r=[pn1, csn], w=["p2ta"])
                tt("dve", tb[0:16, :], pt2[0:16, :], cs[i][0:16, 0, :], ALU.mult, r=[pn2, csn], w=["p2tb"])
                tt("dve", kr[:, 1, :], ta[0:16, :], tb[0:16, :], ALU.add, r=["p2ta", "p2tb"], w=["p2kr1"])
                dma("sp", KR[:, :, tsl].rearrange("c p s -> p c s"), kr[:], r=["p2kr0", "p2kr1"], w=[], key="p2kr")
        S_.barrier()
        if upto == 'P2':
            S_.emit(nc)
            es.close()
            return nc

        with ExitStack() as ph:
            alloc_psum(ph, 8, 0)
            bstr = sb(ph, "abst", [128, 9, 1024], F32)
            lamt = sb(ph, "alam", [128, 256], F32)
            lamp = sb(ph, "alamp", [128, 128], F32)
            lamc = sb(ph, "alamc", [128, 4], F32)
            subg = sb(ph, "asubg", [128, 1], F32)
            kdt = [sb(ph, "akd%d" % i, [128, S], BF16) for i in range(2)]
            vdt = [sb(ph, "avd%d" % i, [128, NB, 128], BF16) for i in range(2)]
            vmt = [sb(ph, "avm%d" % i, [128, NB, 65], BF16) for i in range(2)]
            qt = [sb(ph, "aq%d" % i, [128, 512], BF16) for i in range(2)]
            pT = [sb(ph, "apT%d" % i, [128, 512], BF16) for i in range(4)]
            sbias = [sb(ph, "asb%d" % i, [128, 512], F32) for i in range(2)]
            rr = sb(ph, "arr", [128, 512], F32)
            rr2 = sb(ph, "arr2", [1, 512], F32)
            Rb = [sb(ph, "aRb%d" % i, [128, 512], F32) for i in range(2)]
            t0 = sb(ph, "at0", [128, 512], F32)
            t1 = sb(ph, "at1", [128, 512], F32)
            osq = sb(ph, "aosq", [128, 512], BF16)
            rms = sb(ph, "arms", [128, 512], F32)
            oo = [sb(ph, "aoo%d" % i, [128, 512], BF16) for i in range(2)]
            dma("sp", bstr[:], BST.rearrange("h p n -> p h n"), r=[], w=["abst"], key="abst")
            dma("sp", lamt[:], lam_in[li].partition_broadcast(128), r=[], w=["alam"], key="alam")
            dma("sp", subg[:], subg_in[li], r=[], w=["asubg"], key="asubg")
            tt("dve", lamp[:, 0:64], lamt[:, 0:64], lamt[:, 64:128], ALU.mult, r=["alam"], w=["alamp"])
            tt("dve", lamp[:, 64:128], lamt[:, 128:192], lamt[:, 192:256], ALU.mult, r=["alam", "alamp"], w=["alamp"])
            add("dve", lambda e: e.reduce_sum(out=lamc[:, 0:1], in_=lamp[:, 0:64], axis=AX.X), r=["alamp"], w=["alamc"])
            add("dve", lambda e: e.reduce_sum(out=lamc[:, 1:2], in_=lamp[:, 64:128], axis=AX.X), r=["alamp", "alamc"], w=["alamc"])
            act(lamc[:, 0:2], lamc[:, 0:2], AF.Exp, r=["alamc"], w=["alamc"])
            tt("dve", lamc[:, 2:3], lamc[:, 1:2], lamc[:, 0:1], ALU.subtract, r=["alamc"], w=["alamc"])
            ts("dve", lamc[:, 2:3], lamc[:, 2:3], -lam_init, None, ALU.add, None, r=["alamc"], w=["alamc"])
            for i in range(2):
                add("pool", lambda e, i=i: e.memset(vmt[i][:, :, 64:65], 1.0), w=["avm1_%d" % i])
            PS_S = [(PSB[i], psn[i]) for i in range(4)]
            NPS = 4
            PO = [(PSB[4], psn[4]), (PSB[5], psn[5])]
            PSM = [(PSB[6], psn[6]), (PSB[7], psn[7])]
            sctr = [0]
            pctr = [0]
            for hh in range(16):
                isd = hh < 8
                h = hh % 8
                i = hh % 2
                if isd:
                    dma("sp", kdt[i][:], KD[h], r=[], w=["akd%d" % i], key="akd%d" % i)
                    dma("sp", vdt[i][:], VD[:, h * 128:(h + 1) * 128].rearrange("(b p) e -> p b e", p=128), r=[], w=["avd%d" % i], key="avd%d" % i)
                else:
                    pr, hp = h // 2, h % 2
                    dma("sp", kdt[i][0:64, :], KMN[pr, hp * 64:(hp + 1) * 64, :], r=[], w=["akd%d" % i], key="akd%d" % i)
                    dma("sp", kdt[i][64:80, :], KR[0], r=[], w=["akdr1_%d" % i], key="akdr1_%d" % i)
                    dma("sp", kdt[i][80:96, :], KR[1], r=[], w=["akdr2_%d" % i], key="akdr2_%d" % i)
                    dma("sp", vmt[i][:, :, 0:64], VM[:, h * 64:(h + 1) * 64].rearrange("(b p) e -> p b e", p=128), r=[], w=["avm%d" % i], key="avm%d" % i)
                kn_ = ["akd%d" % i] + ([] if isd else ["akdr1_%d" % i, "akdr2_%d" % i])
                vn_ = ["avd%d" % i] if isd else ["avm%d" % i, "avm1_%d" % i]
                for t in range(NT):
                    qi = (hh * NT + t) % 2
                    tsl = slice(t * 512, (t + 1) * 512)
                    if isd:
                        dma("sp", qt[qi][:], QD[h, :, tsl], r=[], w=["aq%d" % qi], key="aq%d" % qi)
                        qn_ = ["aq%d" % qi]
                    else:
                        pr, hp = h // 2, h % 2
                        dma("sp", qt[qi][0:64, :], QMN[pr, hp * 64:(hp + 1) * 64, tsl], r=[], w=["aq%d" % qi], key="aq%d" % qi)
                        dma("sp", qt[qi][64:80, :], QR[0, h * 16:(h + 1) * 16, tsl], r=[], w=["aqr1_%d" % qi], key="aqr1_%d" % qi)
                        dma("sp", qt[qi][80:96, :], QR[1, h * 16:(h + 1) * 16, tsl], r=[], w=["aqr2_%d" % qi], key="aqr2_%d" % qi)
                        qn_ = ["aq%d" % qi, "aqr1_%d" % qi, "aqr2_%d" % qi]
                    nkb = 4 * t + 4
                    maps = (0, 1) if isd else (0,)

                    def stage1(kb):
                        delta = 512 * t - 128 * kb
                        c0 = max(0, -delta)
                        near = delta < 256
                        ksl = slice(kb * 128, (kb + 1) * 128)
                        res_ = []
                        for m in maps:
                            pt, pn = PS_S[sctr[0] % NPS]
                            sctr[0] += 1
                            pb = pT[pctr[0] % 4]
                            pbn = "apT%d" % (pctr[0] % 4)
                            pctr[0] += 1
                            rows = slice(m * 64, (m + 1) * 64) if isd else slice(0, 96)
                            mm1(pt[:, c0:512], kdt[i][rows, ksl], qt[qi][rows, c0:512], True, True, r=kn_ + qn_, w=[pn])
                            if isd and near:
                                sbt = sbias[m]
                                tt("dve", sbt[:, c0:512], pt[:, c0:512], bstr[:, h, delta + 384 + c0:delta + 384 + 512], ALU.add,
                                   r=[pn, "abst"], w=["asb%d" % m])
                                act(pb[:, c0:512], sbt[:, c0:512], AF.Exp, r=["asb%d" % m], w=[pbn])
                            elif isd:
                                act(pb[:, c0:512], pt[:, c0:512], AF.Exp, r=[pn, "tabb"], w=[pbn], bias=tabb[:, 248 + h:249 + h], scale=1.0)
                            elif delta <= 0:
                                sbt = sbias[0]
                                tt("dve", sbt[:, c0:512], pt[:, c0:512], bstr[:, 8, delta + 384 + c0:delta + 384 + 512], ALU.add,
                                   r=[pn, "abst"], w=["asb0"])
                                act(pb[:, c0:512], sbt[:, c0:512], AF.Exp, r=["asb0"], w=[pbn])
                            else:
                                act(pb[:, c0:512], pt[:, c0:512], AF.Exp, r=[pn], w=[pbn])
                            res_.append((pb, pbn, c0))
                        return res_

                    def stage2(kb, res_):
                        first, last = kb == 0, kb == nkb - 1
                        for m, (pb, pbn, c0) in zip(maps, res_):
                            if isd:
                                mm1(PO[m][0][:, c0:512], vdt[i][:, kb, :], pb[:, c0:512], first, last, r=vn_ + [pbn], w=[PO[m][1]])
                                mm1(PSM[m][0][0:1, c0:512], ones_bf[:, 0:1], pb[:, c0:512], first, last,
                                    r=["ones_bf", pbn], w=[PSM[m][1]])
                            else:
                                mm1(PO[0][0][0:65, c0:512], vmt[i][:, kb, :], pb[:, c0:512], first, last, r=vn_ + [pbn], w=[PO[0][1]])
                    cur = stage1(0)
                    for kb in range(nkb):
                        nxt = stage1(kb + 1) if kb + 1 < nkb else None
                        stage2(kb, cur)
                        cur = nxt
                    oi = (hh * NT + t) % 2
                    if isd:
                        add("dve", lambda e: e.reciprocal(out=rr[0:1, :], in_=PSM[0][0][0:1, :]), r=[PSM[0][1]], w=["arr0"])
                        add("dve", lambda e: e.reciprocal(out=rr2[0:1, :], in_=PSM[1][0][0:1, :]), r=[PSM[1][1]], w=["arr1"])
                        for m in range(2):
                            pt, pn = PS_S[sctr[0] % NPS]
                            sctr[0] += 1
                            rrm = rr if m == 0 else rr2
                            mm1(pt[:], ones_f[0:1, :], rrm[0:1, :], True, True, r=["ones_f", "arr%d" % m], w=[pn])
                            cp("act", Rb[m][:], pt[:], r=[pn], w=["aRb%d" % m])
                        tt("dve", t0[:], PO[0][0][:], Rb[0][:], ALU.mult, r=[PO[0][1], "aRb0"], w=["at0"])
                        tt("dve", t1[:], PO[1][0][:], Rb[1][:], ALU.mult, r=[PO[1][1], "aRb1"], w=["at1"])
                        stt("dve", t0[:], t1[:], lamc[:, 2:3], t0[:], ALU.mult, ALU.add, r=["at0", "at1", "alamc"], w=["at0"])
                        act(osq[:], t0[:], AF.Square, r=["at0"], w=["aosq"])
                        pt, pn = PS_S[sctr[0] % NPS]
                        sctr[0] += 1
                        mm1(pt[:], ones_bf[:], osq[:], True, True, r=["ones_bf", "aosq"], w=[pn])
                        rstd_from(rms[:], pt[:], 1.0 / 128.0, r=[pn], w=["arms"])
                        stt("dve", t1[:], t0[:], subg[:, 0:1], rms[:], ALU.mult, ALU.mult, r=["at0", "asubg", "arms"], w=["at1"])
                        ts("dve", oo[oi][:], t1[:], 1.0 - lam_init, None, ALU.mult, None, r=["at1"], w=["aoo%d" % oi])
                        dma("sp", OAT[h, :, tsl], oo[oi][:], r=["aoo%d" % oi], w=[], key="aoo%d" % oi)
                    else:
                        add("dve", lambda e: e.reciprocal(out=rr[64:65, :], in_=PO[0][0][64:65, :]), r=[PO[0][1]], w=["arr0"])
                        pt, pn = PS_S[sctr[0] % NPS]
                        sctr[0] += 1
                        mm1(pt[0:64, :], ones_f[64:65, 0:64], rr[64:65, :], True, True, r=["ones_f", "arr0"], w=[pn])
                        cp("act", Rb[0][0:64, :], pt[0:64, :], r=[pn], w=["aRb0"])
                        tt("dve", oo[oi][0:64, :], PO[0][0][0:64, :], Rb[0][0:64, :], ALU.mult, r=[PO[0][1], "aRb0"], w=["aoo%d" % oi])
                        dma("sp", OBT[h, :, tsl], oo[oi][0:64, :], r=["aoo%d" % oi], w=[], key="aoo%d" % oi)
        S_.barrier()
        if upto == 'A':
            S_.emit(nc)
            es.close()
            return nc

        with ExitStack() as ph:
            alloc_psum(ph, 6, 2)
            wbd = sb(ph, "mwbd", [128, 8, D], BF16)
            wbm = sb(ph, "mwbm", [64, 8, D], BF16)
            wout = sb(ph, "mwout", [128, 8, D], BF16)
            lng = sb(ph, "mlng", [128, 2, D], F32)
            oa = [sb(ph, "moa%d" % i, [128, 8, 512], BF16) for i in range(2)]
            ob = [sb(ph, "mob%d" % i, [64, 8, 512], BF16) for i in range(2)]
            gs = [sb(ph, "mg%d" % i, [128, 16, 512], BF16) for i in range(2)]
            xs = [sb(ph, "mxs%d" % i, [128, 4, D], F32) for i in range(2)]
            mT = sb(ph, "mmT", [128, 8, 512], BF16)
            tA = [sb(ph, "mtA%d" % i, [128, 512], F32) for i in range(2)]
            tB = [sb(ph, "mtB%d" % i, [128, 512], F32) for i in range(2)]
            zs = [sb(ph, "mzs%d" % i, [128, D], F32) for i in range(2)]
            x1 = [sb(ph, "mx1%d" % i, [128, D], F32) for i in range(2)]
            x1b = sb(ph, "mx1b", [128, D], BF16)
            x1T = sb(ph, "mx1T", [128, 8, 512], BF16)
            st = sb(ph, "mst", [128, 12], F32)
            mv = sb(ph, "mmv", [128, 2], F32)
            rs = sb(ph, "mrs", [128, 1], F32)
            stg_state["tiles"] = [sb(ph, "mstg%d" % i, [128, 1024], F32) for i in range(2)]
            for k in range(8):
                wload(wbd[:, k, :], wbd_in[li, k * 128:(k + 1) * 128, :], 1024, "mwbd")
                wload(wbm[:, k, :], wbm_in[li, k * 64:(k + 1) * 64, :], 1024, "mwbm", parts=64)
                wload(wout[:, k, :], wout_in[li, k * 128:(k + 1) * 128, :], 1024, "mwout")
            dma("sp", lng[:].rearrange("p a d -> p (a d)"), lnm_in[li].rearrange("a d -> (a d)").partition_broadcast(128), r=[], w=["mlng"], key="mlng")

            def ldm(t):
                i = t % 2
                tsl = slice(t * 512, (t + 1) * 512)
                dma("sp", oa[i][:], OAT[:, :, tsl].rearrange("h p s -> p h s"), r=[], w=["moa%d" % i], key="moa%d" % i)
                dma("sp", ob[i][:], OBT[:, :, tsl].rearrange("h p s -> p h s"), r=[], w=["mob%d" % i], key="mob%d" % i)
                dma("sp", gs[i][:], GT[:, :, tsl].rearrange("g p s -> p g s"), r=[], w=["mg%d" % i], key="mg%d" % i)
                dma("sp", xs[i][:], x_src[tsl, :].rearrange("(j p) d -> p j d", p=128), r=[], w=["mxs%d" % i], key="mxs%d" % i)
            ldm(0)
            for t in range(NT):
                i = t % 2
                tsl = slice(t * 512, (t + 1) * 512)
                if t + 1 < NT:
                    ldm(t + 1)
                for db in range(8):
                    dsl = slice(db * 128, (db + 1) * 128)
                    ptA, pnA = nps()
                    mm(ptA[:], [(wbd[:, h, dsl], oa[i][:, h, :]) for h in range(8)], r=["mwbd", "moa%d" % i], w=[pnA])
                    ptB, pnB = nps()
                    mm(ptB[:], [(wbm[:, h, dsl], ob[i][:, h, :]) for h in range(8)], r=["mwbm", "mob%d" % i], w=[pnB])
                    a = db % 2
                    tt("dve", tA[a][:], ptA[:], gs[i][:, db, :], ALU.mult, r=[pnA, "mg%d" % i], w=["mtA%d" % a])
                    tt("dve", tB[a][:], ptB[:], gs[i][:, 8 + db, :], ALU.mult, r=[pnB, "mg%d" % i], w=["mtB%d" % a])
                    tt("pool", mT[:, db, :], tA[a][:], tB[a][:], ALU.add, r=["mtA%d" % a, "mtB%d" % a], w=["mmT%d" % db])
                mTn = ["mmT%d" % db for db in range(8)]
                for j in range(4):
                    a = j % 2
                    for hf in range(2):
                        hsl = slice(hf * 512, (hf + 1) * 512)
                        pt, pn = nps()
                        mm(pt[:], [(mT[:, db, j * 128:(j + 1) * 128], wout[:, db, hsl]) for db in range(8)], r=["mwout"] + mTn, w=[pn])
                        stt("dve", zs[a][:, hsl], xs[i][:, j, hsl], ALPHA, pt[:], ALU.mult, ALU.add, r=["mxs%d" % i, pn], w=["mzs%d_%d" % (a, hf)])
                    zn = ["mzs%d_0" % a, "mzs%d_1" % a]
                    layernorm(add, ts, tt, act, rstd_from, zs[a], x1[a], st, mv, rs, lng, zn, "mx1%d" % a, "m")
                    dma("sp", X1[t * 512 + j * 128:t * 512 + (j + 1) * 128, :], x1[a][:], r=["mx1%d" % a], w=[], key="mx1%d" % a)
                    cp("pool", x1b[:], x1[a][:], r=["mx1%d" % a], w=["mx1b"])
                    for kq in range(2):
                        def tr(e, kq=kq, PSTT=tuple(PSTT)):
                            ins = None
                            for k4 in range(4):
                                kb = kq * 4 + k4
                                ins = e.transpose(out=PSTT[kq][:, k4 * 128:(k4 + 1) * 128],
                                                  in_=x1b[:, kb * 128:(kb + 1) * 128], identity=ident[:])
                            return ins
                        add("pe", tr, r=["mx1b", "ident"], w=["pst%d" % kq])
                        add("act", lambda e, kq=kq, j=j, src=PSTT[kq]: e.copy(out=x1T[:, kq * 4:(kq + 1) * 4, j * 128:(j + 1) * 128],
                                                               in_=src[:, 0:512].rearrange("p (k s) -> p k s", k=4)),
                            r=["pst%d" % kq], w=["mx1T_%d_%d" % (j, kq)])
                dma("sp", X1T[:, :, tsl].rearrange("k p s -> p k s"), x1T[:], r=["mx1T_%d_%d" % (j, kq) for j in range(4) for kq in range(2)], w=[], key="mx1T")
        S_.barrier()
        if upto == 'M':
            S_.emit(nc)
            es.close()
            return nc

        with ExitStack() as ph:
            alloc_psum(ph, 6, 2)
            wpg = sb(ph, "ewpg", [128, 8, D], BF16)
            wpp = sb(ph, "ewpp", [128, 2, D], BF16)
            xT = [sb(ph, "exT%d" % i, [128, 8, 512], BF16) for i in range(2)]
            pp = [sb(ph, "epp%d" % i, [128, 4, 256], F32) for i in range(2)]
            ppb = sb(ph, "eppb", [128, 4, 256], BF16)
            ppT = sb(ph, "eppT", [128, 2, 512], BF16)
            sg = [sb(ph, "esg%d" % i, [128, 512], F32) for i in range(2)]
            ee = [sb(ph, "eee%d" % i, [128, 4, D], F32) for i in range(2)]
            stg_state["tiles"] = [sb(ph, "estg%d" % i, [128, 1024], F32) for i in range(2)]
            for k in range(8):
                wload(wpg[:, k, :], wpg_in[li, k * 128:(k + 1) * 128, :], 1024, "ewpg")
            for k in range(2):
                wload(wpp[:, k, :], wpp_in[li, k * 128:(k + 1) * 128, :], 1024, "ewpp")

            def lde(t):
                i = t % 2
                tsl = slice(t * 512, (t + 1) * 512)
                dma("sp", xT[i][:], X1T[:, :, tsl].rearrange("k p s -> p k s"), r=[], w=["exT%d" % i], key="exT%d" % i)
                dma("sp", pp[i][:], p_in[li, tsl, :].rearrange("(j p) c -> p j c", p=128), r=[], w=["epp%d" % i], key="epp%d" % i)
            lde(0)
            for t in range(NT):
                i = t % 2
                tsl = slice(t * 512, (t + 1) * 512)
                if t + 1 < NT:
                    lde(t + 1)
                cp("pool", ppb[:], pp[i][:], r=["epp%d" % i], w=["eppb"])
                for kq in range(2):
                    def tr(e, kq=kq, PSTT=tuple(PSTT)):
                        ins = None
                        for j in range(4):
                            ins = e.transpose(out=PSTT[kq][:, j * 128:(j + 1) * 128],
                                              in_=ppb[:, j, kq * 128:(kq + 1) * 128], identity=ident[:])
                        return ins
                    add("pe", tr, r=["eppb", "ident"], w=["pst%d" % kq])
                    cp("act", ppT[:, kq, :], PSTT[kq][:, 0:512], r=["pst%d" % kq], w=["eppT%d" % kq])
                for j in range(4):
                    for hf in range(2):
                        hsl = slice(hf * 512, (hf + 1) * 512)
                        a = hf
                        ptA, pnA = nps()
                        mm(ptA[:], [(xT[i][:, kb, j * 128:(j + 1) * 128], wpg[:, kb, hsl]) for kb in range(8)], r=["ewpg", "exT%d" % i], w=[pnA])
                        ptB, pnB = nps()
                        mm(ptB[:], [(ppT[:, kq, j * 128:(j + 1) * 128], wpp[:, kq, hsl]) for kq in range(2)], r=["ewpp", "eppT0", "eppT1"], w=[pnB])
                        act(sg[a][:], ptA[:], AF.Sigmoid, r=[pnA], w=["esg%d" % a])
                        tt("dve", ee[i][:, j, hsl], sg[a][:], ptB[:], ALU.mult, r=["esg%d" % a, pnB], w=["eee%d_%d_%d" % (i, j, hf)])
                dma("sp", EE[tsl, :].rearrange("(j p) d -> p j d", p=128), ee[i][:],
                    r=["eee%d_%d_%d" % (i, j, hf) for j in range(4) for hf in range(2)], w=[], key="eee%d" % i)
        S_.barrier()
        if upto == 'E':
            S_.emit(nc)
            es.close()
            return nc

        moe = (li % 2 == 1)
        lj = li // 2
        if moe:
            with ExitStack() as ph2:
                rwb = sb(ph2, "frw", [128, 8, D], F32)
                rx1 = [sb(ph2, "rx1%d" % i, [128, D], F32) for i in range(2)]
                lg = sb(ph2, "flg", [128, 8], F32)
                junk = sb(ph2, "fjunk", [128, D], F32)
                mx8 = sb(ph2, "fmx8", [128, 8], F32)
                msk = sb(ph2, "fmsk", [128, 8], F32)
                ex = sb(ph2, "fex", [128, 8], F32)
                den = sb(ph2, "fden", [128, 2], F32)
                dma("sp", rwb[:].rearrange("p a d -> p (a d)"), rw_in[lj].partition_broadcast(128), r=[], w=["frw"], key="frw")
                for sbi in range(NB):
                    a = sbi % 2
                    dma("sp", rx1[a][:], X1[sbi * 128:(sbi + 1) * 128, :], r=[], w=["rx1%d" % a], key="rx1%d" % a)
                    for ex_ in range(8):
                        add("dve", lambda e, a=a, ex_=ex_: e.scalar_tensor_tensor(out=junk[:], in0=rx1[a][:], scalar=1.0, in1=rwb[:, ex_, :], op0=ALU.mult, op1=ALU.mult, accum_out=lg[:, ex_:ex_ + 1]),
                            r=["rx1%d" % a, "frw", "flg", "fjunk"], w=["flg", "fjunk"])
                    add("dve", lambda e: e.max(out=mx8[:], in_=lg[:]), r=["flg"], w=["fmx8"])
                    ts("dve", msk[:], lg[:], mx8[:, 1:2], None, ALU.is_ge, None, r=["flg", "fmx8"], w=["fmsk"])
                    ts("dve", ex[:], lg[:], mx8[:, 0:1], None, ALU.subtract, None, r=["flg", "fmx8"], w=["fex"])
                    act(ex[:], ex[:], AF.Exp, r=["fex"], w=["fex"])
                    tt("dve", ex[:], ex[:], msk[:], ALU.mult, r=["fex", "fmsk"], w=["fex"])
                    add("dve", lambda e: e.reduce_sum(out=den[:, 0:1], in_=ex[:], axis=AX.X), r=["fex"], w=["fden"])
                    add("dve", lambda e: e.reciprocal(out=den[:, 1:2], in_=den[:, 0:1]), r=["fden"], w=["fden"])
                    ts("dve", gates[:, sbi, :], ex[:], den[:, 1:2], None, ALU.mult, None, r=["fex", "fden"], w=["gates"])
            S_.barrier()

        with ExitStack() as ph:
            alloc_psum(ph, 8, 0)
            NSUB = TG // 128
            NCB = 7
            Y = sb(ph, "fY", [128, NSUB, D], F32)
            xT = sb(ph, "fxT", [128, 8, TG], BF16)
            wa = [sb(ph, "fwa%d" % i, [128, 8, NCB * 128], BF16) for i in range(2)]
            wb = [sb(ph, "fwb%d" % i, [128, 8, NCB * 128], BF16) for i in range(2)]
            wc = [sb(ph, "fwc%d" % i, [128, NCB, D], BF16) for i in range(2)]
            GTt = [sb(ph, "fG%d" % i, [128, NCB, 512], BF16) for i in range(2)]
            sl = [sb(ph, "fsl%d" % i, [128, 512], F32) for i in range(2)]
            x1 = sb(ph, "fx1", [128, D], F32)
            zz = sb(ph, "fzz", [128, D], F32)
            xo = sb(ph, "fxo", [128, D], F32)
            lng = sb(ph, "flng", [128, 2, D], F32)
            st = sb(ph, "fst", [128, 12], F32)
            mv = sb(ph, "fmv", [128, 2], F32)
            rs = sb(ph, "frs", [128, 1], F32)
            stg_state["tiles"] = [sb(ph, "fstg%d" % i, [128, 1024], F32) for i in range(3)]
            dma("sp", lng[:].rearrange("p a d -> p (a d)"), lnf_in[li].rearrange("a d -> (a d)").partition_broadcast(128), r=[], w=["flng"], key="flng")
            FF = EXP_FF if moe else DENSE_FF
            nblk = FF // 128
            chunks = []
            b0 = 0
            while b0 < nblk:
                nb_ = min(NCB, nblk - b0)
                if nblk - b0 - nb_ in (1, 2) and nb_ > 3:
                    nb_ -= 2
                chunks.append((b0, nb_))
                b0 += nb_
            nexp = 8 if moe else 1
            seq = [(g, ex_, c) for g in range(NG) for ex_ in range(nexp) for c in range(len(chunks))]

            def wjobs(k):
                g, ex_, c = seq[k]
                i = k % 2
                b0, nb_ = chunks[c]
                if moe:
                    s1, s3, s2 = ew1_in[lj, ex_], ew3_in[lj, ex_], ew2_in[lj, ex_]
                else:
                    s1, s3, s2 = dw1_in[lj], dw3_in[lj], dw2_in[lj]
                jobs = []
                wdt = nb_ * 128
                for kb in range(8):
                    jobs.append(lambda kb=kb: wload(wa[i][:, kb, 0:wdt], s1[kb * 128:(kb + 1) * 128, b0 * 128:b0 * 128 + wdt], wdt, "fwa%d_%d" % (i, kb)))
                    jobs.append(lambda kb=kb: wload(wb[i][:, kb, 0:wdt], s3[kb * 128:(kb + 1) * 128, b0 * 128:b0 * 128 + wdt], wdt, "fwb%d_%d" % (i, kb)))
                for jb in range(nb_):
                    jobs.append(lambda jb=jb: wload(wc[i][:, jb, :], s2[(b0 + jb) * 128:(b0 + jb + 1) * 128, :], 1024, "fwc%d_%d" % (i, jb)))
                return jobs
            for jfn in wjobs(0):
                jfn()
            gctr = [0]
            for k, (g, ex_, c) in enumerate(seq):
                i = k % 2
                b0, nb_ = chunks[c]
                gsl = slice(g * TG, (g + 1) * TG)
                if ex_ == 0 and c == 0:
                    dma("sp", xT[:], X1T[:, :, gsl].rearrange("k p s -> p k s"), r=[], w=["fxT"], key="fxT")
                    dma("sp", Y[:], EE[gsl, :].rearrange("(j p) d -> p j d", p=128), r=[], w=["fY%d" % s_ for s_ in range(NSUB)], key="fY")
                pend = wjobs(k + 1) if k + 1 < len(seq) else []
                pend.reverse()

                def pump(n=1):
                    for _ in range(n):
                        if pend:
                            pend.pop()()
                wan = ["fwa%d_%d" % (i, kb) for kb in range(8)]
                wbn = ["fwb%d_%d" % (i, kb) for kb in range(8)]
                for tl in range(TG // 512):
                    gi = gctr[0] % 2
                    gctr[0] += 1
                    tsl = slice(tl * 512, (tl + 1) * 512)
                    for jb in range(nb_):
                        fsl = slice(jb * 128, (jb + 1) * 128)
                        pump(1)
                        pt1, pn1 = nps()
                        mm(pt1[:], [(wa[i][:, kb, fsl], xT[:, kb, tsl]) for kb in range(8)], r=wan + ["fxT"], w=[pn1])
                        pt3, pn3 = nps()
                        mm(pt3[:], [(wb[i][:, kb, fsl], xT[:, kb, tsl]) for kb in range(8)], r=wbn + ["fxT"], w=[pn3])
                        a = jb % 2
                        act(sl[a][:], pt1[:], AF.Silu, r=[pn1], w=["fsl%d" % a])
                        tt("dve", GTt[gi][:, jb, :], sl[a][:], pt3[:], ALU.mult, r=["fsl%d" % a, pn3], w=["fG%d_%d" % (gi, jb)])
                    Gn = ["fG%d_%d" % (gi, jb) for jb in range(nb_)]
                    wcn = ["fwc%d_%d" % (i, jb) for jb in range(nb_)]
                    for j4 in range(4):
                        sub = tl * 4 + j4
                        sbi = g * NSUB + sub
                        for hf in range(2):
                            hsl = slice(hf * 512, (hf + 1) * 512)
                            pump(1)
                            pt, pn = nps()
                            mm(pt[:], [(GTt[gi][:, jb, j4 * 128:(j4 + 1) * 128], wc[i][:, jb, hsl]) for jb in range(nb_)], r=wcn + Gn, w=[pn])
                            gcol = gates[:, sbi, ex_:ex_ + 1] if moe else ccol[:, 2:3]
                            stt("dve", Y[:, sub, hsl], pt[:], gcol, Y[:, sub, hsl], ALU.mult, ALU.add,
                                r=[pn, "fY%d" % sub, "gates", "ccol"], w=["fY%d" % sub])
                pump(100)
                if ex_ == nexp - 1 and c == len(chunks) - 1:
                    for sub in range(NSUB):
                        r0 = g * TG + sub * 128
                        dma("sp", x1[:], X1[r0:r0 + 128, :], r=[], w=["fx1"], key="fx1")
                        stt("dve", zz[:], x1[:], ALPHA, Y[:, sub, :], ALU.mult, ALU.add, r=["fx1", "fY%d" % sub], w=["fzz"])
                        layernorm(add, ts, tt, act, rstd_from, zz, xo, st, mv, rs, lng, ["fzz"], "fxo", "f")
                        dma("sp", x_dst[r0:r0 + 128, :], xo[:], r=["fxo"], w=[], key="fxo")
        S_.barrier()

    S_.emit(nc)
    es.close()
    return nc


def layernorm(add, ts, tt, act, rstd_from, z, xo, st, mv, rs, lng, zn, xon, pfx):
    stn, mvn, rsn = pfx + "st", pfx + "mv", pfx + "rs"
    add("dve", lambda e: e.bn_stats(out=st[:, 0:6], in_=z[:, 0:512]), r=zn, w=[stn])
    add("dve", lambda e: e.bn_stats(out=st[:, 6:12], in_=z[:, 512:1024]), r=zn + [stn], w=[stn])
    add("dve", lambda e: e.bn_aggr(out=mv[:], in_=st[:]), r=[stn], w=[mvn])
    rstd_from(rs[:], mv[:, 1:2], 1.0, r=[mvn], w=[rsn])
    ts("dve", xo[:], z[:], mv[:, 0:1], rs[:, 0:1], ALU.subtract, ALU.mult, r=zn + [mvn, rsn], w=[xon])
    tt("pool", xo[:], xo[:], lng[:, 0, :], ALU.mult, r=[xon, pfx + "lng"], w=[xon])
    tt("pool", xo[:], xo[:], lng[:, 1, :], ALU.add, r=[xon, pfx + "lng"], w=[xon])


def prep_inputs(inp, b, S, L):
    f = np.float32
    def c(a):
        return np.ascontiguousarray(a)
    NM = L // 2
    wuq = np.asarray(inp["w_uq"])[:L].reshape(L, 384, 8, 96)
    wuq_p = np.concatenate([wuq[..., 0:64].reshape(L, 384, 512), wuq[..., 64:80].reshape(L, 384, 128),
                            wuq[..., 80:96].reshape(L, 384, 128)], axis=-1)
    wukv = np.asarray(inp["w_ukv"])[:L].reshape(L, 256, 8, 128)
    wukv_p = np.concatenate([wukv[..., 0:64].reshape(L, 256, 512), wukv[..., 64:128].reshape(L, 256, 512)], axis=-1)
    lam = np.concatenate([np.asarray(inp[k])[:L] for k in ("lambda_q1", "lambda_k1", "lambda_q2", "lambda_k2")], axis=-1).reshape(L, 1, 256)
    d = {
        "x": c(np.asarray(inp["x"])[b, :S]),
        "p": c(np.asarray(inp["p"])[:L, b, :S]),
        "pos": c(np.asarray(inp["positions"])[b, :S].reshape(1, S).astype(np.int32)),
        "tab": c(np.asarray(inp["rel_bias_table"]).reshape(1, 256)),
        "w_in": c(np.asarray(inp["w_in"])[:L]),
        "bg": c(np.asarray(inp["b_gate"])[:L].reshape(L, 16, 128).transpose(0, 2, 1)),
        "lam": c(lam),
        "subg": c(np.asarray(inp["diff_subln_g"])[:L].reshape(L, 128, 1)),
        "gq": c(np.asarray(inp["mla_q_norm_g"])[:L].reshape(L, 3, 128).transpose(0, 2, 1)),
        "wuq": c(wuq_p),
        "gkv": c(np.asarray(inp["mla_kv_norm_g"])[:L].reshape(L, 2, 128).transpose(0, 2, 1)),
        "wukv": c(wukv_p),
        "wbd": c(np.asarray(inp["w_branch_diff"])[:L]),
        "wbm": c(np.asarray(inp["w_branch_mla"])[:L]),
        "wout": c(np.asarray(inp["w_out"])[:L]),
        "lnm": c(np.stack([np.asarray(inp["ln_mix_g"])[:L], np.asarray(inp["ln_mix_b"])[:L]], axis=1)),
        "dw1": c(np.asarray(inp["dense_w1"])[:(L + 1) // 2]),
        "dw3": c(np.asarray(inp["dense_w3"])[:(L + 1) // 2]),
        "dw2": c(np.asarray(inp["dense_w2"])[:(L + 1) // 2]),
        "rw": c(np.asarray(inp["router_w"])[:max(NM, 1)].transpose(0, 2, 1).reshape(max(NM, 1), 1, 8 * 1024)),
        "ew1": c(np.asarray(inp["expert_w1"])[:max(NM, 1)]),
        "ew3": c(np.asarray(inp["expert_w3"])[:max(NM, 1)]),
        "ew2": c(np.asarray(inp["expert_w2"])[:max(NM, 1)]),
        "wpg": c(np.asarray(inp["w_ple_gate"])[:L]),
        "wpp": c(np.asarray(inp["w_ple_proj"])[:L]),
        "lnf": c(np.stack([np.asarray(inp["ln_ffn_g"])[:L], np.asarray(inp["ln_ffn_b"])[:L]], axis=1)),
    }
    cst = np.zeros((128, 4), f)
    half = 16
    invf = (np.float32(10000.0) ** (-np.arange(half, dtype=f) / np.float32(half))).astype(f)
    cst[:, 0] = np.tile(invf, 8)
    d["cst"] = cst
    return {k: np.ascontiguousarray(v) for k, v in d.items()}


_NC_CACHE = {}


def kernel(**inputs):
    S, L, NCORES = 4096, 4, 8
    if "nc" not in _NC_CACHE:
        _NC_CACHE["nc"] = build(S, L, TG=1024)
    nc = _NC_CACHE["nc"]
    in_maps = [prep_inputs(inputs, b, S, L) for b in range(NCORES)]
    res = run_bass_kernel_spmd(nc, in_maps, core_ids=list(range(NCORES)))
    return np.stack([np.asarray(r["out"], dtype=np.float32) for r in res.results], axis=0)
```

```python
import os
import math
from contextlib import ExitStack
import numpy as np
import concourse.bass as bass
import concourse.mybir as mybir
from concourse.bass_utils import run_bass_kernel_spmd

F32 = mybir.dt.float32
BF16 = mybir.dt.bfloat16
I32 = mybir.dt.int32
AF = mybir.ActivationFunctionType
ALU = mybir.AluOpType
AX = mybir.AxisListType

ENGS = ("pe", "act", "dve", "pool", "sp")
SEM_MAX = 20000
NSLOT = 84


class Op:
    __slots__ = ("eng", "fn", "deps", "dma", "slot", "idx", "sig", "ordn", "barrier")

    def __init__(self, eng, fn, dma):
        self.eng, self.fn, self.dma = eng, fn, dma
        self.deps = []
        self.sig = False
        self.ordn = None
        self.slot = None
        self.barrier = False


class Sched:
    def __init__(self):
        self.ops = {e: [] for e in ENGS}
        self.state = {}
        self.slot_of = {}
        self.slot_cnt = [0] * NSLOT
        self.slot_last = [None] * NSLOT
        self.nslot_used = 0
        self.max_slots = 0

    def add(self, eng, fn, r=(), w=(), dma=None):
        op = Op(eng, fn, dma)
        w = list(w) + [b for b in r if b.startswith('ps')]
        r = [b for b in r if not b.startswith('ps')]
        deps = []
        for b in r:
            st = self.state.setdefault(b, [None, []])
            if st[0] is not None:
                deps.append(st[0])
        for b in w:
            st = self.state.setdefault(b, [None, []])
            if st[0] is not None:
                deps.append(st[0])
            deps.extend(st[1])
        seen = set()
        for d in deps:
            if id(d) in seen:
                continue
            seen.add(id(d))
            if d.dma is None and op.dma is None and d.eng == "pe" and eng == "pe":
                continue
            op.deps.append(d)
            if d.dma is None:
                d.sig = True
        if dma is not None:
            if dma not in self.slot_of:
                assert self.nslot_used < NSLOT, "out of dma semaphore slots"
                self.slot_of[dma] = self.nslot_used
                self.nslot_used += 1
                self.max_slots = max(self.max_slots, self.nslot_used)
            sl = self.slot_of[dma]
            self.slot_cnt[sl] += 1
            assert self.slot_cnt[sl] * 16 < 32000, "dma sem count too large: %s" % dma
            op.slot = sl
            op.ordn = self.slot_cnt[sl]
            self.slot_last[sl] = op
        op.idx = len(self.ops[eng])
        self.ops[eng].append(op)
        for b in r:
            self.state[b][1].append(op)
        for b in w:
            self.state[b] = [op, []]
        return op

    def barrier(self):
        used = self.nslot_used
        c = Op("sp", None, None)
        c.barrier = True
        for e in ENGS:
            if e == "sp":
                continue
            for op in reversed(self.ops[e]):
                if op.dma is None:
                    if not op.barrier:
                        c.deps.append(op)
                        op.sig = True
                    break
        for sl in range(used):
            if self.slot_last[sl] is not None:
                c.deps.append(self.slot_last[sl])
        c.slot = used
        c.sig = True
        c.idx = len(self.ops["sp"])
        self.ops["sp"].append(c)
        for e in ENGS:
            if e == "sp":
                continue
            d = Op(e, None, None)
            d.barrier = True
            d.deps.append(c)
            d.idx = len(self.ops[e])
            self.ops[e].append(d)
        self.state = {}
        self.slot_of = {}
        self.slot_cnt = [0] * NSLOT
        self.slot_last = [None] * NSLOT
        self.nslot_used = 0

    def emit(self, nc):
        from contextlib import ExitStack
        self.barrier()
        nsig = {}
        for e in ENGS:
            n = 0
            for op in self.ops[e]:
                if op.dma is None and op.sig:
                    op.ordn = n
                    n += 1
            nsig[e] = n
        with ExitStack() as es:
            esem = {}
            for e in ENGS:
                k = max(1, (nsig[e] + SEM_MAX - 1) // SEM_MAX)
                esem[e] = [es.enter_context(nc.semaphore("s_%s_%d" % (e, i))) for i in range(k)]
            dsem = [es.enter_context(nc.semaphore("d_%d" % i)) for i in range(self.max_slots)]
            block = es.enter_context(nc.Block())

            def target(d):
                if d.dma is not None:
                    return dsem[d.slot], 16 * d.ordn
                return esem[d.eng][d.ordn // SEM_MAX], d.ordn % SEM_MAX + 1

            def run(e, eng):
                waited = {}
                for op in self.ops[e]:
                    for d in op.deps:
                        s, v = target(d)
                        if waited.get(s.name, 0) >= v:
                            continue
                        waited[s.name] = v
                        eng.wait_ge(s, v)
                    if op.barrier:
                        if e == "sp":
                            for sl in range(op.slot):
                                eng.sem_clear(dsem[sl])
                            ins = eng.nop()
                        else:
                            ins = eng.nop()
                        for k in list(waited.keys()):
                            if k.startswith("d_"):
                                del waited[k]
                    else:
                        ins = op.fn(eng)
                    if op.dma is not None:
                        ins.then_inc(dsem[op.slot], 16)
                    elif op.sig:
                        ins.then_inc(esem[e][op.ordn // SEM_MAX], 1)

            @block.tensor
            def _(eng):
                run("pe", eng)

            @block.scalar
            def _(eng):
                run("act", eng)

            @block.vector
            def _(eng):
                run("dve", eng)

            @block.gpsimd
            def _(eng):
                run("pool", eng)

            @block.sync
            def _(eng):
                run("sp", eng)


D = 1024
NQ = 384
NKV = 256
COLQ, COLK, COLV, COLCQ, COLCKV, COLKR, COLG = 0, 1024, 2048, 3072, 3456, 3712, 3744
INC = 5792
DENSE_FF = 2816
EXP_FF = 3584
NEG = -30000.0
DEPTH_TOTAL = 4
ALPHA = (2.0 * DEPTH_TOTAL) ** 0.25
EPS = 1e-5


def t5_thresholds():
    n = np.arange(0, 4096, dtype=np.int32)
    nf = np.maximum(n, 1).astype(np.float32)
    large = 16 + ((np.log(nf / np.float32(16)) / np.float32(math.log(128 / 16))) * np.float32(16)).astype(np.int32)
    large = np.minimum(large, 31)
    bucket = np.where(n < 16, n, large)
    th = []
    for j in range(1, 32):
        th.append(int(np.argmax(bucket >= j)))
    return th


def build(S, L, TG=1024, dbg=(), upto=None):
    nc = bass.Bass("TRN2", target_bir_lowering=False)
    NT = S // 512
    NB = S // 128
    NG = S // TG
    ND = (L + 1) // 2
    NM = L // 2
    es = ExitStack()
    S_ = Sched()
    add = S_.add

    def din(name, shape, dt=F32):
        return nc.dram_tensor(name, list(shape), dt, kind="ExternalInput").ap()

    def dscr(name, shape, dt):
        kind = "ExternalOutput" if name in dbg else "Internal"
        return nc.dram_tensor(name, list(shape), dt, kind=kind).ap()

    x_in = din("x", [S, D])
    p_in = din("p", [L, S, 256])
    pos_in = din("pos", [1, S], I32)
    tab_in = din("tab", [1, 256])
    w_in = din("w_in", [L, D, INC])
    bg_in = din("bg", [L, 128, 16])
    lam_in = din("lam", [L, 1, 256])
    subg_in = din("subg", [L, 128, 1])
    gq_in = din("gq", [L, 128, 3])
    wuq_in = din("wuq", [L, NQ, 768])
    gkv_in = din("gkv", [L, 128, 2])
    wukv_in = din("wukv", [L, NKV, 1024])
    wbd_in = din("wbd", [L, 1024, D])
    wbm_in = din("wbm", [L, 512, D])
    wout_in = din("wout", [L, D, D])
    lnm_in = din("lnm", [L, 2, D])
    dw1_in = din("dw1", [ND, D, DENSE_FF])
    dw3_in = din("dw3", [ND, D, DENSE_FF])
    dw2_in = din("dw2", [ND, DENSE_FF, D])
    rw_in = din("rw", [max(NM, 1), 1, 8 * D])
    ew1_in = din("ew1", [max(NM, 1), 8, D, EXP_FF])
    ew3_in = din("ew3", [max(NM, 1), 8, D, EXP_FF])
    ew2_in = din("ew2", [max(NM, 1), 8, EXP_FF, D])
    wpg_in = din("wpg", [L, D, D])
    wpp_in = din("wpp", [L, 256, D])
    lnf_in = din("lnf", [L, 2, D])
    cst_in = din("cst", [128, 4])
    out = nc.dram_tensor("out", [S, D], F32, kind="ExternalOutput").ap()

    XT = dscr("XT", [8, 128, S], BF16)
    QD = dscr("QD", [8, 128, S], BF16)
    KD = dscr("KD", [8, 128, S], BF16)
    VD = dscr("VD", [S, 1024], BF16)
    GT = dscr("GT", [16, 128, S], BF16)
    QMN = dscr("QMN", [4, 128, S], BF16)
    KMN = dscr("KMN", [4, 128, S], BF16)
    QR = dscr("QR", [2, 128, S], BF16)
    KR = dscr("KR", [2, 16, S], BF16)
    VM = dscr("VM", [S, 512], BF16)
    OAT = dscr("OAT", [8, 128, S], BF16)
    OBT = dscr("OBT", [8, 64, S], BF16)
    X1 = dscr("X1", [S, D], F32)
    X1T = dscr("X1T", [8, 128, S], BF16)
    EE = dscr("EE", [S, D], F32)
    XR = dscr("XR", [S, D], F32)
    CS = dscr("CS", [2, 128, S], F32)
    BST = dscr("BST", [9, 128, 1024], F32)

    sbc = [0]

    def sb(stack, name, shape, dt):
        sbc[0] += 1
        return stack.enter_context(nc.sbuf_tensor("sb_%s_%d" % (name, sbc[0]), list(shape), dt))

    ident = sb(es, "ident", [128, 128], BF16)
    ones_bf = sb(es, "ones_bf", [128, 128], BF16)
    ones_f = sb(es, "ones_f", [128, 128], F32)
    cst = sb(es, "cst", [128, 4], F32)
    ccol = sb(es, "ccol", [128, 8], F32)
    tabb = sb(es, "tabb", [128, 256], F32)
    gates = sb(es, "gates", [128, NB, 8], F32)
    PSB = []
    PSTT = []
    psn = ["ps%d" % i for i in range(8)]
    psc = [0]

    def alloc_psum(ph, nf, nbf):
        psc[0] += 1
        PSB[:] = [ph.enter_context(nc.psum_tensor("psb%d_%d" % (i, psc[0]), [128, 512], F32)) for i in range(nf)]
        PSTT[:] = [ph.enter_context(nc.psum_tensor("pst%d_%d" % (i, psc[0]), [128, 1024], BF16)) for i in range(nbf)]
    ring = [0]

    def nps():
        i = ring[0] % len(PSB)
        ring[0] += 1
        return PSB[i], psn[i]

    def mm(out_ap, pairs, r, w):
        pairs = list(pairs)

        def fn(e):
            ins = None
            n = len(pairs)
            for i, (l, rh) in enumerate(pairs):
                ins = e.matmul(out_ap, lhsT=l, rhs=rh, start=(i == 0), stop=(i == n - 1))
            return ins
        add("pe", fn, r=r, w=w)

    def mm1(out_ap, l, rh, start, stop, r, w):
        add("pe", lambda e: e.matmul(out_ap, lhsT=l, rhs=rh, start=start, stop=stop), r=r, w=w)

    def dma(eng, out_ap, in_ap, r, w, key):
        add(eng, lambda e: e.dma_start(out=out_ap, in_=in_ap), r=r, w=w, dma=key)

    def act(out_ap, in_ap, func, r, w, bias=None, scale=None):
        kw = {}
        if bias is not None:
            kw["bias"] = bias
        if scale is not None:
            kw["scale"] = scale
        add("act", lambda e: e.activation(out=out_ap, in_=in_ap, func=func, **kw), r=r, w=w)

    def tt(eng, out_ap, a, b, op, r, w):
        add(eng, lambda e: e.tensor_tensor(out=out_ap, in0=a, in1=b, op=op), r=r, w=w)

    def ts(eng, out_ap, a, s1, s2, op0, op1, r, w):
        if op1 is None:
            add(eng, lambda e: e.tensor_scalar(out=out_ap, in0=a, scalar1=s1, scalar2=None, op0=op0), r=r, w=w)
        else:
            add(eng, lambda e: e.tensor_scalar(out=out_ap, in0=a, scalar1=s1, scalar2=s2, op0=op0, op1=op1), r=r, w=w)

    def stt(eng, out_ap, a, sc, b, op0, op1, r, w):
        add(eng, lambda e: e.scalar_tensor_tensor(out=out_ap, in0=a, scalar=sc, in1=b, op0=op0, op1=op1), r=r, w=w)

    def cp(eng, out_ap, in_ap, r, w):
        if eng == "act":
            add("act", lambda e: e.copy(out=out_ap, in_=in_ap), r=r, w=w)
        else:
            add(eng, lambda e: e.tensor_copy(out=out_ap, in_=in_ap), r=r, w=w)

    STGW = 2048
    stg_state = {"tiles": None, "n": 0}
    cast_rot = ("pool", "act", "pool", "dve")

    def wload(dst_ap, src_ap, width, wname, parts=128):
        tiles = stg_state["tiles"]
        k = stg_state["n"]
        stg_state["n"] += 1
        i = k % len(tiles)
        st_ = tiles[i]
        dma("sp", st_[0:parts, 0:width], src_ap, r=[], w=["stg%d" % i], key="stg%d" % i)
        cp(cast_rot[k % 4], dst_ap, st_[0:parts, 0:width], r=["stg%d" % i], w=[wname])

    def rstd_from(out_ap, in_ap, scale, r, w):
        act(out_ap, in_ap, AF.Ln, r=r + ["ccol"], w=w, bias=ccol[:, 0:1], scale=scale)
        act(out_ap, out_ap, AF.Exp, r=w, w=w, scale=-0.5)

    add("pool", lambda e: e.memset(ident[:], 0.0), w=["ident"])
    add("pool", lambda e: e.affine_select(out=ident[:], in_=ident[:], compare_op=ALU.not_equal, fill=1.0,
                                          base=0, pattern=[[-1, 128]], channel_multiplier=1),
        r=["ident"], w=["ident"])
    add("dve", lambda e: e.memset(ones_bf[:], 1.0), w=["ones_bf"])
    add("dve", lambda e: e.memset(ones_f[:], 1.0), w=["ones_f"])
    add("dve", lambda e: e.memset(ccol[:, 0:1], EPS), w=["ccol"])
    add("dve", lambda e: e.memset(ccol[:, 1:2], -math.pi), r=["ccol"], w=["ccol"])
    add("dve", lambda e: e.memset(ccol[:, 2:3], 1.0), r=["ccol"], w=["ccol"])
    add("dve", lambda e: e.memset(ccol[:, 3:4], 0.0), r=["ccol"], w=["ccol"])
    dma("sp", cst[:], cst_in, r=[], w=["cst"], key="cst")
    dma("sp", tabb[:], tab_in.partition_broadcast(128), r=[], w=["tabb"], key="tabb")

    with ExitStack() as ph:
        posi = sb(ph, "posi", [128, S], I32)
        posf = sb(ph, "posf", [128, S], F32)
        ang = sb(ph, "ang", [128, S], F32)
        tmpc = sb(ph, "tmpc", [128, S], F32)
        dma("sp", posi[:], pos_in.partition_broadcast(128), r=[], w=["posi"], key="posi")
        cp("dve", posf[:], posi[:], r=["posi"], w=["posf"])
        ts("dve", ang[:], posf[:], cst[:, 0:1], None, ALU.mult, None, r=["posf", "cst"], w=["ang"])
        C1 = 6.28125
        C2 = 2.0 * math.pi - C1
        PI_IN = 3.1415925
        indc = sb(ph, "indc", [128, S], F32)
        ts("dve", tmpc[:], ang[:], 1.0 / (2.0 * math.pi), None, ALU.mult, None, r=["ang"], w=["tmpc"])
        cp("dve", posi[:], tmpc[:], r=["tmpc", "posf"], w=["posi"])
        cp("dve", posf[:], posi[:], r=["posi", "ang"], w=["posf"])
        stt("dve", tmpc[:], posf[:], -C1, ang[:], ALU.mult, ALU.add, r=["posf", "ang"], w=["tmpc"])
        stt("dve", tmpc[:], posf[:], -C2, tmpc[:], ALU.mult, ALU.add, r=["posf", "tmpc"], w=["tmpc"])

        def wrap(tn, t):
            ts("dve", indc[:], t[:], math.pi, None, ALU.is_gt, None, r=[tn], w=["indc"])
            stt("dve", t[:], indc[:], -2.0 * math.pi, t[:], ALU.mult, ALU.add, r=["indc", tn], w=[tn])
            ts("dve", indc[:], t[:], -math.pi, None, ALU.is_lt, None, r=[tn], w=["indc"])
            stt("dve", t[:], indc[:], 2.0 * math.pi, t[:], ALU.mult, ALU.add, r=["indc", tn], w=[tn])
            ts("dve", t[:], t[:], PI_IN, -PI_IN, ALU.min, ALU.max, r=[tn], w=[tn])
        wrap("tmpc", tmpc)
        ts("dve", ang[:], tmpc[:], 0.5 * math.pi, None, ALU.add, None, r=["tmpc"], w=["ang"])
        wrap("ang", ang)
        act(ang[:], ang[:], AF.Sin, r=["ang"], w=["ang"])
        dma("sp", CS[0], ang[:], r=["ang"], w=[], key="ang")
        act(tmpc[:], tmpc[:], AF.Sin, r=["tmpc"], w=["tmpc"])
        dma("sp", CS[1], tmpc[:], r=["tmpc"], w=[], key="tmpc")
    S_.barrier()
    with ExitStack() as ph:
        nmi = sb(ph, "nmi", [128, 1024], I32)
        nmat = sb(ph, "nmat", [128, 1024], F32)
        ind = sb(ph, "ind", [128, 1024], F32)
        mstrip = sb(ph, "mstrip", [128, 1024], F32)
        dtab = sb(ph, "dtab", [128, 256], F32)
        bst = [sb(ph, "bst%d" % h, [128, 1024], F32) for h in range(8)]
        add("pool", lambda e: e.iota(nmi[:], pattern=[[1, 1024]], base=-384, channel_multiplier=-1), w=["nmi"])
        cp("dve", nmat[:], nmi[:], r=["nmi"], w=["nmat"])
        tt("dve", dtab[:, 8:256], tabb[:, 8:256], tabb[:, 0:248], ALU.subtract, r=["tabb"], w=["dtab"])
        ts("dve", ind[:], nmat[:], 0.0, None, ALU.is_ge, None, r=["nmat"], w=["ind"])
        ts("dve", mstrip[:], ind[:], 1.0, -NEG, ALU.subtract, ALU.mult, r=["ind"], w=["mstrip"])
        for h in range(8):
            ts("pool", bst[h][:], mstrip[:], tabb[:, h:h + 1], None, ALU.add, None, r=["mstrip", "tabb"], w=["bst%d" % h])
        th = t5_thresholds()
        for j in range(1, 32):
            ts("dve", ind[:], nmat[:], float(th[j - 1]), None, ALU.is_ge, None, r=["nmat"], w=["ind"])
            for h in range(8):
                eng = "dve"
                stt(eng, bst[h][:], ind[:], dtab[:, j * 8 + h:j * 8 + h + 1], bst[h][:], ALU.mult, ALU.add,
                    r=["ind", "dtab", "bst%d" % h], w=["bst%d" % h])
        for h in range(8):
            dma("sp", BST[h], bst[h][:], r=["bst%d" % h], w=[], key="bst%d" % h)
        dma("sp", BST[8], mstrip[:], r=["mstrip"], w=[], key="mstrip")
    S_.barrier()
    if upto == 'C':
        S_.emit(nc)
        es.close()
        return nc

    for li in range(L):
        lam_init = 0.8 - 0.6 * math.exp(-0.3 * li)
        x_src = x_in if li == 0 else XR
        x_dst = out if li == L - 1 else XR

        with ExitStack() as ph:
            alloc_psum(ph, 6, 2)
            w1 = sb(ph, "p1w", [128, 8, 3072], BF16)
            xs = [sb(ph, "p1xs%d" % i, [128, 4, 1024], F32) for i in range(2)]
            xb = sb(ph, "p1xb", [128, 4, 1024], BF16)
            xT = [sb(ph, "p1xT%d" % i, [128, 8, 512], BF16) for i in range(2)]
            qd = [sb(ph, "p1qd%d" % i, [128, 8, 512], BF16) for i in range(2)]
            kd = [sb(ph, "p1kd%d" % i, [128, 8, 512], BF16) for i in range(2)]
            vd = [sb(ph, "p1vd%d" % i, [128, 4, 1024], BF16) for i in range(2)]
            stg_state["tiles"] = [sb(ph, "p1stg%d" % i, [128, STGW], F32) for i in range(3)]
            for kb in range(8):
                for c3 in range(0, 3072, STGW):
                    wd = min(STGW, 3072 - c3)
                    wload(w1[:, kb, c3:c3 + wd], w_in[li, kb * 128:(kb + 1) * 128, c3:c3 + wd], wd, "p1w%d_%d" % (kb, c3))
            wn = ["p1w%d_%d" % (kb, c3) for kb in range(8) for c3 in range(0, 3072, STGW)]

            def ldx(t):
                i = t % 2
                dma("sp", xs[i][:], x_src[t * 512:(t + 1) * 512, :].rearrange("(j p) d -> p j d", p=128),
                    r=[], w=["p1xs%d" % i], key="p1xs%d" % i)
            ldx(0)
            for t in range(NT):
                i = t % 2
                if t + 1 < NT:
                    ldx(t + 1)
                import os
                cut = int(os.environ.get("P1CUT", "99"))
                if cut < 1:
                    continue
                for j in range(4):
                    cp("dve" if j % 2 == 0 else "pool", xb[:, j, :], xs[i][:, j, :], r=["p1xs%d" % i], w=["p1xb%d" % j])
                if cut < 2:
                    continue
                for kb in range(8):
                    def tr(e, kb=kb, PSTT=tuple(PSTT)):
                        ins = None
                        for j in range(4):
                            ins = e.transpose(out=PSTT[kb % 2][:, j * 128:(j + 1) * 128],
                                              in_=xb[:, j, kb * 128:(kb + 1) * 128], identity=ident[:])
                        return ins
                    sub_ = os.environ.get("P1SUB", "")
                    if sub_ == "one" and kb > 0:
                        continue
                    add("pe", tr, r=["p1xb%d" % j for j in range(4)] + ["ident"], w=["pst%d" % (kb % 2)])
                    if sub_ == "tr":
                        continue
                    cp("act" if kb % 2 else "dve", xT[i][:, kb, :], PSTT[kb % 2][:, 0:512],
                       r=["pst%d" % (kb % 2)], w=["p1xT%d_%d" % (i, kb)])
                xTn = ["p1xT%d_%d" % (i, kb) for kb in range(8)]
                if cut < 3:
                    continue
                dma("sp", XT[:, :, t * 512:(t + 1) * 512].rearrange("k p s -> p k s"), xT[i][:], r=xTn, w=[], key="p1xT%d" % i)
                if cut < 4:
                    continue
                for (col, dst, dstn, DR, sc) in ((COLQ, qd, "p1qd", QD, 0.125), (COLK, kd, "p1kd", KD, 1.0)):
                    for h in range(8):
                        pt, pn = nps()
                        mm(pt[:], [(w1[:, kb, col + h * 128:col + (h + 1) * 128], xT[i][:, kb, :]) for kb in range(8)],
                           r=wn + xTn, w=[pn])
                        if h % 2 == 0:
                            act(dst[i][:, h, :], pt[:], AF.Copy, r=[pn], w=["%s%d_%d" % (dstn, i, h)], scale=sc)
                        else:
                            ts("dve", dst[i][:, h, :], pt[:], sc, None, ALU.mult, None, r=[pn], w=["%s%d_%d" % (dstn, i, h)])
                    dma("sp", DR[:, :, t * 512:(t + 1) * 512].rearrange("h p s -> p h s"), dst[i][:],
                        r=["%s%d_%d" % (dstn, i, h) for h in range(8)], w=[], key="%s%d" % (dstn, i))
                if cut < 5:
                    continue
                for j in range(4):
                    for hf in range(2):
                        pt, pn = nps()
                        mm(pt[:], [(xT[i][:, kb, j * 128:(j + 1) * 128], w1[:, kb, COLV + hf * 512:COLV + (hf + 1) * 512]) for kb in range(8)],
                           r=wn + xTn, w=[pn])
                        cp("act" if hf else "dve", vd[i][:, j, hf * 512:(hf + 1) * 512], pt[:], r=[pn], w=["p1vd%d_%d_%d" % (i, j, hf)])
                dma("sp", VD[t * 512:(t + 1) * 512, :].rearrange("(j p) e -> p j e", p=128), vd[i][:],
                    r=["p1vd%d_%d_%d" % (i, j, hf) for j in range(4) for hf in range(2)], w=[], key="p1vd%d" % i)
        S_.barrier()
        if upto == 'P1':
            S_.emit(nc)
            es.close()
            return nc

        with ExitStack() as ph:
            alloc_psum(ph, 8, 0)
            w2 = sb(ph, "p2w", [128, 8, INC - 3072], BF16)
            W2O = 3072
            wq_st = sb(ph, "p2wqs", [128, 3, 768], F32)
            wkv_st = sb(ph, "p2wkvs", [128, 2, 1024], F32)
            wq = sb(ph, "p2wq", [128, 3, 768], BF16)
            wkv = sb(ph, "p2wkv", [128, 2, 1024], BF16)
            gq = sb(ph, "p2gq", [128, 3], F32)
            gkv = sb(ph, "p2gkv", [128, 2], F32)
            bg = sb(ph, "p2bg", [128, 16], F32)
            xT = [sb(ph, "p2xT%d" % i, [128, 8, 512], BF16) for i in range(2)]
            cs = [sb(ph, "p2cs%d" % i, [128, 2, 512], F32) for i in range(2)]
            gsb = sb(ph, "p2g", [128, 16, 512], BF16)
            cqb = sb(ph, "p2cqb", [128, 3, 512], BF16)
            sqq = sb(ph, "p2sqq", [128, 3, 512], BF16)
            ckb = sb(ph, "p2ckb", [128, 2, 512], BF16)
            sqk = sb(ph, "p2sqk", [128, 2, 512], BF16)
            rq = sb(ph, "p2rq", [128, 512], F32)
            rk = sb(ph, "p2rk", [128, 512], F32)
            rkc = sb(ph, "p2rkc", [128, 4], F32)
            qn = sb(ph, "p2qn", [128, 4, 512], BF16)
            kn = sb(ph, "p2kn", [128, 4, 512], BF16)
            vm = sb(ph, "p2vm", [128, 4, 512], BF16)
            x1s = sb(ph, "p2x1s", [128, 512], F32)
            x2s = sb(ph, "p2x2s", [128, 512], F32)
            ta = sb(ph, "p2ta", [128, 512], F32)
            tb = sb(ph, "p2tb", [128, 512], F32)
            qr = sb(ph, "p2qr", [128, 2, 512], BF16)
            kr = sb(ph, "p2kr", [16, 2, 512], BF16)
            stg_state["tiles"] = [sb(ph, "p2stg%d" % i, [128, STGW], F32) for i in range(2)]
            W2W = INC - 3072
            for kb in range(8):
                for c3 in range(0, W2W, STGW):
                    wd = min(STGW, W2W - c3)
                    wload(w2[:, kb, c3:c3 + wd], w_in[li, kb * 128:(kb + 1) * 128, 3072 + c3:3072 + c3 + wd], wd, "p2w%d_%d" % (kb, c3))
            wn = ["p2w%d_%d" % (kb, c3) for kb in range(8) for c3 in range(0, W2W, STGW)]
            dma("sp", wq_st[:], wuq_in[li].rearrange("(k p) c -> p k c", p=128), r=[], w=["p2wqs"], key="p2wqs")
            dma("sp", wkv_st[:], wukv_in[li].rearrange("(k p) c -> p k c", p=128), r=[], w=["p2wkvs"], key="p2wkvs")
            dma("sp", gq[:], gq_in[li], r=[], w=["p2gq"], key="p2gq")
            dma("sp", gkv[:], gkv_in[li], r=[], w=["p2gkv"], key="p2gkv")
            dma("sp", bg[:], bg_in[li], r=[], w=["p2bg"], key="p2bg")
            for k in range(3):
                ts("dve", wq[:, k, :], wq_st[:, k, :], gq[:, k:k + 1], None, ALU.mult, None, r=["p2wqs", "p2gq"], w=["p2wq"])
            for k in range(2):
                ts("dve", wkv[:, k, :], wkv_st[:, k, :], gkv[:, k:k + 1], None, ALU.mult, None, r=["p2wkvs", "p2gkv"], w=["p2wkv"])

            def ldt(t):
                i = t % 2
                dma("sp", xT[i][:], XT[:, :, t * 512:(t + 1) * 512].rearrange("k p s -> p k s"), r=[], w=["p2xT%d" % i], key="p2xT%d" % i)
                dma("sp", cs[i][:], CS[:, :, t * 512:(t + 1) * 512].rearrange("c p s -> p c s"), r=[], w=["p2cs%d" % i], key="p2cs%d" % i)
            ldt(0)
            MSC = 96.0 ** -0.5
            for t in range(NT):
                i = t % 2
                if t + 1 < NT:
                    ldt(t + 1)
                xn = ["p2xT%d" % i]
                tsl = slice(t * 512, (t + 1) * 512)
                for gb in range(16):
                    pt, pn = nps()
                    c0 = COLG - W2O + gb * 128
                    mm(pt[:], [(w2[:, kb, c0:c0 + 128], xT[i][:, kb, :]) for kb in range(8)], r=wn + xn, w=[pn])
                    act(gsb[:, gb, :], pt[:], AF.Sigmoid, r=[pn, "p2bg"], w=["p2g%d" % gb], bias=bg[:, gb:gb + 1], scale=1.0)
                dma("sp", GT[:, :, tsl].rearrange("g p s -> p g s"), gsb[:], r=["p2g%d" % gb for gb in range(16)], w=[], key="p2g")
                for b in range(3):
                    pt, pn = nps()
                    c0 = COLCQ - W2O + b * 128
                    mm(pt[:], [(w2[:, kb, c0:c0 + 128], xT[i][:, kb, :]) for kb in range(8)], r=wn + xn, w=[pn])
                    cp("dve", cqb[:, b, :], pt[:], r=[pn], w=["p2cqb%d" % b])
                    act(sqq[:, b, :], pt[:], AF.Square, r=[pn], w=["p2sqq%d" % b])
                pt, pn = nps()
                mm(pt[:], [(ones_bf[:], sqq[:, b, :]) for b in range(3)], r=["ones_bf"] + ["p2sqq%d" % b for b in range(3)], w=[pn])
                rstd_from(rq[:], pt[:], 1.0 / NQ, r=[pn], w=["p2rq"])
                ts("dve", rq[:], rq[:], MSC, None, ALU.mult, None, r=["p2rq"], w=["p2rq"])
                cqn = ["p2cqb%d" % b for b in range(3)]
                for pr in range(4):
                    pt, pn = nps()
                    mm(pt[:], [(wq[:, k, pr * 128:(pr + 1) * 128], cqb[:, k, :]) for k in range(3)], r=["p2wq"] + cqn, w=[pn])
                    tt("dve", qn[:, pr, :], pt[:], rq[:], ALU.mult, r=[pn, "p2rq"], w=["p2qn%d" % pr])
                dma("sp", QMN[:, :, tsl].rearrange("j p s -> p j s"), qn[:], r=["p2qn%d" % pr for pr in range(4)], w=[], key="p2qn")
                pt1, pn1 = nps()
                mm(pt1[:], [(wq[:, k, 512:640], cqb[:, k, :]) for k in range(3)], r=["p2wq"] + cqn, w=[pn1])
                pt2, pn2 = nps()
                mm(pt2[:], [(wq[:, k, 640:768], cqb[:, k, :]) for k in range(3)], r=["p2wq"] + cqn, w=[pn2])
                tt("dve", x1s[:], pt1[:], rq[:], ALU.mult, r=[pn1, "p2rq"], w=["p2x1s"])
                tt("dve", x2s[:], pt2[:], rq[:], ALU.mult, r=[pn2, "p2rq"], w=["p2x2s"])
                csn = "p2cs%d" % i
                tt("dve", ta[:], x1s[:], cs[i][:, 0, :], ALU.mult, r=["p2x1s", csn], w=["p2ta"])
                tt("pool", tb[:], x2s[:], cs[i][:, 1, :], ALU.mult, r=["p2x2s", csn], w=["p2tb"])
                tt("dve", qr[:, 0, :], ta[:], tb[:], ALU.subtract, r=["p2ta", "p2tb"], w=["p2qr0"])
                tt("dve", ta[:], x1s[:], cs[i][:, 1, :], ALU.mult, r=["p2x1s", csn], w=["p2ta"])
                tt("pool", tb[:], x2s[:], cs[i][:, 0, :], ALU.mult, r=["p2x2s", csn], w=["p2tb"])
                tt("dve", qr[:, 1, :], ta[:], tb[:], ALU.add, r=["p2ta", "p2tb"], w=["p2qr1"])
                dma("sp", QR[:, :, tsl].rearrange("c p s -> p c s"), qr[:], r=["p2qr0", "p2qr1"], w=[], key="p2qr")
                for b in range(2):
                    pt, pn = nps()
                    c0 = COLCKV - W2O + b * 128
                    mm(pt[:], [(w2[:, kb, c0:c0 + 128], xT[i][:, kb, :]) for kb in range(8)], r=wn + xn, w=[pn])
                    cp("dve", ckb[:, b, :], pt[:], r=[pn], w=["p2ckb%d" % b])
                    act(sqk[:, b, :], pt[:], AF.Square, r=[pn], w=["p2sqk%d" % b])
                sqn = ["p2sqk%d" % b for b in range(2)]
                pt, pn = nps()
                mm(pt[:], [(ones_bf[:], sqk[:, b, :]) for b in range(2)], r=["ones_bf"] + sqn, w=[pn])
                rstd_from(rk[:], pt[:], 1.0 / NKV, r=[pn], w=["p2rk"])
                ptc, pnc = nps()
                for j in range(4):
                    mm(ptc[:, j:j + 1], [(sqk[:, b, j * 128:(j + 1) * 128], ones_bf[:, 0:1]) for b in range(2)], r=["ones_bf"] + sqn, w=[pnc])
                rstd_from(rkc[:], ptc[:, 0:4], 1.0 / NKV, r=[pnc], w=["p2rkc"])
                ckn = ["p2ckb%d" % b for b in range(2)]
                for pr in range(4):
                    pt, pn = nps()
                    mm(pt[:], [(wkv[:, k, pr * 128:(pr + 1) * 128], ckb[:, k, :]) for k in range(2)], r=["p2wkv"] + ckn, w=[pn])
                    tt("dve", kn[:, pr, :], pt[:], rk[:], ALU.mult, r=[pn, "p2rk"], w=["p2kn%d" % pr])
                dma("sp", KMN[:, :, tsl].rearrange("j p s -> p j s"), kn[:], r=["p2kn%d" % pr for pr in range(4)], w=[], key="p2kn")
                for j in range(4):
                    pt, pn = nps()
                    mm(pt[:], [(ckb[:, k, j * 128:(j + 1) * 128], wkv[:, k, 512:1024]) for k in range(2)], r=["p2wkv"] + ckn, w=[pn])
                    act(vm[:, j, :], pt[:], AF.Copy, r=[pn, "p2rkc"], w=["p2vm%d" % j], scale=rkc[:, j:j + 1])
                dma("sp", VM[tsl, :].rearrange("(j p) e -> p j e", p=128), vm[:], r=["p2vm%d" % j for j in range(4)], w=[], key="p2vm")
                pt1, pn1 = nps()
                c0 = COLKR - W2O
                mm(pt1[0:16, :], [(w2[:, kb, c0:c0 + 16], xT[i][:, kb, :]) for kb in range(8)], r=wn + xn, w=[pn1])
                pt2, pn2 = nps()
                mm(pt2[0:16, :], [(w2[:, kb, c0 + 16:c0 + 32], xT[i][:, kb, :]) for kb in range(8)], r=wn + xn, w=[pn2])
                tt("dve", ta[0:16, :], pt1[0:16, :], cs[i][0:16, 0, :], ALU.mult, r=[pn1, csn], w=["p2ta"])
                tt("dve", tb[0:16, :], pt2[0:16, :], cs[i][0:16, 1, :], ALU.mult, r=[pn2, csn], w=["p2tb"])
                tt("dve", kr[:, 0, :], ta[0:16, :], tb[0:16, :], ALU.subtract, r=["p2ta", "p2tb"], w=["p2kr0"])
                tt("dve", ta[0:16, :], pt1[0:16, :], cs[i][0:16, 1, :], ALU.mult, r=[pn1, csn], w=["p2ta"])
                tt("dve", tb[0:16, :], pt2[0:16, :], cs[i][0:16, 0, :], ALU.mult, r=[pn2, csn], w=["p2tb"])
                tt("dve", kr[:, 1, :], ta[0:16, :], tb[0:16, :], ALU.add, r=["p2ta", "p2tb"], w=["p2kr1"])
                dma("sp", KR[:, :, tsl].rearrange("c p s -> p c s"), kr[:], r=["p2kr0", "p2kr1"], w=[], key="p2kr")
        S_.barrier()
        if upto == 'P2':
            S_.emit(nc)
            es.close()
            return nc

        with ExitStack() as ph:
            alloc_psum(ph, 8, 0)
            bstr = sb(ph, "abst", [128, 9, 1024], F32)
            lamt = sb(ph, "alam", [128, 256], F32)
            lamp = sb(ph, "alamp", [128, 128], F32)
            lamc = sb(ph, "alamc", [128, 4], F32)
            subg = sb(ph, "asubg", [128, 1], F32)
            kdt = [sb(ph, "akd%d" % i, [128, S], BF16) for i in range(2)]
            vdt = [sb(ph, "avd%d" % i, [128, NB, 128], BF16) for i in range(2)]
            vmt = [sb(ph, "avm%d" % i, [128, NB, 65], BF16) for i in range(2)]
            qt = [sb(ph, "aq%d" % i, [128, 512], BF16) for i in range(2)]
            NPT = 6
            pT = [sb(ph, "apT%d" % i, [128, 512], BF16) for i in range(NPT)]
            sbias = [sb(ph, "asb%d" % i, [128, 512], F32) for i in range(4)]
            accS = [sb(ph, "aacc%d" % i, [128, 512], F32) for i in range(2)]
            accB = [sb(ph, "aaccb%d" % i, [128, 512], BF16) for i in range(2)]
            sbc2 = [0]
            rr = sb(ph, "arr", [128, 512], F32)
            rr2 = sb(ph, "arr2", [1, 512], F32)
            Rb = [sb(ph, "aRb%d" % i, [128, 512], F32) for i in range(2)]
            t0 = sb(ph, "at0", [128, 512], F32)
            t1 = sb(ph, "at1", [128, 512], F32)
            osq = sb(ph, "aosq", [128, 512], BF16)
            rms = sb(ph, "arms", [128, 512], F32)
            oo = [sb(ph, "aoo%d" % i, [128, 512], BF16) for i in range(2)]
            dma("sp", bstr[:], BST.rearrange("h p n -> p h n"), r=[], w=["abst"], key="abst")
            dma("sp", lamt[:], lam_in[li].partition_broadcast(128), r=[], w=["alam"], key="alam")
            dma("sp", subg[:], subg_in[li], r=[], w=["asubg"], key="asubg")
            tt("dve", lamp[:, 0:64], lamt[:, 0:64], lamt[:, 64:128], ALU.mult, r=["alam"], w=["alamp"])
            tt("dve", lamp[:, 64:128], lamt[:, 128:192], lamt[:, 192:256], ALU.mult, r=["alam", "alamp"], w=["alamp"])
            add("dve", lambda e: e.reduce_sum(out=lamc[:, 0:1], in_=lamp[:, 0:64], axis=AX.X), r=["alamp"], w=["alamc"])
            add("dve", lambda e: e.reduce_sum(out=lamc[:, 1:2], in_=lamp[:, 64:128], axis=AX.X), r=["alamp", "alamc"], w=["alamc"])
            act(lamc[:, 0:2], lamc[:, 0:2], AF.Exp, r=["alamc"], w=["alamc"])
            tt("dve", lamc[:, 2:3], lamc[:, 1:2], lamc[:, 0:1], ALU.subtract, r=["alamc"], w=["alamc"])
            ts("dve", lamc[:, 2:3], lamc[:, 2:3], -lam_init, None, ALU.add, None, r=["alamc"], w=["alamc"])
            for i in range(2):
                add("pool", lambda e, i=i: e.memset(vmt[i][:, :, 64:65], 1.0), w=["avm1_%d" % i])
            PS_S = [(PSB[i], psn[i]) for i in (0, 1, 2, 3, 6, 7)]
            NPS = 6
            PO = [(PSB[4], psn[4]), (PSB[5], psn[5])]
            sctr = [0]
            pctr = [0]
            for hh in range(16):
                isd = hh < 8
                h = hh % 8
                i = hh % 2
                if isd:
                    dma("sp", kdt[i][:], KD[h], r=[], w=["akd%d" % i], key="akd%d" % i)
                    dma("sp", vdt[i][:], VD[:, h * 128:(h + 1) * 128].rearrange("(b p) e -> p b e", p=128), r=[], w=["avd%d" % i], key="avd%d" % i)
                else:
                    pr, hp = h // 2, h % 2
                    dma("sp", kdt[i][0:64, :], KMN[pr, hp * 64:(hp + 1) * 64, :], r=[], w=["akd%d" % i], key="akd%d" % i)
                    dma("sp", kdt[i][64:80, :], KR[0], r=[], w=["akdr1_%d" % i], key="akdr1_%d" % i)
                    dma("sp", kdt[i][80:96, :], KR[1], r=[], w=["akdr2_%d" % i], key="akdr2_%d" % i)
                    dma("sp", vmt[i][:, :, 0:64], VM[:, h * 64:(h + 1) * 64].rearrange("(b p) e -> p b e", p=128), r=[], w=["avm%d" % i], key="avm%d" % i)
                kn_ = ["akd%d" % i] + ([] if isd else ["akdr1_%d" % i, "akdr2_%d" % i])
                vn_ = ["avd%d" % i] if isd else ["avm%d" % i, "avm1_%d" % i]
                for t in range(NT):
                    qi = (hh * NT + t) % 2
                    tsl = slice(t * 512, (t + 1) * 512)
                    if isd:
                        dma("sp", qt[qi][:], QD[h, :, tsl], r=[], w=["aq%d" % qi], key="aq%d" % qi)
                        qn_ = ["aq%d" % qi]
                    else:
                        pr, hp = h // 2, h % 2
                        dma("sp", qt[qi][0:64, :], QMN[pr, hp * 64:(hp + 1) * 64, tsl], r=[], w=["aq%d" % qi], key="aq%d" % qi)
                        dma("sp", qt[qi][64:80, :], QR[0, h * 16:(h + 1) * 16, tsl], r=[], w=["aqr1_%d" % qi], key="aqr1_%d" % qi)
                        dma("sp", qt[qi][80:96, :], QR[1, h * 16:(h + 1) * 16, tsl], r=[], w=["aqr2_%d" % qi], key="aqr2_%d" % qi)
                        qn_ = ["aq%d" % qi, "aqr1_%d" % qi, "aqr2_%d" % qi]
                    nkb = 4 * t + 4
                    maps = (0, 1) if isd else (0,)

                    def stage1(kb):
                        delta = 512 * t - 128 * kb
                        c0 = max(0, -delta)
                        near = delta < 256
                        ksl = slice(kb * 128, (kb + 1) * 128)
                        res_ = []
                        for m in maps:
                            pt, pn = PS_S[sctr[0] % NPS]
                            sctr[0] += 1
                            pb = pT[pctr[0] % NPT]
                            pbn = "apT%d" % (pctr[0] % NPT)
                            pctr[0] += 1
                            rows = slice(m * 64, (m + 1) * 64) if isd else slice(0, 96)
                            mm1(pt[:, c0:512], kdt[i][rows, ksl], qt[qi][rows, c0:512], True, True, r=kn_ + qn_, w=[pn])
                            if isd and near:
                                si = sbc2[0] % 4
                                sbc2[0] += 1
                                sbt = sbias[si]
                                tt("dve", sbt[:, c0:512], pt[:, c0:512], bstr[:, h, delta + 384 + c0:delta + 384 + 512], ALU.add,
                                   r=[pn, "abst"], w=["asb%d" % si])
                                act(pb[:, c0:512], sbt[:, c0:512], AF.Exp, r=["asb%d" % si], w=[pbn])
                            elif isd:
                                act(pb[:, c0:512], pt[:, c0:512], AF.Exp, r=[pn, "tabb"], w=[pbn], bias=tabb[:, 248 + h:249 + h], scale=1.0)
                            elif delta <= 0:
                                si = sbc2[0] % 4
                                sbc2[0] += 1
                                sbt = sbias[si]
                                tt("dve", sbt[:, c0:512], pt[:, c0:512], bstr[:, 8, delta + 384 + c0:delta + 384 + 512], ALU.add,
                                   r=[pn, "abst"], w=["asb%d" % si])
                                act(pb[:, c0:512], sbt[:, c0:512], AF.Exp, r=["asb%d" % si], w=[pbn])
                            else:
                                act(pb[:, c0:512], pt[:, c0:512], AF.Exp, r=[pn], w=[pbn])
                            res_.append((pb, pbn, c0))
                        return res_

                    def stage2(kb, res_):
                        first, last = kb == 0, kb == nkb - 1
                        for m, (pb, pbn, c0) in zip(maps, res_):
                            if isd:
                                mm1(PO[m][0][:, c0:512], vdt[i][:, kb, :], pb[:, c0:512], first, last, r=vn_ + [pbn], w=[PO[m][1]])
                                if first:
                                    cp("dve", accS[m][:], pb[:], r=[pbn], w=["aacc%d" % m])
                                else:
                                    tt("dve", accS[m][:, c0:512], accS[m][:, c0:512], pb[:, c0:512], ALU.add, r=[pbn, "aacc%d" % m], w=["aacc%d" % m])
                            else:
                                mm1(PO[0][0][0:65, c0:512], vmt[i][:, kb, :], pb[:, c0:512], first, last, r=vn_ + [pbn], w=[PO[0][1]])
                    q_ = [stage1(0)]
                    if nkb > 1:
                        q_.append(stage1(1))
                    for kb in range(nkb):
                        if kb + 2 < nkb:
                            q_.append(stage1(kb + 2))
                        stage2(kb, q_.pop(0))
                    oi = (hh * NT + t) % 2
                    if isd:
                        for m in range(2):
                            cp("dve", accB[m][:], accS[m][:], r=["aacc%d" % m], w=["aaccb%d" % m])
                            pts, pns = PS_S[sctr[0] % NPS]
                            sctr[0] += 1
                            mm1(pts[0:1, :], ones_bf[:, 0:1], accB[m][:], True, True, r=["ones_bf", "aaccb%d" % m], w=[pns])
                            rrm = rr if m == 0 else rr2
                            act(rrm[0:1, :], pts[0:1, :], AF.Ln, r=[pns], w=["arr%d" % m])
                            act(rrm[0:1, :], rrm[0:1, :], AF.Exp, r=["arr%d" % m], w=["arr%d" % m], scale=-1.0)
                            pt, pn = PS_S[sctr[0] % NPS]
                            sctr[0] += 1
                            mm1(pt[:], ones_f[0:1, :], rrm[0:1, :], True, True, r=["ones_f", "arr%d" % m], w=[pn])
                            cp("act", Rb[m][:], pt[:], r=[pn], w=["aRb%d" % m])
                        tt("dve", t0[:], PO[0][0][:], Rb[0][:], ALU.mult, r=[PO[0][1], "aRb0"], w=["at0"])
                        tt("dve", t1[:], PO[1][0][:], Rb[1][:], ALU.mult, r=[PO[1][1], "aRb1"], w=["at1"])
                        stt("dve", t0[:], t1[:], lamc[:, 2:3], t0[:], ALU.mult, ALU.add, r=["at0", "at1", "alamc"], w=["at0"])
                        act(osq[:], t0[:], AF.Square, r=["at0"], w=["aosq"])
                        pt, pn = PS_S[sctr[0] % NPS]
                        sctr[0] += 1
                        mm1(pt[:], ones_bf[:], osq[:], True, True, r=["ones_bf", "aosq"], w=[pn])
                        rstd_from(rms[:], pt[:], 1.0 / 128.0, r=[pn], w=["arms"])
                        stt("dve", t1[:], t0[:], subg[:, 0:1], rms[:], ALU.mult, ALU.mult, r=["at0", "asubg", "arms"], w=["at1"])
                        ts("dve", oo[oi][:], t1[:], 1.0 - lam_init, None, ALU.mult, None, r=["at1"], w=["aoo%d" % oi])
                        dma("sp", OAT[h, :, tsl], oo[oi][:], r=["aoo%d" % oi], w=[], key="aoo%d" % oi)
                    else:
                        act(rr[64:65, :], PO[0][0][64:65, :], AF.Ln, r=[PO[0][1]], w=["arr0"])
                        act(rr[64:65, :], rr[64:65, :], AF.Exp, r=["arr0"], w=["arr0"], scale=-1.0)
                        pt, pn = PS_S[sctr[0] % NPS]
                        sctr[0] += 1
                        mm1(pt[0:64, :], ones_f[64:65, 0:64], rr[64:65, :], True, True, r=["ones_f", "arr0"], w=[pn])
                        cp("act", Rb[0][0:64, :], pt[0:64, :], r=[pn], w=["aRb0"])
                        tt("dve", oo[oi][0:64, :], PO[0][0][0:64, :], Rb[0][0:64, :], ALU.mult, r=[PO[0][1], "aRb0"], w=["aoo%d" % oi])
                        dma("sp", OBT[h, :, tsl], oo[oi][0:64, :], r=["aoo%d" % oi], w=[], key="aoo%d" % oi)
        S_.barrier()
        if upto == 'A':
            S_.emit(nc)
            es.close()
            return nc

        with ExitStack() as ph:
            alloc_psum(ph, 6, 2)
            wbd = sb(ph, "mwbd", [128, 8, D], BF16)
            wbm = sb(ph, "mwbm", [64, 8, D], BF16)
            wout = sb(ph, "mwout", [128, 8, D], BF16)
            lng = sb(ph, "mlng", [128, 2, D], F32)
            oa = [sb(ph, "moa%d" % i, [128, 8, 512], BF16) for i in range(2)]
            ob = [sb(ph, "mob%d" % i, [64, 8, 512], BF16) for i in range(2)]
            gs = [sb(ph, "mg%d" % i, [128, 16, 512], BF16) for i in range(2)]
            xs = [sb(ph, "mxs%d" % i, [128, 4, D], F32) for i in range(2)]
            mT = sb(ph, "mmT", [128, 8, 512], BF16)
            tA = [sb(ph, "mtA%d" % i, [128, 512], F32) for i in range(2)]
            tB = [sb(ph, "mtB%d" % i, [128, 512], F32) for i in range(2)]
            zs = [sb(ph, "mzs%d" % i, [128, D], F32) for i in range(2)]
            x1 = [sb(ph, "mx1%d" % i, [128, D], F32) for i in range(2)]
            x1b = sb(ph, "mx1b", [128, D], BF16)
            x1T = sb(ph, "mx1T", [128, 8, 512], BF16)
            st = sb(ph, "mst", [128, 12], F32)
            mv = sb(ph, "mmv", [128, 2], F32)
            rs = sb(ph, "mrs", [128, 1], F32)
            stg_state["tiles"] = [sb(ph, "mstg%d" % i, [128, 1024], F32) for i in range(2)]
            for k in range(8):
                wload(wbd[:, k, :], wbd_in[li, k * 128:(k + 1) * 128, :], 1024, "mwbd")
                wload(wbm[:, k, :], wbm_in[li, k * 64:(k + 1) * 64, :], 1024, "mwbm", parts=64)
                wload(wout[:, k, :], wout_in[li, k * 128:(k + 1) * 128, :], 1024, "mwout")
            dma("sp", lng[:].rearrange("p a d -> p (a d)"), lnm_in[li].rearrange("a d -> (a d)").partition_broadcast(128), r=[], w=["mlng"], key="mlng")

            def ldm(t):
                i = t % 2
                tsl = slice(t * 512, (t + 1) * 512)
                dma("sp", oa[i][:], OAT[:, :, tsl].rearrange("h p s -> p h s"), r=[], w=["moa%d" % i], key="moa%d" % i)
                dma("sp", ob[i][:], OBT[:, :, tsl].rearrange("h p s -> p h s"), r=[], w=["mob%d" % i], key="mob%d" % i)
                dma("sp", gs[i][:], GT[:, :, tsl].rearrange("g p s -> p g s"), r=[], w=["mg%d" % i], key="mg%d" % i)
                dma("sp", xs[i][:], x_src[tsl, :].rearrange("(j p) d -> p j d", p=128), r=[], w=["mxs%d" % i], key="mxs%d" % i)
            ldm(0)
            for t in range(NT):
                i = t % 2
                tsl = slice(t * 512, (t + 1) * 512)
                if t + 1 < NT:
                    ldm(t + 1)
                for db in range(8):
                    dsl = slice(db * 128, (db + 1) * 128)
                    ptA, pnA = nps()
                    mm(ptA[:], [(wbd[:, h, dsl], oa[i][:, h, :]) for h in range(8)], r=["mwbd", "moa%d" % i], w=[pnA])
                    ptB, pnB = nps()
                    mm(ptB[:], [(wbm[:, h, dsl], ob[i][:, h, :]) for h in range(8)], r=["mwbm", "mob%d" % i], w=[pnB])
                    a = db % 2
                    tt("dve", tA[a][:], ptA[:], gs[i][:, db, :], ALU.mult, r=[pnA, "mg%d" % i], w=["mtA%d" % a])
                    tt("dve", tB[a][:], ptB[:], gs[i][:, 8 + db, :], ALU.mult, r=[pnB, "mg%d" % i], w=["mtB%d" % a])
                    tt("pool", mT[:, db, :], tA[a][:], tB[a][:], ALU.add, r=["mtA%d" % a, "mtB%d" % a], w=["mmT%d" % db])
                mTn = ["mmT%d" % db for db in range(8)]
                for j in range(4):
                    a = j % 2
                    for hf in range(2):
                        hsl = slice(hf * 512, (hf + 1) * 512)
                        pt, pn = nps()
                        mm(pt[:], [(mT[:, db, j * 128:(j + 1) * 128], wout[:, db, hsl]) for db in range(8)], r=["mwout"] + mTn, w=[pn])
                        stt("dve", zs[a][:, hsl], xs[i][:, j, hsl], ALPHA, pt[:], ALU.mult, ALU.add, r=["mxs%d" % i, pn], w=["mzs%d_%d" % (a, hf)])
                    zn = ["mzs%d_0" % a, "mzs%d_1" % a]
                    layernorm(add, ts, tt, act, rstd_from, zs[a], x1[a], st, mv, rs, lng, zn, "mx1%d" % a, "m")
                    dma("sp", X1[t * 512 + j * 128:t * 512 + (j + 1) * 128, :], x1[a][:], r=["mx1%d" % a], w=[], key="mx1%d" % a)
                    cp("pool", x1b[:], x1[a][:], r=["mx1%d" % a], w=["mx1b"])
                    for kq in range(2):
                        def tr(e, kq=kq, PSTT=tuple(PSTT)):
                            ins = None
                            for k4 in range(4):
                                kb = kq * 4 + k4
                                ins = e.transpose(out=PSTT[kq][:, k4 * 128:(k4 + 1) * 128],
                                                  in_=x1b[:, kb * 128:(kb + 1) * 128], identity=ident[:])
                            return ins
                        add("pe", tr, r=["mx1b", "ident"], w=["pst%d" % kq])
                        add("act", lambda e, kq=kq, j=j, src=PSTT[kq]: e.copy(out=x1T[:, kq * 4:(kq + 1) * 4, j * 128:(j + 1) * 128],
                                                               in_=src[:, 0:512].rearrange("p (k s) -> p k s", k=4)),
                            r=["pst%d" % kq], w=["mx1T_%d_%d" % (j, kq)])
                dma("sp", X1T[:, :, tsl].rearrange("k p s -> p k s"), x1T[:], r=["mx1T_%d_%d" % (j, kq) for j in range(4) for kq in range(2)], w=[], key="mx1T")
        S_.barrier()
        if upto == 'M':
            S_.emit(nc)
            es.close()
            return nc

        with ExitStack() as ph:
            alloc_psum(ph, 6, 2)
            wpg = sb(ph, "ewpg", [128, 8, D], BF16)
            wpp = sb(ph, "ewpp", [128, 2, D], BF16)
            xT = [sb(ph, "exT%d" % i, [128, 8, 512], BF16) for i in range(2)]
            pp = [sb(ph, "epp%d" % i, [128, 4, 256], F32) for i in range(2)]
            ppb = sb(ph, "eppb", [128, 4, 256], BF16)
            ppT = sb(ph, "eppT", [128, 2, 512], BF16)
            sg = [sb(ph, "esg%d" % i, [128, 512], F32) for i in range(2)]
            ee = [sb(ph, "eee%d" % i, [128, 4, D], F32) for i in range(2)]
            stg_state["tiles"] = [sb(ph, "estg%d" % i, [128, 1024], F32) for i in range(2)]
            for k in range(8):
                wload(wpg[:, k, :], wpg_in[li, k * 128:(k + 1) * 128, :], 1024, "ewpg")
            for k in range(2):
                wload(wpp[:, k, :], wpp_in[li, k * 128:(k + 1) * 128, :], 1024, "ewpp")

            def lde(t):
                i = t % 2
                tsl = slice(t * 512, (t + 1) * 512)
                dma("sp", xT[i][:], X1T[:, :, tsl].rearrange("k p s -> p k s"), r=[], w=["exT%d" % i], key="exT%d" % i)
                dma("sp", pp[i][:], p_in[li, tsl, :].rearrange("(j p) c -> p j c", p=128), r=[], w=["epp%d" % i], key="epp%d" % i)
            lde(0)
            for t in range(NT):
                i = t % 2
                tsl = slice(t * 512, (t + 1) * 512)
                if t + 1 < NT:
                    lde(t + 1)
                cp("pool", ppb[:], pp[i][:], r=["epp%d" % i], w=["eppb"])
                for kq in range(2):
                    def tr(e, kq=kq, PSTT=tuple(PSTT)):
                        ins = None
                        for j in range(4):
                            ins = e.transpose(out=PSTT[kq][:, j * 128:(j + 1) * 128],
                                              in_=ppb[:, j, kq * 128:(kq + 1) * 128], identity=ident[:])
                        return ins
                    add("pe", tr, r=["eppb", "ident"], w=["pst%d" % kq])
                    cp("act", ppT[:, kq, :], PSTT[kq][:, 0:512], r=["pst%d" % kq], w=["eppT%d" % kq])
                for j in range(4):
                    for hf in range(2):
                        hsl = slice(hf * 512, (hf + 1) * 512)
                        a = hf
                        ptA, pnA = nps()
                        mm(ptA[:], [(xT[i][:, kb, j * 128:(j + 1) * 128], wpg[:, kb, hsl]) for kb in range(8)], r=["ewpg", "exT%d" % i], w=[pnA])
                        ptB, pnB = nps()
                        mm(ptB[:], [(ppT[:, kq, j * 128:(j + 1) * 128], wpp[:, kq, hsl]) for kq in range(2)], r=["ewpp", "eppT0", "eppT1"], w=[pnB])
                        act(sg[a][:], ptA[:], AF.Sigmoid, r=[pnA], w=["esg%d" % a])
                        tt("dve", ee[i][:, j, hsl], sg[a][:], ptB[:], ALU.mult, r=["esg%d" % a, pnB], w=["eee%d_%d_%d" % (i, j, hf)])
                dma("sp", EE[tsl, :].rearrange("(j p) d -> p j d", p=128), ee[i][:],
                    r=["eee%d_%d_%d" % (i, j, hf) for j in range(4) for hf in range(2)], w=[], key="eee%d" % i)
        S_.barrier()
        if upto == 'E':
            S_.emit(nc)
            es.close()
            return nc

        moe = (li % 2 == 1)
        lj = li // 2
        if moe:
            with ExitStack() as ph2:
                rwb = sb(ph2, "frw", [128, 8, D], F32)
                rx1 = [sb(ph2, "rx1%d" % i, [128, D], F32) for i in range(2)]
                lg = sb(ph2, "flg", [128, 8], F32)
                junk = sb(ph2, "fjunk", [128, D], F32)
                mx8 = sb(ph2, "fmx8", [128, 8], F32)
                msk = sb(ph2, "fmsk", [128, 8], F32)
                ex = sb(ph2, "fex", [128, 8], F32)
                den = sb(ph2, "fden", [128, 2], F32)
                dma("sp", rwb[:].rearrange("p a d -> p (a d)"), rw_in[lj].partition_broadcast(128), r=[], w=["frw"], key="frw")
                for sbi in range(NB):
                    a = sbi % 2
                    dma("sp", rx1[a][:], X1[sbi * 128:(sbi + 1) * 128, :], r=[], w=["rx1%d" % a], key="rx1%d" % a)
                    for ex_ in range(8):
                        add("dve", lambda e, a=a, ex_=ex_: e.scalar_tensor_tensor(out=junk[:], in0=rx1[a][:], scalar=1.0, in1=rwb[:, ex_, :], op0=ALU.mult, op1=ALU.mult, accum_out=lg[:, ex_:ex_ + 1]),
                            r=["rx1%d" % a, "frw", "flg", "fjunk"], w=["flg", "fjunk"])
                    add("dve", lambda e: e.max(out=mx8[:], in_=lg[:]), r=["flg"], w=["fmx8"])
                    ts("dve", msk[:], lg[:], mx8[:, 1:2], None, ALU.is_ge, None, r=["flg", "fmx8"], w=["fmsk"])
                    ts("dve", ex[:], lg[:], mx8[:, 0:1], None, ALU.subtract, None, r=["flg", "fmx8"], w=["fex"])
                    act(ex[:], ex[:], AF.Exp, r=["fex"], w=["fex"])
                    tt("dve", ex[:], ex[:], msk[:], ALU.mult, r=["fex", "fmsk"], w=["fex"])
                    add("dve", lambda e: e.reduce_sum(out=den[:, 0:1], in_=ex[:], axis=AX.X), r=["fex"], w=["fden"])
                    add("dve", lambda e: e.reciprocal(out=den[:, 1:2], in_=den[:, 0:1]), r=["fden"], w=["fden"])
                    ts("dve", gates[:, sbi, :], ex[:], den[:, 1:2], None, ALU.mult, None, r=["fex", "fden"], w=["gates"])
            S_.barrier()

        with ExitStack() as ph:
            alloc_psum(ph, 8, 0)
            NSUB = TG // 128
            NCB = 7
            Y = sb(ph, "fY", [128, NSUB, D], F32)
            xT = sb(ph, "fxT", [128, 8, TG], BF16)
            wa = [sb(ph, "fwa%d" % i, [128, 8, NCB * 128], BF16) for i in range(2)]
            wb = [sb(ph, "fwb%d" % i, [128, 8, NCB * 128], BF16) for i in range(2)]
            wc = [sb(ph, "fwc%d" % i, [128, NCB, D], BF16) for i in range(2)]
            GTt = [sb(ph, "fG%d" % i, [128, NCB, 512], BF16) for i in range(2)]
            sl = [sb(ph, "fsl%d" % i, [128, 512], F32) for i in range(2)]
            x1 = sb(ph, "fx1", [128, D], F32)
            zz = sb(ph, "fzz", [128, D], F32)
            xo = sb(ph, "fxo", [128, D], F32)
            lng = sb(ph, "flng", [128, 2, D], F32)
            st = sb(ph, "fst", [128, 12], F32)
            mv = sb(ph, "fmv", [128, 2], F32)
            rs = sb(ph, "frs", [128, 1], F32)
            stg_state["tiles"] = [sb(ph, "fstg%d" % i, [128, 1024], F32) for i in range(3)]
            dma("sp", lng[:].rearrange("p a d -> p (a d)"), lnf_in[li].rearrange("a d -> (a d)").partition_broadcast(128), r=[], w=["flng"], key="flng")
            FF = EXP_FF if moe else DENSE_FF
            nblk = FF // 128
            chunks = []
            b0 = 0
            while b0 < nblk:
                nb_ = min(NCB, nblk - b0)
                if nblk - b0 - nb_ in (1, 2) and nb_ > 3:
                    nb_ -= 2
                chunks.append((b0, nb_))
                b0 += nb_
            nexp = 8 if moe else 1
            seq = [(g, ex_, c) for g in range(NG) for ex_ in range(nexp) for c in range(len(chunks))]

            def wjobs(k):
                g, ex_, c = seq[k]
                i = k % 2
                b0, nb_ = chunks[c]
                if moe:
                    s1, s3, s2 = ew1_in[lj, ex_], ew3_in[lj, ex_], ew2_in[lj, ex_]
                else:
                    s1, s3, s2 = dw1_in[lj], dw3_in[lj], dw2_in[lj]
                jobs = []
                wdt = nb_ * 128
                for kb in range(8):
                    jobs.append(lambda kb=kb: wload(wa[i][:, kb, 0:wdt], s1[kb * 128:(kb + 1) * 128, b0 * 128:b0 * 128 + wdt], wdt, "fwa%d_%d" % (i, kb)))
                    jobs.append(lambda kb=kb: wload(wb[i][:, kb, 0:wdt], s3[kb * 128:(kb + 1) * 128, b0 * 128:b0 * 128 + wdt], wdt, "fwb%d_%d" % (i, kb)))
                for jb in range(nb_):
                    jobs.append(lambda jb=jb: wload(wc[i][:, jb, :], s2[(b0 + jb) * 128:(b0 + jb + 1) * 128, :], 1024, "fwc%d_%d" % (i, jb)))
                return jobs
            for jfn in wjobs(0):
                jfn()
            gctr = [0]
            for k, (g, ex_, c) in enumerate(seq):
                i = k % 2
                b0, nb_ = chunks[c]
                gsl = slice(g * TG, (g + 1) * TG)
                if ex_ == 0 and c == 0:
                    dma("sp", xT[:], X1T[:, :, gsl].rearrange("k p s -> p k s"), r=[], w=["fxT"], key="fxT")
                    dma("sp", Y[:], EE[gsl, :].rearrange("(j p) d -> p j d", p=128), r=[], w=["fY%d" % s_ for s_ in range(NSUB)], key="fY")
                pend = wjobs(k + 1) if k + 1 < len(seq) else []
                pend.reverse()

                def pump(n=1):
                    for _ in range(n):
                        if pend:
                            pend.pop()()
                wan = ["fwa%d_%d" % (i, kb) for kb in range(8)]
                wbn = ["fwb%d_%d" % (i, kb) for kb in range(8)]
                for tl in range(TG // 512):
                    gi = gctr[0] % 2
                    gctr[0] += 1
                    tsl = slice(tl * 512, (tl + 1) * 512)
                    for jb in range(nb_):
                        fsl = slice(jb * 128, (jb + 1) * 128)
                        pump(1)
                        pt1, pn1 = nps()
                        mm(pt1[:], [(wa[i][:, kb, fsl], xT[:, kb, tsl]) for kb in range(8)], r=wan + ["fxT"], w=[pn1])
                        pt3, pn3 = nps()
                        mm(pt3[:], [(wb[i][:, kb, fsl], xT[:, kb, tsl]) for kb in range(8)], r=wbn + ["fxT"], w=[pn3])
                        a = jb % 2
                        act(sl[a][:], pt1[:], AF.Silu, r=[pn1], w=["fsl%d" % a])
                        tt("dve", GTt[gi][:, jb, :], sl[a][:], pt3[:], ALU.mult, r=["fsl%d" % a, pn3], w=["fG%d_%d" % (gi, jb)])
                    Gn = ["fG%d_%d" % (gi, jb) for jb in range(nb_)]
                    wcn = ["fwc%d_%d" % (i, jb) for jb in range(nb_)]
                    for j4 in range(4):
                        sub = tl * 4 + j4
                        sbi = g * NSUB + sub
                        for hf in range(2):
                            hsl = slice(hf * 512, (hf + 1) * 512)
                            pump(1)
                            pt, pn = nps()
                            mm(pt[:], [(GTt[gi][:, jb, j4 * 128:(j4 + 1) * 128], wc[i][:, jb, hsl]) for jb in range(nb_)], r=wcn + Gn, w=[pn])
                            gcol = gates[:, sbi, ex_:ex_ + 1] if moe else ccol[:, 2:3]
                            stt("dve", Y[:, sub, hsl], pt[:], gcol, Y[:, sub, hsl], ALU.mult, ALU.add,
                                r=[pn, "fY%d" % sub, "gates", "ccol"], w=["fY%d" % sub])
                pump(100)
                if ex_ == nexp - 1 and c == len(chunks) - 1:
                    for sub in range(NSUB):
                        r0 = g * TG + sub * 128
                        dma("sp", x1[:], X1[r0:r0 + 128, :], r=[], w=["fx1"], key="fx1")
                        stt("dve", zz[:], x1[:], ALPHA, Y[:, sub, :], ALU.mult, ALU.add, r=["fx1", "fY%d" % sub], w=["fzz"])
                        layernorm(add, ts, tt, act, rstd_from, zz, xo, st, mv, rs, lng, ["fzz"], "fxo", "f")
                        dma("sp", x_dst[r0:r0 + 128, :], xo[:], r=["fxo"], w=[], key="fxo")
        S_.barrier()

    S_.emit(nc)
    es.close()
    return nc


def layernorm(add, ts, tt, act, rstd_from, z, xo, st, mv, rs, lng, zn, xon, pfx):
    stn, mvn, rsn = pfx + "st", pfx + "mv", pfx + "rs"
    add("dve", lambda e: e.bn_stats(out=st[:, 0:6], in_=z[:, 0:512]), r=zn, w=[stn])
    add("dve", lambda e: e.bn_stats(out=st[:, 6:12], in_=z[:, 512:1024]), r=zn + [stn], w=[stn])
    add("dve", lambda e: e.bn_aggr(out=mv[:], in_=st[:]), r=[stn], w=[mvn])
    rstd_from(rs[:], mv[:, 1:2], 1.0, r=[mvn], w=[rsn])
    ts("dve", xo[:], z[:], mv[:, 0:1], rs[:, 0:1], ALU.subtract, ALU.mult, r=zn + [mvn, rsn], w=[xon])
    tt("pool", xo[:], xo[:], lng[:, 0, :], ALU.mult, r=[xon, pfx + "lng"], w=[xon])
    tt("pool", xo[:], xo[:], lng[:, 1, :], ALU.add, r=[xon, pfx + "lng"], w=[xon])


def prep_inputs(inp, b, S, L):
    f = np.float32
    def c(a):
        return np.ascontiguousarray(a)
    NM = L // 2
    wuq = np.asarray(inp["w_uq"])[:L].reshape(L, 384, 8, 96)
    wuq_p = np.concatenate([wuq[..., 0:64].reshape(L, 384, 512), wuq[..., 64:80].reshape(L, 384, 128),
                            wuq[..., 80:96].reshape(L, 384, 128)], axis=-1)
    wukv = np.asarray(inp["w_ukv"])[:L].reshape(L, 256, 8, 128)
    wukv_p = np.concatenate([wukv[..., 0:64].reshape(L, 256, 512), wukv[..., 64:128].reshape(L, 256, 512)], axis=-1)
    lam = np.concatenate([np.asarray(inp[k])[:L] for k in ("lambda_q1", "lambda_k1", "lambda_q2", "lambda_k2")], axis=-1).reshape(L, 1, 256)
    d = {
        "x": c(np.asarray(inp["x"])[b, :S]),
        "p": c(np.asarray(inp["p"])[:L, b, :S]),
        "pos": c(np.asarray(inp["positions"])[b, :S].reshape(1, S).astype(np.int32)),
        "tab": c(np.asarray(inp["rel_bias_table"]).reshape(1, 256)),
        "w_in": c(np.asarray(inp["w_in"])[:L]),
        "bg": c(np.asarray(inp["b_gate"])[:L].reshape(L, 16, 128).transpose(0, 2, 1)),
        "lam": c(lam),
        "subg": c(np.asarray(inp["diff_subln_g"])[:L].reshape(L, 128, 1)),
        "gq": c(np.asarray(inp["mla_q_norm_g"])[:L].reshape(L, 3, 128).transpose(0, 2, 1)),
        "wuq": c(wuq_p),
        "gkv": c(np.asarray(inp["mla_kv_norm_g"])[:L].reshape(L, 2, 128).transpose(0, 2, 1)),
        "wukv": c(wukv_p),
        "wbd": c(np.asarray(inp["w_branch_diff"])[:L]),
        "wbm": c(np.asarray(inp["w_branch_mla"])[:L]),
        "wout": c(np.asarray(inp["w_out"])[:L]),
        "lnm": c(np.stack([np.asarray(inp["ln_mix_g"])[:L], np.asarray(inp["ln_mix_b"])[:L]], axis=1)),
        "dw1": c(np.asarray(inp["dense_w1"])[:(L + 1) // 2]),
        "dw3": c(np.asarray(inp["dense_w3"])[:(L + 1) // 2]),
        "dw2": c(np.asarray(inp["dense_w2"])[:(L + 1) // 2]),
        "rw": c(np.asarray(inp["router_w"])[:max(NM, 1)].transpose(0, 2, 1).reshape(max(NM, 1), 1, 8 * 1024)),
        "ew1": c(np.asarray(inp["expert_w1"])[:max(NM, 1)]),
        "ew3": c(np.asarray(inp["expert_w3"])[:max(NM, 1)]),
        "ew2": c(np.asarray(inp["expert_w2"])[:max(NM, 1)]),
        "wpg": c(np.asarray(inp["w_ple_gate"])[:L]),
        "wpp": c(np.asarray(inp["w_ple_proj"])[:L]),
        "lnf": c(np.stack([np.asarray(inp["ln_ffn_g"])[:L], np.asarray(inp["ln_ffn_b"])[:L]], axis=1)),
    }
    cst = np.zeros((128, 4), f)
    half = 16
    invf = (np.float32(10000.0) ** (-np.arange(half, dtype=f) / np.float32(half))).astype(f)
    cst[:, 0] = np.tile(invf, 8)
    d["cst"] = cst
    return {k: np.ascontiguousarray(v) for k, v in d.items()}


_NC_CACHE = {}


def kernel(**inputs):
    S, L, NCORES = 4096, 4, 8
    if "nc" not in _NC_CACHE:
        _NC_CACHE["nc"] = build(S, L, TG=1024)
    nc = _NC_CACHE["nc"]
    in_maps = [prep_inputs(inputs, b, S, L) for b in range(NCORES)]
    res = run_bass_kernel_spmd(nc, in_maps, core_ids=list(range(NCORES)))
    return np.stack([np.asarray(r["out"], dtype=np.float32) for r in res.results], axis=0)
```

```python
import os
import math
from contextlib import ExitStack
import numpy as np
import concourse.bass as bass
import concourse.mybir as mybir
from concourse.bass_utils import run_bass_kernel_spmd

F32 = mybir.dt.float32
BF16 = mybir.dt.bfloat16
I32 = mybir.dt.int32
AF = mybir.ActivationFunctionType
ALU = mybir.AluOpType
AX = mybir.AxisListType

ENGS = ("pe", "act", "dve", "pool", "sp")
SEM_MAX = 20000
NSLOT = 84


class Op:
    __slots__ = ("eng", "fn", "deps", "dma", "slot", "idx", "sig", "ordn", "barrier")

    def __init__(self, eng, fn, dma):
        self.eng, self.fn, self.dma = eng, fn, dma
        self.deps = []
        self.sig = False
        self.ordn = None
        self.slot = None
        self.barrier = False


class Sched:
    def __init__(self):
        self.ops = {e: [] for e in ENGS}
        self.state = {}
        self.slot_of = {}
        self.slot_cnt = [0] * NSLOT
        self.slot_last = [None] * NSLOT
        self.nslot_used = 0
        self.max_slots = 0

    def add(self, eng, fn, r=(), w=(), dma=None):
        op = Op(eng, fn, dma)
        w = list(w) + [b for b in r if b.startswith('ps')]
        r = [b for b in r if not b.startswith('ps')]
        deps = []
        for b in r:
            st = self.state.setdefault(b, [None, []])
            if st[0] is not None:
                deps.append(st[0])
        for b in w:
            st = self.state.setdefault(b, [None, []])
            if st[0] is not None:
                deps.append(st[0])
            deps.extend(st[1])
        seen = set()
        for d in deps:
            if id(d) in seen:
                continue
            seen.add(id(d))
            if d.dma is None and op.dma is None and d.eng == "pe" and eng == "pe":
                continue
            op.deps.append(d)
            if d.dma is None:
                d.sig = True
        if dma is not None:
            if dma not in self.slot_of:
                assert self.nslot_used < NSLOT, "out of dma semaphore slots"
                self.slot_of[dma] = self.nslot_used
                self.nslot_used += 1
                self.max_slots = max(self.max_slots, self.nslot_used)
            sl = self.slot_of[dma]
            self.slot_cnt[sl] += 1
            assert self.slot_cnt[sl] * 16 < 32000, "dma sem count too large: %s" % dma
            op.slot = sl
            op.ordn = self.slot_cnt[sl]
            self.slot_last[sl] = op
        op.idx = len(self.ops[eng])
        self.ops[eng].append(op)
        for b in r:
            self.state[b][1].append(op)
        for b in w:
            self.state[b] = [op, []]
        return op

    def barrier(self):
        used = self.nslot_used
        c = Op("sp", None, None)
        c.barrier = True
        for e in ENGS:
            if e == "sp":
                continue
            for op in reversed(self.ops[e]):
                if op.dma is None:
                    if not op.barrier:
                        c.deps.append(op)
                        op.sig = True
                    break
        for sl in range(used):
            if self.slot_last[sl] is not None:
                c.deps.append(self.slot_last[sl])
        c.slot = used
        c.sig = True
        c.idx = len(self.ops["sp"])
        self.ops["sp"].append(c)
        for e in ENGS:
            if e == "sp":
                continue
            d = Op(e, None, None)
            d.barrier = True
            d.deps.append(c)
            d.idx = len(self.ops[e])
            self.ops[e].append(d)
        self.state = {}
        self.slot_of = {}
        self.slot_cnt = [0] * NSLOT
        self.slot_last = [None] * NSLOT
        self.nslot_used = 0

    def emit(self, nc):
        from contextlib import ExitStack
        self.barrier()
        nsig = {}
        for e in ENGS:
            n = 0
            for op in self.ops[e]:
                if op.dma is None and op.sig:
                    op.ordn = n
                    n += 1
            nsig[e] = n
        with ExitStack() as es:
            esem = {}
            for e in ENGS:
                k = max(1, (nsig[e] + SEM_MAX - 1) // SEM_MAX)
                esem[e] = [es.enter_context(nc.semaphore("s_%s_%d" % (e, i))) for i in range(k)]
            dsem = [es.enter_context(nc.semaphore("d_%d" % i)) for i in range(self.max_slots)]
            block = es.enter_context(nc.Block())

            def target(d):
                if d.dma is not None:
                    return dsem[d.slot], 16 * d.ordn
                return esem[d.eng][d.ordn // SEM_MAX], d.ordn % SEM_MAX + 1

            def run(e, eng):
                waited = {}
                for op in self.ops[e]:
                    for d in op.deps:
                        s, v = target(d)
                        if waited.get(s.name, 0) >= v:
                            continue
                        waited[s.name] = v
                        eng.wait_ge(s, v)
                    if op.barrier:
                        if e == "sp":
                            for sl in range(op.slot):
                                eng.sem_clear(dsem[sl])
                            ins = eng.nop()
                        else:
                            ins = eng.nop()
                        for k in list(waited.keys()):
                            if k.startswith("d_"):
                                del waited[k]
                    else:
                        ins = op.fn(eng)
                    if op.dma is not None:
                        ins.then_inc(dsem[op.slot], 16)
                    elif op.sig:
                        ins.then_inc(esem[e][op.ordn // SEM_MAX], 1)

            @block.tensor
            def _(eng):
                run("pe", eng)

            @block.scalar
            def _(eng):
                run("act", eng)

            @block.vector
            def _(eng):
                run("dve", eng)

            @block.gpsimd
            def _(eng):
                run("pool", eng)

            @block.sync
            def _(eng):
                run("sp", eng)


D = 1024
NQ = 384
NKV = 256
COLQ, COLK, COLV, COLCQ, COLCKV, COLKR, COLG = 0, 1024, 2048, 3072, 3456, 3712, 3744
INC = 5792
DENSE_FF = 2816
EXP_FF = 3584
NEG = -30000.0
DEPTH_TOTAL = 4
ALPHA = (2.0 * DEPTH_TOTAL) ** 0.25
EPS = 1e-5


def t5_thresholds():
    n = np.arange(0, 4096, dtype=np.int32)
    nf = np.maximum(n, 1).astype(np.float32)
    large = 16 + ((np.log(nf / np.float32(16)) / np.float32(math.log(128 / 16))) * np.float32(16)).astype(np.int32)
    large = np.minimum(large, 31)
    bucket = np.where(n < 16, n, large)
    th = []
    for j in range(1, 32):
        th.append(int(np.argmax(bucket >= j)))
    return th


def build(S, L, TG=1024, dbg=(), upto=None):
    nc = bass.Bass("TRN2", target_bir_lowering=False)
    NT = S // 512
    NB = S // 128
    NG = S // TG
    ND = (L + 1) // 2
    NM = L // 2
    es = ExitStack()
    S_ = Sched()
    add = S_.add

    def din(name, shape, dt=F32):
        return nc.dram_tensor(name, list(shape), dt, kind="ExternalInput").ap()

    def dscr(name, shape, dt):
        kind = "ExternalOutput" if name in dbg else "Internal"
        return nc.dram_tensor(name, list(shape), dt, kind=kind).ap()

    x_in = din("x", [S, D])
    p_in = din("p", [L, S, 256])
    pos_in = din("pos", [1, S], I32)
    tab_in = din("tab", [1, 256])
    w_in = din("w_in", [L, D, INC])
    bg_in = din("bg", [L, 128, 16])
    lam_in = din("lam", [L, 1, 256])
    subg_in = din("subg", [L, 128, 1])
    gq_in = din("gq", [L, 128, 3])
    wuq_in = din("wuq", [L, NQ, 768])
    gkv_in = din("gkv", [L, 128, 2])
    wukv_in = din("wukv", [L, NKV, 1024])
    wbd_in = din("wbd", [L, 1024, D])
    wbm_in = din("wbm", [L, 512, D])
    wout_in = din("wout", [L, D, D])
    lnm_in = din("lnm", [L, 2, D])
    dw1_in = din("dw1", [ND, D, DENSE_FF])
    dw3_in = din("dw3", [ND, D, DENSE_FF])
    dw2_in = din("dw2", [ND, DENSE_FF, D])
    rw_in = din("rw", [max(NM, 1), 1, 8 * D])
    ew1_in = din("ew1", [max(NM, 1), 8, D, EXP_FF])
    ew3_in = din("ew3", [max(NM, 1), 8, D, EXP_FF])
    ew2_in = din("ew2", [max(NM, 1), 8, EXP_FF, D])
    wpg_in = din("wpg", [L, D, D])
    wpp_in = din("wpp", [L, 256, D])
    lnf_in = din("lnf", [L, 2, D])
    cst_in = din("cst", [128, 4])
    out = nc.dram_tensor("out", [S, D], F32, kind="ExternalOutput").ap()

    XT = dscr("XT", [8, 128, S], BF16)
    QD = dscr("QD", [8, 128, S], BF16)
    KD = dscr("KD", [8, 128, S], BF16)
    VD = dscr("VD", [S, 1024], BF16)
    GT = dscr("GT", [16, 128, S], BF16)
    QMN = dscr("QMN", [4, 128, S], BF16)
    KMN = dscr("KMN", [4, 128, S], BF16)
    QR = dscr("QR", [2, 128, S], BF16)
    KR = dscr("KR", [2, 16, S], BF16)
    VM = dscr("VM", [S, 512], BF16)
    OAT = dscr("OAT", [8, 128, S], BF16)
    OBT = dscr("OBT", [8, 64, S], BF16)
    X1 = dscr("X1", [S, D], F32)
    X1T = dscr("X1T", [8, 128, S], BF16)
    EE = dscr("EE", [S, D], F32)
    XR = dscr("XR", [S, D], F32)
    CS = dscr("CS", [2, 128, S], F32)
    BST = dscr("BST", [9, 128, 1024], F32)

    sbc = [0]

    def sb(stack, name, shape, dt):
        sbc[0] += 1
        return stack.enter_context(nc.sbuf_tensor("sb_%s_%d" % (name, sbc[0]), list(shape), dt))

    ident = sb(es, "ident", [128, 128], BF16)
    ones_bf = sb(es, "ones_bf", [128, 128], BF16)
    ones_f = sb(es, "ones_f", [128, 128], F32)
    cst = sb(es, "cst", [128, 4], F32)
    ccol = sb(es, "ccol", [128, 8], F32)
    tabb = sb(es, "tabb", [128, 256], F32)
    gates = sb(es, "gates", [128, NB, 8], F32)
    PSB = []
    PSTT = []
    psn = ["ps%d" % i for i in range(8)]
    psc = [0]

    def alloc_psum(ph, nf, nbf):
        psc[0] += 1
        PSB[:] = [ph.enter_context(nc.psum_tensor("psb%d_%d" % (i, psc[0]), [128, 512], F32)) for i in range(nf)]
        PSTT[:] = [ph.enter_context(nc.psum_tensor("pst%d_%d" % (i, psc[0]), [128, 1024], BF16)) for i in range(nbf)]
    ring = [0]

    def nps():
        i = ring[0] % len(PSB)
        ring[0] += 1
        return PSB[i], psn[i]

    def mm(out_ap, pairs, r, w):
        pairs = list(pairs)

        def fn(e):
            ins = None
            n = len(pairs)
            for i, (l, rh) in enumerate(pairs):
                ins = e.matmul(out_ap, lhsT=l, rhs=rh, start=(i == 0), stop=(i == n - 1))
            return ins
        add("pe", fn, r=r, w=w)

    def mm1(out_ap, l, rh, start, stop, r, w):
        add("pe", lambda e: e.matmul(out_ap, lhsT=l, rhs=rh, start=start, stop=stop), r=r, w=w)

    def dma(eng, out_ap, in_ap, r, w, key):
        add(eng, lambda e: e.dma_start(out=out_ap, in_=in_ap), r=r, w=w, dma=key)

    def act(out_ap, in_ap, func, r, w, bias=None, scale=None):
        kw = {}
        if bias is not None:
            kw["bias"] = bias
        if scale is not None:
            kw["scale"] = scale
        add("act", lambda e: e.activation(out=out_ap, in_=in_ap, func=func, **kw), r=r, w=w)

    def tt(eng, out_ap, a, b, op, r, w):
        add(eng, lambda e: e.tensor_tensor(out=out_ap, in0=a, in1=b, op=op), r=r, w=w)

    def ts(eng, out_ap, a, s1, s2, op0, op1, r, w):
        if op1 is None:
            add(eng, lambda e: e.tensor_scalar(out=out_ap, in0=a, scalar1=s1, scalar2=None, op0=op0), r=r, w=w)
        else:
            add(eng, lambda e: e.tensor_scalar(out=out_ap, in0=a, scalar1=s1, scalar2=s2, op0=op0, op1=op1), r=r, w=w)

    def stt(eng, out_ap, a, sc, b, op0, op1, r, w):
        add(eng, lambda e: e.scalar_tensor_tensor(out=out_ap, in0=a, scalar=sc, in1=b, op0=op0, op1=op1), r=r, w=w)

    def cp(eng, out_ap, in_ap, r, w):
        if eng == "act":
            add("act", lambda e: e.copy(out=out_ap, in_=in_ap), r=r, w=w)
        else:
            add(eng, lambda e: e.tensor_copy(out=out_ap, in_=in_ap), r=r, w=w)

    STGW = 2048
    stg_state = {"tiles": None, "n": 0}
    cast_rot = ("pool", "act", "pool", "dve")

    def wload(dst_ap, src_ap, width, wname, parts=128):
        tiles = stg_state["tiles"]
        k = stg_state["n"]
        stg_state["n"] += 1
        i = k % len(tiles)
        st_ = tiles[i]
        dma("sp", st_[0:parts, 0:width], src_ap, r=[], w=["stg%d" % i], key="stg%d" % i)
        cp(cast_rot[k % 4], dst_ap, st_[0:parts, 0:width], r=["stg%d" % i], w=[wname])

    def rstd_from(out_ap, in_ap, scale, r, w):
        act(out_ap, in_ap, AF.Ln, r=r + ["ccol"], w=w, bias=ccol[:, 0:1], scale=scale)
        act(out_ap, out_ap, AF.Exp, r=w, w=w, scale=-0.5)

    add("pool", lambda e: e.memset(ident[:], 0.0), w=["ident"])
    add("pool", lambda e: e.affine_select(out=ident[:], in_=ident[:], compare_op=ALU.not_equal, fill=1.0,
                                          base=0, pattern=[[-1, 128]], channel_multiplier=1),
        r=["ident"], w=["ident"])
    add("dve", lambda e: e.memset(ones_bf[:], 1.0), w=["ones_bf"])
    add("dve", lambda e: e.memset(ones_f[:], 1.0), w=["ones_f"])
    add("dve", lambda e: e.memset(ccol[:, 0:1], EPS), w=["ccol"])
    add("dve", lambda e: e.memset(ccol[:, 1:2], -math.pi), r=["ccol"], w=["ccol"])
    add("dve", lambda e: e.memset(ccol[:, 2:3], 1.0), r=["ccol"], w=["ccol"])
    add("dve", lambda e: e.memset(ccol[:, 3:4], 0.0), r=["ccol"], w=["ccol"])
    dma("sp", cst[:], cst_in, r=[], w=["cst"], key="cst")
    dma("sp", tabb[:], tab_in.partition_broadcast(128), r=[], w=["tabb"], key="tabb")

    with ExitStack() as ph:
        posi = sb(ph, "posi", [128, S], I32)
        posf = sb(ph, "posf", [128, S], F32)
        ang = sb(ph, "ang", [128, S], F32)
        tmpc = sb(ph, "tmpc", [128, S], F32)
        dma("sp", posi[:], pos_in.partition_broadcast(128), r=[], w=["posi"], key="posi")
        cp("dve", posf[:], posi[:], r=["posi"], w=["posf"])
        ts("dve", ang[:], posf[:], cst[:, 0:1], None, ALU.mult, None, r=["posf", "cst"], w=["ang"])
        C1 = 6.28125
        C2 = 2.0 * math.pi - C1
        PI_IN = 3.1415925
        indc = sb(ph, "indc", [128, S], F32)
        ts("dve", tmpc[:], ang[:], 1.0 / (2.0 * math.pi), None, ALU.mult, None, r=["ang"], w=["tmpc"])
        cp("dve", posi[:], tmpc[:], r=["tmpc", "posf"], w=["posi"])
        cp("dve", posf[:], posi[:], r=["posi", "ang"], w=["posf"])
        stt("dve", tmpc[:], posf[:], -C1, ang[:], ALU.mult, ALU.add, r=["posf", "ang"], w=["tmpc"])
        stt("dve", tmpc[:], posf[:], -C2, tmpc[:], ALU.mult, ALU.add, r=["posf", "tmpc"], w=["tmpc"])

        def wrap(tn, t):
            ts("dve", indc[:], t[:], math.pi, None, ALU.is_gt, None, r=[tn], w=["indc"])
            stt("dve", t[:], indc[:], -2.0 * math.pi, t[:], ALU.mult, ALU.add, r=["indc", tn], w=[tn])
            ts("dve", indc[:], t[:], -math.pi, None, ALU.is_lt, None, r=[tn], w=["indc"])
            stt("dve", t[:], indc[:], 2.0 * math.pi, t[:], ALU.mult, ALU.add, r=["indc", tn], w=[tn])
            ts("dve", t[:], t[:], PI_IN, -PI_IN, ALU.min, ALU.max, r=[tn], w=[tn])
        wrap("tmpc", tmpc)
        ts("dve", ang[:], tmpc[:], 0.5 * math.pi, None, ALU.add, None, r=["tmpc"], w=["ang"])
        wrap("ang", ang)
        act(ang[:], ang[:], AF.Sin, r=["ang"], w=["ang"])
        dma("sp", CS[0], ang[:], r=["ang"], w=[], key="ang")
        act(tmpc[:], tmpc[:], AF.Sin, r=["tmpc"], w=["tmpc"])
        dma("sp", CS[1], tmpc[:], r=["tmpc"], w=[], key="tmpc")
    S_.barrier()
    with ExitStack() as ph:
        nmi = sb(ph, "nmi", [128, 1024], I32)
        nmat = sb(ph, "nmat", [128, 1024], F32)
        ind = sb(ph, "ind", [128, 1024], F32)
        mstrip = sb(ph, "mstrip", [128, 1024], F32)
        dtab = sb(ph, "dtab", [128, 256], F32)
        bst = [sb(ph, "bst%d" % h, [128, 1024], F32) for h in range(8)]
        add("pool", lambda e: e.iota(nmi[:], pattern=[[1, 1024]], base=-384, channel_multiplier=-1), w=["nmi"])
        cp("dve", nmat[:], nmi[:], r=["nmi"], w=["nmat"])
        tt("dve", dtab[:, 8:256], tabb[:, 8:256], tabb[:, 0:248], ALU.subtract, r=["tabb"], w=["dtab"])
        ts("dve", ind[:], nmat[:], 0.0, None, ALU.is_ge, None, r=["nmat"], w=["ind"])
        ts("dve", mstrip[:], ind[:], 1.0, -NEG, ALU.subtract, ALU.mult, r=["ind"], w=["mstrip"])
        for h in range(8):
            ts("pool", bst[h][:], mstrip[:], tabb[:, h:h + 1], None, ALU.add, None, r=["mstrip", "tabb"], w=["bst%d" % h])
        th = t5_thresholds()
        for j in range(1, 32):
            ts("dve", ind[:], nmat[:], float(th[j - 1]), None, ALU.is_ge, None, r=["nmat"], w=["ind"])
            for h in range(8):
                eng = "dve"
                stt(eng, bst[h][:], ind[:], dtab[:, j * 8 + h:j * 8 + h + 1], bst[h][:], ALU.mult, ALU.add,
                    r=["ind", "dtab", "bst%d" % h], w=["bst%d" % h])
        for h in range(8):
            dma("sp", BST[h], bst[h][:], r=["bst%d" % h], w=[], key="bst%d" % h)
        dma("sp", BST[8], mstrip[:], r=["mstrip"], w=[], key="mstrip")
    S_.barrier()
    if upto == 'C':
        S_.emit(nc)
        es.close()
        return nc

    for li in range(L):
        lam_init = 0.8 - 0.6 * math.exp(-0.3 * li)
        x_src = x_in if li == 0 else XR
        x_dst = out if li == L - 1 else XR

        with ExitStack() as ph:
            alloc_psum(ph, 6, 2)
            w1 = sb(ph, "p1w", [128, 8, 3072], BF16)
            xs = [sb(ph, "p1xs%d" % i, [128, 4, 1024], F32) for i in range(2)]
            xb = sb(ph, "p1xb", [128, 4, 1024], BF16)
            xT = [sb(ph, "p1xT%d" % i, [128, 8, 512], BF16) for i in range(2)]
            qd = [sb(ph, "p1qd%d" % i, [128, 8, 512], BF16) for i in range(2)]
            kd = [sb(ph, "p1kd%d" % i, [128, 8, 512], BF16) for i in range(2)]
            vd = [sb(ph, "p1vd%d" % i, [128, 4, 1024], BF16) for i in range(2)]
            stg_state["tiles"] = [sb(ph, "p1stg%d" % i, [128, STGW], F32) for i in range(3)]
            for kb in range(8):
                for c3 in range(0, 3072, STGW):
                    wd = min(STGW, 3072 - c3)
                    wload(w1[:, kb, c3:c3 + wd], w_in[li, kb * 128:(kb + 1) * 128, c3:c3 + wd], wd, "p1w%d_%d" % (kb, c3))
            wn = ["p1w%d_%d" % (kb, c3) for kb in range(8) for c3 in range(0, 3072, STGW)]

            def ldx(t):
                i = t % 2
                dma("sp", xs[i][:], x_src[t * 512:(t + 1) * 512, :].rearrange("(j p) d -> p j d", p=128),
                    r=[], w=["p1xs%d" % i], key="p1xs%d" % i)
            ldx(0)
            for t in range(NT):
                i = t % 2
                if t + 1 < NT:
                    ldx(t + 1)
                import os
                cut = int(os.environ.get("P1CUT", "99"))
                if cut < 1:
                    continue
                for j in range(4):
                    cp("dve" if j % 2 == 0 else "pool", xb[:, j, :], xs[i][:, j, :], r=["p1xs%d" % i], w=["p1xb%d" % j])
                if cut < 2:
                    continue
                for kb in range(8):
                    def tr(e, kb=kb, PSTT=tuple(PSTT)):
                        ins = None
                        for j in range(4):
                            ins = e.transpose(out=PSTT[kb % 2][:, j * 128:(j + 1) * 128],
                                              in_=xb[:, j, kb * 128:(kb + 1) * 128], identity=ident[:])
                        return ins
                    sub_ = os.environ.get("P1SUB", "")
                    if sub_ == "one" and kb > 0:
                        continue
                    add("pe", tr, r=["p1xb%d" % j for j in range(4)] + ["ident"], w=["pst%d" % (kb % 2)])
                    if sub_ == "tr":
                        continue
                    cp("act" if kb % 2 else "dve", xT[i][:, kb, :], PSTT[kb % 2][:, 0:512],
                       r=["pst%d" % (kb % 2)], w=["p1xT%d_%d" % (i, kb)])
                xTn = ["p1xT%d_%d" % (i, kb) for kb in range(8)]
                if cut < 3:
                    continue
                dma("sp", XT[:, :, t * 512:(t + 1) * 512].rearrange("k p s -> p k s"), xT[i][:], r=xTn, w=[], key="p1xT%d" % i)
                if cut < 4:
                    continue
                for (col, dst, dstn, DR, sc) in ((COLQ, qd, "p1qd", QD, 0.125), (COLK, kd, "p1kd", KD, 1.0)):
                    for h in range(8):
                        pt, pn = nps()
                        mm(pt[:], [(w1[:, kb, col + h * 128:col + (h + 1) * 128], xT[i][:, kb, :]) for kb in range(8)],
                           r=wn + xTn, w=[pn])
                        if h % 2 == 0:
                            act(dst[i][:, h, :], pt[:], AF.Copy, r=[pn], w=["%s%d_%d" % (dstn, i, h)], scale=sc)
                        else:
                            ts("dve", dst[i][:, h, :], pt[:], sc, None, ALU.mult, None, r=[pn], w=["%s%d_%d" % (dstn, i, h)])
                    dma("sp", DR[:, :, t * 512:(t + 1) * 512].rearrange("h p s -> p h s"), dst[i][:],
                        r=["%s%d_%d" % (dstn, i, h) for h in range(8)], w=[], key="%s%d" % (dstn, i))
                if cut < 5:
                    continue
                for j in range(4):
                    for hf in range(2):
                        pt, pn = nps()
                        mm(pt[:], [(xT[i][:, kb, j * 128:(j + 1) * 128], w1[:, kb, COLV + hf * 512:COLV + (hf + 1) * 512]) for kb in range(8)],
                           r=wn + xTn, w=[pn])
                        cp("act" if hf else "dve", vd[i][:, j, hf * 512:(hf + 1) * 512], pt[:], r=[pn], w=["p1vd%d_%d_%d" % (i, j, hf)])
                dma("sp", VD[t * 512:(t + 1) * 512, :].rearrange("(j p) e -> p j e", p=128), vd[i][:],
                    r=["p1vd%d_%d_%d" % (i, j, hf) for j in range(4) for hf in range(2)], w=[], key="p1vd%d" % i)
        S_.barrier()
        if upto == 'P1':
            S_.emit(nc)
            es.close()
            return nc

        with ExitStack() as ph:
            alloc_psum(ph, 8, 0)
            w2 = sb(ph, "p2w", [128, 8, INC - 3072], BF16)
            W2O = 3072
            wq_st = sb(ph, "p2wqs", [128, 3, 768], F32)
            wkv_st = sb(ph, "p2wkvs", [128, 2, 1024], F32)
            wq = sb(ph, "p2wq", [128, 3, 768], BF16)
            wkv = sb(ph, "p2wkv", [128, 2, 1024], BF16)
            gq = sb(ph, "p2gq", [128, 3], F32)
            gkv = sb(ph, "p2gkv", [128, 2], F32)
            bg = sb(ph, "p2bg", [128, 16], F32)
            xT = [sb(ph, "p2xT%d" % i, [128, 8, 512], BF16) for i in range(2)]
            cs = [sb(ph, "p2cs%d" % i, [128, 2, 512], F32) for i in range(2)]
            gsb = sb(ph, "p2g", [128, 16, 512], BF16)
            cqb = sb(ph, "p2cqb", [128, 3, 512], BF16)
            sqq = sb(ph, "p2sqq", [128, 3, 512], BF16)
            ckb = sb(ph, "p2ckb", [128, 2, 512], BF16)
            sqk = sb(ph, "p2sqk", [128, 2, 512], BF16)
            rq = sb(ph, "p2rq", [128, 512], F32)
            rk = sb(ph, "p2rk", [128, 512], F32)
            rkc = sb(ph, "p2rkc", [128, 4], F32)
            qn = sb(ph, "p2qn", [128, 4, 512], BF16)
            kn = sb(ph, "p2kn", [128, 4, 512], BF16)
            vm = sb(ph, "p2vm", [128, 4, 512], BF16)
            x1s = sb(ph, "p2x1s", [128, 512], F32)
            x2s = sb(ph, "p2x2s", [128, 512], F32)
            ta = sb(ph, "p2ta", [128, 512], F32)
            tb = sb(ph, "p2tb", [128, 512], F32)
            qr = sb(ph, "p2qr", [128, 2, 512], BF16)
            kr = sb(ph, "p2kr", [16, 2, 512], BF16)
            stg_state["tiles"] = [sb(ph, "p2stg%d" % i, [128, STGW], F32) for i in range(2)]
            W2W = INC - 3072
            for kb in range(8):
                for c3 in range(0, W2W, STGW):
                    wd = min(STGW, W2W - c3)
                    wload(w2[:, kb, c3:c3 + wd], w_in[li, kb * 128:(kb + 1) * 128, 3072 + c3:3072 + c3 + wd], wd, "p2w%d_%d" % (kb, c3))
            wn = ["p2w%d_%d" % (kb, c3) for kb in range(8) for c3 in range(0, W2W, STGW)]
            dma("sp", wq_st[:], wuq_in[li].rearrange("(k p) c -> p k c", p=128), r=[], w=["p2wqs"], key="p2wqs")
            dma("sp", wkv_st[:], wukv_in[li].rearrange("(k p) c -> p k c", p=128), r=[], w=["p2wkvs"], key="p2wkvs")
            dma("sp", gq[:], gq_in[li], r=[], w=["p2gq"], key="p2gq")
            dma("sp", gkv[:], gkv_in[li], r=[], w=["p2gkv"], key="p2gkv")
            dma("sp", bg[:], bg_in[li], r=[], w=["p2bg"], key="p2bg")
            for k in range(3):
                ts("dve", wq[:, k, :], wq_st[:, k, :], gq[:, k:k + 1], None, ALU.mult, None, r=["p2wqs", "p2gq"], w=["p2wq"])
            for k in range(2):
                ts("dve", wkv[:, k, :], wkv_st[:, k, :], gkv[:, k:k + 1], None, ALU.mult, None, r=["p2wkvs", "p2gkv"], w=["p2wkv"])

            def ldt(t):
                i = t % 2
                dma("sp", xT[i][:], XT[:, :, t * 512:(t + 1) * 512].rearrange("k p s -> p k s"), r=[], w=["p2xT%d" % i], key="p2xT%d" % i)
                dma("sp", cs[i][:], CS[:, :, t * 512:(t + 1) * 512].rearrange("c p s -> p c s"), r=[], w=["p2cs%d" % i], key="p2cs%d" % i)
            ldt(0)
            MSC = 96.0 ** -0.5
            for t in range(NT):
                i = t % 2
                if t + 1 < NT:
                    ldt(t + 1)
                xn = ["p2xT%d" % i]
                tsl = slice(t * 512, (t + 1) * 512)
                for gb in range(16):
                    pt, pn = nps()
                    c0 = COLG - W2O + gb * 128
                    mm(pt[:], [(w2[:, kb, c0:c0 + 128], xT[i][:, kb, :]) for kb in range(8)], r=wn + xn, w=[pn])
                    act(gsb[:, gb, :], pt[:], AF.Sigmoid, r=[pn, "p2bg"], w=["p2g%d" % gb], bias=bg[:, gb:gb + 1], scale=1.0)
                dma("sp", GT[:, :, tsl].rearrange("g p s -> p g s"), gsb[:], r=["p2g%d" % gb for gb in range(16)], w=[], key="p2g")
                for b in range(3):
                    pt, pn = nps()
                    c0 = COLCQ - W2O + b * 128
                    mm(pt[:], [(w2[:, kb, c0:c0 + 128], xT[i][:, kb, :]) for kb in range(8)], r=wn + xn, w=[pn])
                    cp("dve", cqb[:, b, :], pt[:], r=[pn], w=["p2cqb%d" % b])
                    act(sqq[:, b, :], pt[:], AF.Square, r=[pn], w=["p2sqq%d" % b])
                pt, pn = nps()
                mm(pt[:], [(ones_bf[:], sqq[:, b, :]) for b in range(3)], r=["ones_bf"] + ["p2sqq%d" % b for b in range(3)], w=[pn])
                rstd_from(rq[:], pt[:], 1.0 / NQ, r=[pn], w=["p2rq"])
                ts("dve", rq[:], rq[:], MSC, None, ALU.mult, None, r=["p2rq"], w=["p2rq"])
                cqn = ["p2cqb%d" % b for b in range(3)]
                for pr in range(4):
                    pt, pn = nps()
                    mm(pt[:], [(wq[:, k, pr * 128:(pr + 1) * 128], cqb[:, k, :]) for k in range(3)], r=["p2wq"] + cqn, w=[pn])
                    tt("dve", qn[:, pr, :], pt[:], rq[:], ALU.mult, r=[pn, "p2rq"], w=["p2qn%d" % pr])
                dma("sp", QMN[:, :, tsl].rearrange("j p s -> p j s"), qn[:], r=["p2qn%d" % pr for pr in range(4)], w=[], key="p2qn")
                pt1, pn1 = nps()
                mm(pt1[:], [(wq[:, k, 512:640], cqb[:, k, :]) for k in range(3)], r=["p2wq"] + cqn, w=[pn1])
                pt2, pn2 = nps()
                mm(pt2[:], [(wq[:, k, 640:768], cqb[:, k, :]) for k in range(3)], r=["p2wq"] + cqn, w=[pn2])
                tt("dve", x1s[:], pt1[:], rq[:], ALU.mult, r=[pn1, "p2rq"], w=["p2x1s"])
                tt("dve", x2s[:], pt2[:], rq[:], ALU.mult, r=[pn2, "p2rq"], w=["p2x2s"])
                csn = "p2cs%d" % i
                tt("dve", ta[:], x1s[:], cs[i][:, 0, :], ALU.mult, r=["p2x1s", csn], w=["p2ta"])
                tt("pool", tb[:], x2s[:], cs[i][:, 1, :], ALU.mult, r=["p2x2s", csn], w=["p2tb"])
                tt("dve", qr[:, 0, :], ta[:], tb[:], ALU.subtract, r=["p2ta", "p2tb"], w=["p2qr0"])
                tt("dve", ta[:], x1s[:], cs[i][:, 1, :], ALU.mult, r=["p2x1s", csn], w=["p2ta"])
                tt("pool", tb[:], x2s[:], cs[i][:, 0, :], ALU.mult, r=["p2x2s", csn], w=["p2tb"])
                tt("dve", qr[:, 1, :], ta[:], tb[:], ALU.add, r=["p2ta", "p2tb"], w=["p2qr1"])
                dma("sp", QR[:, :, tsl].rearrange("c p s -> p c s"), qr[:], r=["p2qr0", "p2qr1"], w=[], key="p2qr")
                for b in range(2):
                    pt, pn = nps()
                    c0 = COLCKV - W2O + b * 128
                    mm(pt[:], [(w2[:, kb, c0:c0 + 128], xT[i][:, kb, :]) for kb in range(8)], r=wn + xn, w=[pn])
                    cp("dve", ckb[:, b, :], pt[:], r=[pn], w=["p2ckb%d" % b])
                    act(sqk[:, b, :], pt[:], AF.Square, r=[pn], w=["p2sqk%d" % b])
                sqn = ["p2sqk%d" % b for b in range(2)]
                pt, pn = nps()
                mm(pt[:], [(ones_bf[:], sqk[:, b, :]) for b in range(2)], r=["ones_bf"] + sqn, w=[pn])
                rstd_from(rk[:], pt[:], 1.0 / NKV, r=[pn], w=["p2rk"])
                ptc, pnc = nps()
                for j in range(4):
                    mm(ptc[:, j:j + 1], [(sqk[:, b, j * 128:(j + 1) * 128], ones_bf[:, 0:1]) for b in range(2)], r=["ones_bf"] + sqn, w=[pnc])
                rstd_from(rkc[:], ptc[:, 0:4], 1.0 / NKV, r=[pnc], w=["p2rkc"])
                ckn = ["p2ckb%d" % b for b in range(2)]
                for pr in range(4):
                    pt, pn = nps()
                    mm(pt[:], [(wkv[:, k, pr * 128:(pr + 1) * 128], ckb[:, k, :]) for k in range(2)], r=["p2wkv"] + ckn, w=[pn])
                    tt("dve", kn[:, pr, :], pt[:], rk[:], ALU.mult, r=[pn, "p2rk"], w=["p2kn%d" % pr])
                dma("sp", KMN[:, :, tsl].rearrange("j p s -> p j s"), kn[:], r=["p2kn%d" % pr for pr in range(4)], w=[], key="p2kn")
                for j in range(4):
                    pt, pn = nps()
                    mm(pt[:], [(ckb[:, k, j * 128:(j + 1) * 128], wkv[:, k, 512:1024]) for k in range(2)], r=["p2wkv"] + ckn, w=[pn])
                    act(vm[:, j, :], pt[:], AF.Copy, r=[pn, "p2rkc"], w=["p2vm%d" % j], scale=rkc[:, j:j + 1])
                dma("sp", VM[tsl, :].rearrange("(j p) e -> p j e", p=128), vm[:], r=["p2vm%d" % j for j in range(4)], w=[], key="p2vm")
                pt1, pn1 = nps()
                c0 = COLKR - W2O
                mm(pt1[0:16, :], [(w2[:, kb, c0:c0 + 16], xT[i][:, kb, :]) for kb in range(8)], r=wn + xn, w=[pn1])
                pt2, pn2 = nps()
                mm(pt2[0:16, :], [(w2[:, kb, c0 + 16:c0 + 32], xT[i][:, kb, :]) for kb in range(8)], r=wn + xn, w=[pn2])
                tt("dve", ta[0:16, :], pt1[0:16, :], cs[i][0:16, 0, :], ALU.mult, r=[pn1, csn], w=["p2ta"])
                tt("dve", tb[0:16, :], pt2[0:16, :], cs[i][0:16, 1, :], ALU.mult, r=[pn2, csn], w=["p2tb"])
                tt("dve", kr[:, 0, :], ta[0:16, :], tb[0:16, :], ALU.subtract, r=["p2ta", "p2tb"], w=["p2kr0"])
                tt("dve", ta[0:16, :], pt1[0:16, :], cs[i][0:16, 1, :], ALU.mult, r=[pn1, csn], w=["p2ta"])
                tt("dve", tb[0:16, :], pt2[0:16, :], cs[i][0:16, 0, :], ALU.mult, r=[pn2, csn], w=["p2tb"])
                tt("dve", kr[:, 1, :], ta[0:16, :], tb[0:16, :], ALU.add, r=["p2ta", "p2tb"], w=["p2kr1"])
                dma("sp", KR[:, :, tsl].rearrange("c p s -> p c s"), kr[:], r=["p2kr0", "p2kr1"], w=[], key="p2kr")
        S_.barrier()
        if upto == 'P2':
            S_.emit(nc)
            es.close()
            return nc

        with ExitStack() as ph:
            alloc_psum(ph, 8, 0)
            bstr = sb(ph, "abst", [128, 9, 1024], F32)
            lamt = sb(ph, "alam", [128, 256], F32)
            lamp = sb(ph, "alamp", [128, 128], F32)
            lamc = sb(ph, "alamc", [128, 4], F32)
            subg = sb(ph, "asubg", [128, 1], F32)
            kdt = [sb(ph, "akd%d" % i, [128, S], BF16) for i in range(2)]
            vdt = [sb(ph, "avd%d" % i, [128, NB, 128], BF16) for i in range(2)]
            vmt = [sb(ph, "avm%d" % i, [128, NB, 65], BF16) for i in range(2)]
            qt = [sb(ph, "aq%d" % i, [128, 512], BF16) for i in range(2)]
            NPT = 6
            pT = [sb(ph, "apT%d" % i, [128, 512], BF16) for i in range(NPT)]
            sbias = [sb(ph, "asb%d" % i, [128, 512], F32) for i in range(4)]
            accS = [sb(ph, "aacc%d" % i, [128, 512], F32) for i in range(4)]
            accB = [sb(ph, "aaccb%d" % i, [128, 512], BF16) for i in range(2)]
            sbc2 = [0]
            rr = sb(ph, "arr", [128, 512], F32)
            rr2 = sb(ph, "arr2", [1, 512], F32)
            Rb = [sb(ph, "aRb%d" % i, [128, 512], F32) for i in range(2)]
            t0 = sb(ph, "at0", [128, 512], F32)
            t1 = sb(ph, "at1", [128, 512], F32)
            osq = sb(ph, "aosq", [128, 512], BF16)
            rms = sb(ph, "arms", [128, 512], F32)
            oo = [sb(ph, "aoo%d" % i, [128, 512], BF16) for i in range(2)]
            dma("sp", bstr[:], BST.rearrange("h p n -> p h n"), r=[], w=["abst"], key="abst")
            dma("sp", lamt[:], lam_in[li].partition_broadcast(128), r=[], w=["alam"], key="alam")
            dma("sp", subg[:], subg_in[li], r=[], w=["asubg"], key="asubg")
            tt("dve", lamp[:, 0:64], lamt[:, 0:64], lamt[:, 64:128], ALU.mult, r=["alam"], w=["alamp"])
            tt("dve", lamp[:, 64:128], lamt[:, 128:192], lamt[:, 192:256], ALU.mult, r=["alam", "alamp"], w=["alamp"])
            add("dve", lambda e: e.reduce_sum(out=lamc[:, 0:1], in_=lamp[:, 0:64], axis=AX.X), r=["alamp"], w=["alamc"])
            add("dve", lambda e: e.reduce_sum(out=lamc[:, 1:2], in_=lamp[:, 64:128], axis=AX.X), r=["alamp", "alamc"], w=["alamc"])
            act(lamc[:, 0:2], lamc[:, 0:2], AF.Exp, r=["alamc"], w=["alamc"])
            tt("dve", lamc[:, 2:3], lamc[:, 1:2], lamc[:, 0:1], ALU.subtract, r=["alamc"], w=["alamc"])
            ts("dve", lamc[:, 2:3], lamc[:, 2:3], -lam_init, None, ALU.add, None, r=["alamc"], w=["alamc"])
            for i in range(2):
                add("pool", lambda e, i=i: e.memset(vmt[i][:, :, 64:65], 1.0), w=["avm1_%d" % i])
            PS_S = [(PSB[i], psn[i]) for i in range(4)]
            NPS = 4
            POS = [[(PSB[4], psn[4]), (PSB[5], psn[5])], [(PSB[6], psn[6]), (PSB[7], psn[7])]]
            sctr = [0]
            pctr = [0]
            pending = []
            tctr = [0]

            def make_post(isd, h, t, PO, am, oi):
                tsl = slice(t * 512, (t + 1) * 512)
                segs = []
                if isd:
                    def seg1():
                        for m in range(2):
                            cp("dve", accB[m][:], accS[am + m][:], r=["aacc%d" % (am + m)], w=["aaccb%d" % m])
                            pts, pns = PS_S[sctr[0] % NPS]
                            sctr[0] += 1
                            mm1(pts[0:1, :], ones_bf[:, 0:1], accB[m][:], True, True, r=["ones_bf", "aaccb%d" % m], w=[pns])
                            rrm = rr if m == 0 else rr2
                            act(rrm[0:1, :], pts[0:1, :], AF.Ln, r=[pns], w=["arr%d" % m])
                            act(rrm[0:1, :], rrm[0:1, :], AF.Exp, r=["arr%d" % m], w=["arr%d" % m], scale=-1.0)

                    def seg2():
                        for m in range(2):
                            rrm = rr if m == 0 else rr2
                            pt, pn = PS_S[sctr[0] % NPS]
                            sctr[0] += 1
                            mm1(pt[:], ones_f[0:1, :], rrm[0:1, :], True, True, r=["ones_f", "arr%d" % m], w=[pn])
                            cp("act", Rb[m][:], pt[:], r=[pn], w=["aRb%d" % m])

                    def seg3():
                        tt("dve", t0[:], PO[0][0][:], Rb[0][:], ALU.mult, r=[PO[0][1], "aRb0"], w=["at0"])
                        tt("dve", t1[:], PO[1][0][:], Rb[1][:], ALU.mult, r=[PO[1][1], "aRb1"], w=["at1"])
                        stt("dve", t0[:], t1[:], lamc[:, 2:3], t0[:], ALU.mult, ALU.add, r=["at0", "at1", "alamc"], w=["at0"])
                        act(osq[:], t0[:], AF.Square, r=["at0"], w=["aosq"])

                    def seg4():
                        pt, pn = PS_S[sctr[0] % NPS]
                        sctr[0] += 1
                        mm1(pt[:], ones_bf[:], osq[:], True, True, r=["ones_bf", "aosq"], w=[pn])
                        rstd_from(rms[:], pt[:], 1.0 / 128.0, r=[pn], w=["arms"])
                        stt("dve", t1[:], t0[:], subg[:, 0:1], rms[:], ALU.mult, ALU.mult, r=["at0", "asubg", "arms"], w=["at1"])
                        ts("dve", oo[oi][:], t1[:], 1.0 - lam_init, None, ALU.mult, None, r=["at1"], w=["aoo%d" % oi])
                        dma("sp", OAT[h, :, tsl], oo[oi][:], r=["aoo%d" % oi], w=[], key="aoo%d" % oi)
                    segs = [seg1, seg2, seg3, seg4]
                else:
                    def seg1():
                        act(rr[64:65, :], PO[0][0][64:65, :], AF.Ln, r=[PO[0][1]], w=["arr0"])
                        act(rr[64:65, :], rr[64:65, :], AF.Exp, r=["arr0"], w=["arr0"], scale=-1.0)
                        pt, pn = PS_S[sctr[0] % NPS]
                        sctr[0] += 1
                        mm1(pt[0:64, :], ones_f[64:65, 0:64], rr[64:65, :], True, True, r=["ones_f", "arr0"], w=[pn])
                        cp("act", Rb[0][0:64, :], pt[0:64, :], r=[pn], w=["aRb0"])

                    def seg2():
                        tt("dve", oo[oi][0:64, :], PO[0][0][0:64, :], Rb[0][0:64, :], ALU.mult, r=[PO[0][1], "aRb0"], w=["aoo%d" % oi])
                        dma("sp", OBT[h, :, tsl], oo[oi][0:64, :], r=["aoo%d" % oi], w=[], key="aoo%d" % oi)
                    segs = [seg1, seg2]
                return segs

            for hh in range(16):
                isd = hh < 8
                h = hh % 8
                i = hh % 2
                if isd:
                    dma("sp", kdt[i][:], KD[h], r=[], w=["akd%d" % i], key="akd%d" % i)
                    dma("sp", vdt[i][:], VD[:, h * 128:(h + 1) * 128].rearrange("(b p) e -> p b e", p=128), r=[], w=["avd%d" % i], key="avd%d" % i)
                else:
                    pr, hp = h // 2, h % 2
                    dma("sp", kdt[i][0:64, :], KMN[pr, hp * 64:(hp + 1) * 64, :], r=[], w=["akd%d" % i], key="akd%d" % i)
                    dma("sp", kdt[i][64:80, :], KR[0], r=[], w=["akdr1_%d" % i], key="akdr1_%d" % i)
                    dma("sp", kdt[i][80:96, :], KR[1], r=[], w=["akdr2_%d" % i], key="akdr2_%d" % i)
                    dma("sp", vmt[i][:, :, 0:64], VM[:, h * 64:(h + 1) * 64].rearrange("(b p) e -> p b e", p=128), r=[], w=["avm%d" % i], key="avm%d" % i)
                kn_ = ["akd%d" % i] + ([] if isd else ["akdr1_%d" % i, "akdr2_%d" % i])
                vn_ = ["avd%d" % i] if isd else ["avm%d" % i, "avm1_%d" % i]
                for t in range(NT):
                    tc = tctr[0]
                    tctr[0] += 1
                    qi = tc % 2
                    PO = POS[tc % 2]
                    am = 2 * (tc % 2)
                    tsl = slice(t * 512, (t + 1) * 512)
                    if isd:
                        dma("sp", qt[qi][:], QD[h, :, tsl], r=[], w=["aq%d" % qi], key="aq%d" % qi)
                        qn_ = ["aq%d" % qi]
                    else:
                        pr, hp = h // 2, h % 2
                        dma("sp", qt[qi][0:64, :], QMN[pr, hp * 64:(hp + 1) * 64, tsl], r=[], w=["aq%d" % qi], key="aq%d" % qi)
                        dma("sp", qt[qi][64:80, :], QR[0, h * 16:(h + 1) * 16, tsl], r=[], w=["aqr1_%d" % qi], key="aqr1_%d" % qi)
                        dma("sp", qt[qi][80:96, :], QR[1, h * 16:(h + 1) * 16, tsl], r=[], w=["aqr2_%d" % qi], key="aqr2_%d" % qi)
                        qn_ = ["aq%d" % qi, "aqr1_%d" % qi, "aqr2_%d" % qi]
                    nkb = 4 * t + 4
                    maps = (0, 1) if isd else (0,)

                    def stage1(kb):
                        delta = 512 * t - 128 * kb
                        c0 = max(0, -delta)
                        near = delta < 256
                        ksl = slice(kb * 128, (kb + 1) * 128)
                        res_ = []
                        for m in maps:
                            pt, pn = PS_S[sctr[0] % NPS]
                            sctr[0] += 1
                            pb = pT[pctr[0] % NPT]
                            pbn = "apT%d" % (pctr[0] % NPT)
                            pctr[0] += 1
                            rows = slice(m * 64, (m + 1) * 64) if isd else slice(0, 96)
                            mm1(pt[:, c0:512], kdt[i][rows, ksl], qt[qi][rows, c0:512], True, True, r=kn_ + qn_, w=[pn])
                            if (isd and near) or ((not isd) and delta <= 0):
                                si = sbc2[0] % 4
                                sbc2[0] += 1
                                sbt = sbias[si]
                                bi = h if isd else 8
                                tt("dve", sbt[:, c0:512], pt[:, c0:512], bstr[:, bi, delta + 384 + c0:delta + 384 + 512], ALU.add,
                                   r=[pn, "abst"], w=["asb%d" % si])
                                act(pb[:, c0:512], sbt[:, c0:512], AF.Exp, r=["asb%d" % si], w=[pbn])
                            elif isd:
                                act(pb[:, c0:512], pt[:, c0:512], AF.Exp, r=[pn, "tabb"], w=[pbn], bias=tabb[:, 248 + h:249 + h], scale=1.0)
                            else:
                                act(pb[:, c0:512], pt[:, c0:512], AF.Exp, r=[pn], w=[pbn])
                            res_.append((pb, pbn, c0))
                        return res_

                    def stage2(kb, res_):
                        first, last = kb == 0, kb == nkb - 1
                        for m, (pb, pbn, c0) in zip(maps, res_):
                            if isd:
                                mm1(PO[m][0][:, c0:512], vdt[i][:, kb, :], pb[:, c0:512], first, last, r=vn_ + [pbn], w=[PO[m][1]])
                                an = "aacc%d" % (am + m)
                                if first:
                                    cp("dve", accS[am + m][:], pb[:], r=[pbn], w=[an])
                                else:
                                    tt("dve", accS[am + m][:, c0:512], accS[am + m][:, c0:512], pb[:, c0:512], ALU.add, r=[pbn, an], w=[an])
                            else:
                                mm1(PO[0][0][0:65, c0:512], vmt[i][:, kb, :], pb[:, c0:512], first, last, r=vn_ + [pbn], w=[PO[0][1]])
                    q_ = [stage1(0), stage1(1)]
                    for kb in range(nkb):
                        if kb + 2 < nkb:
                            q_.append(stage1(kb + 2))
                        if pending:
                            pending.pop(0)()
                        stage2(kb, q_.pop(0))
                    while pending:
                        pending.pop(0)()
                    pending.extend(make_post(isd, h, t, PO, am, tc % 2))
            while pending:
                pending.pop(0)()
        S_.barrier()
        if upto == 'A':
            S_.emit(nc)
            es.close()
            return nc

        with ExitStack() as ph:
            alloc_psum(ph, 6, 2)
            wbd = sb(ph, "mwbd", [128, 8, D], BF16)
            wbm = sb(ph, "mwbm", [64, 8, D], BF16)
            wout = sb(ph, "mwout", [128, 8, D], BF16)
            lng = sb(ph, "mlng", [128, 2, D], F32)
            oa = [sb(ph, "moa%d" % i, [128, 8, 512], BF16) for i in range(2)]
            ob = [sb(ph, "mob%d" % i, [64, 8, 512], BF16) for i in range(2)]
            gs = [sb(ph, "mg%d" % i, [128, 16, 512], BF16) for i in range(2)]
            xs = [sb(ph, "mxs%d" % i, [128, 4, D], F32) for i in range(2)]
            mT = sb(ph, "mmT", [128, 8, 512], BF16)
            tA = [sb(ph, "mtA%d" % i, [128, 512], F32) for i in range(2)]
            tB = [sb(ph, "mtB%d" % i, [128, 512], F32) for i in range(2)]
            zs = [sb(ph, "mzs%d" % i, [128, D], F32) for i in range(2)]
            x1 = [sb(ph, "mx1%d" % i, [128, D], F32) for i in range(2)]
            x1b = sb(ph, "mx1b", [128, D], BF16)
            x1T = sb(ph, "mx1T", [128, 8, 512], BF16)
            st = sb(ph, "mst", [128, 12], F32)
            mv = sb(ph, "mmv", [128, 2], F32)
            rs = sb(ph, "mrs", [128, 1], F32)
            stg_state["tiles"] = [sb(ph, "mstg%d" % i, [128, 1024], F32) for i in range(2)]
            for k in range(8):
                wload(wbd[:, k, :], wbd_in[li, k * 128:(k + 1) * 128, :], 1024, "mwbd")
                wload(wbm[:, k, :], wbm_in[li, k * 64:(k + 1) * 64, :], 1024, "mwbm", parts=64)
                wload(wout[:, k, :], wout_in[li, k * 128:(k + 1) * 128, :], 1024, "mwout")
            dma("sp", lng[:].rearrange("p a d -> p (a d)"), lnm_in[li].rearrange("a d -> (a d)").partition_broadcast(128), r=[], w=["mlng"], key="mlng")

            def ldm(t):
                i = t % 2
                tsl = slice(t * 512, (t + 1) * 512)
                dma("sp", oa[i][:], OAT[:, :, tsl].rearrange("h p s -> p h s"), r=[], w=["moa%d" % i], key="moa%d" % i)
                dma("sp", ob[i][:], OBT[:, :, tsl].rearrange("h p s -> p h s"), r=[], w=["mob%d" % i], key="mob%d" % i)
                dma("sp", gs[i][:], GT[:, :, tsl].rearrange("g p s -> p g s"), r=[], w=["mg%d" % i], key="mg%d" % i)
                dma("sp", xs[i][:], x_src[tsl, :].rearrange("(j p) d -> p j d", p=128), r=[], w=["mxs%d" % i], key="mxs%d" % i)
            ldm(0)
            for t in range(NT):
                i = t % 2
                tsl = slice(t * 512, (t + 1) * 512)
                if t + 1 < NT:
                    ldm(t + 1)
                for db in range(8):
                    dsl = slice(db * 128, (db + 1) * 128)
                    ptA, pnA = nps()
                    mm(ptA[:], [(wbd[:, h, dsl], oa[i][:, h, :]) for h in range(8)], r=["mwbd", "moa%d" % i], w=[pnA])
                    ptB, pnB = nps()
                    mm(ptB[:], [(wbm[:, h, dsl], ob[i][:, h, :]) for h in range(8)], r=["mwbm", "mob%d" % i], w=[pnB])
                    a = db % 2
                    tt("dve", tA[a][:], ptA[:], gs[i][:, db, :], ALU.mult, r=[pnA, "mg%d" % i], w=["mtA%d" % a])
                    tt("dve", tB[a][:], ptB[:], gs[i][:, 8 + db, :], ALU.mult, r=[pnB, "mg%d" % i], w=["mtB%d" % a])
                    tt("pool", mT[:, db, :], tA[a][:], tB[a][:], ALU.add, r=["mtA%d" % a, "mtB%d" % a], w=["mmT%d" % db])
                mTn = ["mmT%d" % db for db in range(8)]
                for j in range(4):
                    a = j % 2
                    for hf in range(2):
                        hsl = slice(hf * 512, (hf + 1) * 512)
                        pt, pn = nps()
                        mm(pt[:], [(mT[:, db, j * 128:(j + 1) * 128], wout[:, db, hsl]) for db in range(8)], r=["mwout"] + mTn, w=[pn])
                        stt("dve", zs[a][:, hsl], xs[i][:, j, hsl], ALPHA, pt[:], ALU.mult, ALU.add, r=["mxs%d" % i, pn], w=["mzs%d_%d" % (a, hf)])
                    zn = ["mzs%d_0" % a, "mzs%d_1" % a]
                    layernorm(add, ts, tt, act, rstd_from, zs[a], x1[a], st, mv, rs, lng, zn, "mx1%d" % a, "m")
                    dma("sp", X1[t * 512 + j * 128:t * 512 + (j + 1) * 128, :], x1[a][:], r=["mx1%d" % a], w=[], key="mx1%d" % a)
                    cp("pool", x1b[:], x1[a][:], r=["mx1%d" % a], w=["mx1b"])
                    for kq in range(2):
                        def tr(e, kq=kq, PSTT=tuple(PSTT)):
                            ins = None
                            for k4 in range(4):
                                kb = kq * 4 + k4
                                ins = e.transpose(out=PSTT[kq][:, k4 * 128:(k4 + 1) * 128],
                                                  in_=x1b[:, kb * 128:(kb + 1) * 128], identity=ident[:])
                            return ins
                        add("pe", tr, r=["mx1b", "ident"], w=["pst%d" % kq])
                        add("act", lambda e, kq=kq, j=j, src=PSTT[kq]: e.copy(out=x1T[:, kq * 4:(kq + 1) * 4, j * 128:(j + 1) * 128],
                                                               in_=src[:, 0:512].rearrange("p (k s) -> p k s", k=4)),
                            r=["pst%d" % kq], w=["mx1T_%d_%d" % (j, kq)])
                dma("sp", X1T[:, :, tsl].rearrange("k p s -> p k s"), x1T[:], r=["mx1T_%d_%d" % (j, kq) for j in range(4) for kq in range(2)], w=[], key="mx1T")
        S_.barrier()
        if upto == 'M':
            S_.emit(nc)
            es.close()
            return nc

        with ExitStack() as ph:
            alloc_psum(ph, 6, 2)
            wpg = sb(ph, "ewpg", [128, 8, D], BF16)
            wpp = sb(ph, "ewpp", [128, 2, D], BF16)
            xT = [sb(ph, "exT%d" % i, [128, 8, 512], BF16) for i in range(2)]
            pp = [sb(ph, "epp%d" % i, [128, 4, 256], F32) for i in range(2)]
            ppb = sb(ph, "eppb", [128, 4, 256], BF16)
            ppT = sb(ph, "eppT", [128, 2, 512], BF16)
            sg = [sb(ph, "esg%d" % i, [128, 512], F32) for i in range(2)]
            ee = [sb(ph, "eee%d" % i, [128, 4, D], F32) for i in range(2)]
            stg_state["tiles"] = [sb(ph, "estg%d" % i, [128, 1024], F32) for i in range(2)]
            for k in range(8):
                wload(wpg[:, k, :], wpg_in[li, k * 128:(k + 1) * 128, :], 1024, "ewpg")
            for k in range(2):
                wload(wpp[:, k, :], wpp_in[li, k * 128:(k + 1) * 128, :], 1024, "ewpp")

            def lde(t):
                i = t % 2
                tsl = slice(t * 512, (t + 1) * 512)
                dma("sp", xT[i][:], X1T[:, :, tsl].rearrange("k p s -> p k s"), r=[], w=["exT%d" % i], key="exT%d" % i)
                dma("sp", pp[i][:], p_in[li, tsl, :].rearrange("(j p) c -> p j c", p=128), r=[], w=["epp%d" % i], key="epp%d" % i)
            lde(0)
            for t in range(NT):
                i = t % 2
                tsl = slice(t * 512, (t + 1) * 512)
                if t + 1 < NT:
                    lde(t + 1)
                cp("pool", ppb[:], pp[i][:], r=["epp%d" % i], w=["eppb"])
                for kq in range(2):
                    def tr(e, kq=kq, PSTT=tuple(PSTT)):
                        ins = None
                        for j in range(4):
                            ins = e.transpose(out=PSTT[kq][:, j * 128:(j + 1) * 128],
                                              in_=ppb[:, j, kq * 128:(kq + 1) * 128], identity=ident[:])
                        return ins
                    add("pe", tr, r=["eppb", "ident"], w=["pst%d" % kq])
                    cp("act", ppT[:, kq, :], PSTT[kq][:, 0:512], r=["pst%d" % kq], w=["eppT%d" % kq])
                for j in range(4):
                    for hf in range(2):
                        hsl = slice(hf * 512, (hf + 1) * 512)
                        a = hf
                        ptA, pnA = nps()
                        mm(ptA[:], [(xT[i][:, kb, j * 128:(j + 1) * 128], wpg[:, kb, hsl]) for kb in range(8)], r=["ewpg", "exT%d" % i], w=[pnA])
                        ptB, pnB = nps()
                        mm(ptB[:], [(ppT[:, kq, j * 128:(j + 1) * 128], wpp[:, kq, hsl]) for kq in range(2)], r=["ewpp", "eppT0", "eppT1"], w=[pnB])
                        act(sg[a][:], ptA[:], AF.Sigmoid, r=[pnA], w=["esg%d" % a])
                        tt("dve", ee[i][:, j, hsl], sg[a][:], ptB[:], ALU.mult, r=["esg%d" % a, pnB], w=["eee%d_%d_%d" % (i, j, hf)])
                dma("sp", EE[tsl, :].rearrange("(j p) d -> p j d", p=128), ee[i][:],
                    r=["eee%d_%d_%d" % (i, j, hf) for j in range(4) for hf in range(2)], w=[], key="eee%d" % i)
        S_.barrier()
        if upto == 'E':
            S_.emit(nc)
            es.close()
            return nc

        moe = (li % 2 == 1)
        lj = li // 2
        if moe:
            with ExitStack() as ph2:
                rwb = sb(ph2, "frw", [128, 8, D], F32)
                rx1 = [sb(ph2, "rx1%d" % i, [128, D], F32) for i in range(2)]
                lg = sb(ph2, "flg", [128, 8], F32)
                junk = sb(ph2, "fjunk", [128, D], F32)
                mx8 = sb(ph2, "fmx8", [128, 8], F32)
                msk = sb(ph2, "fmsk", [128, 8], F32)
                ex = sb(ph2, "fex", [128, 8], F32)
                den = sb(ph2, "fden", [128, 2], F32)
                dma("sp", rwb[:].rearrange("p a d -> p (a d)"), rw_in[lj].partition_broadcast(128), r=[], w=["frw"], key="frw")
                for sbi in range(NB):
                    a = sbi % 2
                    dma("sp", rx1[a][:], X1[sbi * 128:(sbi + 1) * 128, :], r=[], w=["rx1%d" % a], key="rx1%d" % a)
                    for ex_ in range(8):
                        add("dve", lambda e, a=a, ex_=ex_: e.scalar_tensor_tensor(out=junk[:], in0=rx1[a][:], scalar=1.0, in1=rwb[:, ex_, :], op0=ALU.mult, op1=ALU.mult, accum_out=lg[:, ex_:ex_ + 1]),
                            r=["rx1%d" % a, "frw", "flg", "fjunk"], w=["flg", "fjunk"])
                    add("dve", lambda e: e.max(out=mx8[:], in_=lg[:]), r=["flg"], w=["fmx8"])
                    ts("dve", msk[:], lg[:], mx8[:, 1:2], None, ALU.is_ge, None, r=["flg", "fmx8"], w=["fmsk"])
                    ts("dve", ex[:], lg[:], mx8[:, 0:1], None, ALU.subtract, None, r=["flg", "fmx8"], w=["fex"])
                    act(ex[:], ex[:], AF.Exp, r=["fex"], w=["fex"])
                    tt("dve", ex[:], ex[:], msk[:], ALU.mult, r=["fex", "fmsk"], w=["fex"])
                    add("dve", lambda e: e.reduce_sum(out=den[:, 0:1], in_=ex[:], axis=AX.X), r=["fex"], w=["fden"])
                    add("dve", lambda e: e.reciprocal(out=den[:, 1:2], in_=den[:, 0:1]), r=["fden"], w=["fden"])
                    ts("dve", gates[:, sbi, :], ex[:], den[:, 1:2], None, ALU.mult, None, r=["fex", "fden"], w=["gates"])
            S_.barrier()

        with ExitStack() as ph:
            alloc_psum(ph, 8, 0)
            NSUB = TG // 128
            NCB = 7
            Y = sb(ph, "fY", [128, NSUB, D], F32)
            xT = sb(ph, "fxT", [128, 8, TG], BF16)
            wa = [sb(ph, "fwa%d" % i, [128, 8, NCB * 128], BF16) for i in range(2)]
            wb = [sb(ph, "fwb%d" % i, [128, 8, NCB * 128], BF16) for i in range(2)]
            wc = [sb(ph, "fwc%d" % i, [128, NCB, D], BF16) for i in range(2)]
            GTt = [sb(ph, "fG%d" % i, [128, NCB, 512], BF16) for i in range(2)]
            sl = [sb(ph, "fsl%d" % i, [128, 512], F32) for i in range(2)]
            x1 = sb(ph, "fx1", [128, D], F32)
            zz = sb(ph, "fzz", [128, D], F32)
            xo = sb(ph, "fxo", [128, D], F32)
            lng = sb(ph, "flng", [128, 2, D], F32)
            st = sb(ph, "fst", [128, 12], F32)
            mv = sb(ph, "fmv", [128, 2], F32)
            rs = sb(ph, "frs", [128, 1], F32)
            stg_state["tiles"] = [sb(ph, "fstg%d" % i, [128, 1024], F32) for i in range(3)]
            dma("sp", lng[:].rearrange("p a d -> p (a d)"), lnf_in[li].rearrange("a d -> (a d)").partition_broadcast(128), r=[], w=["flng"], key="flng")
            FF = EXP_FF if moe else DENSE_FF
            nblk = FF // 128
            chunks = []
            b0 = 0
            while b0 < nblk:
                nb_ = min(NCB, nblk - b0)
                if nblk - b0 - nb_ in (1, 2) and nb_ > 3:
                    nb_ -= 2
                chunks.append((b0, nb_))
                b0 += nb_
            nexp = 8 if moe else 1
            seq = [(g, ex_, c) for g in range(NG) for ex_ in range(nexp) for c in range(len(chunks))]

            def wjobs(k):
                g, ex_, c = seq[k]
                i = k % 2
                b0, nb_ = chunks[c]
                if moe:
                    s1, s3, s2 = ew1_in[lj, ex_], ew3_in[lj, ex_], ew2_in[lj, ex_]
                else:
                    s1, s3, s2 = dw1_in[lj], dw3_in[lj], dw2_in[lj]
                jobs = []
                wdt = nb_ * 128
                for kb in range(8):
                    jobs.append(lambda kb=kb: wload(wa[i][:, kb, 0:wdt], s1[kb * 128:(kb + 1) * 128, b0 * 128:b0 * 128 + wdt], wdt, "fwa%d_%d" % (i, kb)))
                    jobs.append(lambda kb=kb: wload(wb[i][:, kb, 0:wdt], s3[kb * 128:(kb + 1) * 128, b0 * 128:b0 * 128 + wdt], wdt, "fwb%d_%d" % (i, kb)))
                for jb in range(nb_):
                    jobs.append(lambda jb=jb: wload(wc[i][:, jb, :], s2[(b0 + jb) * 128:(b0 + jb + 1) * 128, :], 1024, "fwc%d_%d" % (i, jb)))
                return jobs
            for jfn in wjobs(0):
                jfn()
            gctr = [0]
            for k, (g, ex_, c) in enumerate(seq):
                i = k % 2
                b0, nb_ = chunks[c]
                gsl = slice(g * TG, (g + 1) * TG)
                if ex_ == 0 and c == 0:
                    dma("sp", xT[:], X1T[:, :, gsl].rearrange("k p s -> p k s"), r=[], w=["fxT"], key="fxT")
                    dma("sp", Y[:], EE[gsl, :].rearrange("(j p) d -> p j d", p=128), r=[], w=["fY%d" % s_ for s_ in range(NSUB)], key="fY")
                pend = wjobs(k + 1) if k + 1 < len(seq) else []
                pend.reverse()

                def pump(n=1):
                    for _ in range(n):
                        if pend:
                            pend.pop()()
                wan = ["fwa%d_%d" % (i, kb) for kb in range(8)]
                wbn = ["fwb%d_%d" % (i, kb) for kb in range(8)]
                for tl in range(TG // 512):
                    gi = gctr[0] % 2
                    gctr[0] += 1
                    tsl = slice(tl * 512, (tl + 1) * 512)
                    for jb in range(nb_):
                        fsl = slice(jb * 128, (jb + 1) * 128)
                        pump(1)
                        pt1, pn1 = nps()
                        mm(pt1[:], [(wa[i][:, kb, fsl], xT[:, kb, tsl]) for kb in range(8)], r=wan + ["fxT"], w=[pn1])
                        pt3, pn3 = nps()
                        mm(pt3[:], [(wb[i][:, kb, fsl], xT[:, kb, tsl]) for kb in range(8)], r=wbn + ["fxT"], w=[pn3])
                        a = jb % 2
                        act(sl[a][:], pt1[:], AF.Silu, r=[pn1], w=["fsl%d" % a])
                        tt("dve", GTt[gi][:, jb, :], sl[a][:], pt3[:], ALU.mult, r=["fsl%d" % a, pn3], w=["fG%d_%d" % (gi, jb)])
                    Gn = ["fG%d_%d" % (gi, jb) for jb in range(nb_)]
                    wcn = ["fwc%d_%d" % (i, jb) for jb in range(nb_)]
                    for j4 in range(4):
                        sub = tl * 4 + j4
                        sbi = g * NSUB + sub
                        for hf in range(2):
                            hsl = slice(hf * 512, (hf + 1) * 512)
                            pump(1)
                            pt, pn = nps()
                            mm(pt[:], [(GTt[gi][:, jb, j4 * 128:(j4 + 1) * 128], wc[i][:, jb, hsl]) for jb in range(nb_)], r=wcn + Gn, w=[pn])
                            gcol = gates[:, sbi, ex_:ex_ + 1] if moe else ccol[:, 2:3]
                            stt("dve", Y[:, sub, hsl], pt[:], gcol, Y[:, sub, hsl], ALU.mult, ALU.add,
                                r=[pn, "fY%d" % sub, "gates", "ccol"], w=["fY%d" % sub])
                pump(100)
                if ex_ == nexp - 1 and c == len(chunks) - 1:
                    for sub in range(NSUB):
                        r0 = g * TG + sub * 128
                        dma("sp", x1[:], X1[r0:r0 + 128, :], r=[], w=["fx1"], key="fx1")
                        stt("dve", zz[:], x1[:], ALPHA, Y[:, sub, :], ALU.mult, ALU.add, r=["fx1", "fY%d" % sub], w=["fzz"])
                        layernorm(add, ts, tt, act, rstd_from, zz, xo, st, mv, rs, lng, ["fzz"], "fxo", "f")
                        dma("sp", x_dst[r0:r0 + 128, :], xo[:], r=["fxo"], w=[], key="fxo")
        S_.barrier()

    S_.emit(nc)
    es.close()
    return nc


def layernorm(add, ts, tt, act, rstd_from, z, xo, st, mv, rs, lng, zn, xon, pfx):
    stn, mvn, rsn = pfx + "st", pfx + "mv", pfx + "rs"
    add("dve", lambda e: e.bn_stats(out=st[:, 0:6], in_=z[:, 0:512]), r=zn, w=[stn])
    add("dve", lambda e: e.bn_stats(out=st[:, 6:12], in_=z[:, 512:1024]), r=zn + [stn], w=[stn])
    add("dve", lambda e: e.bn_aggr(out=mv[:], in_=st[:]), r=[stn], w=[mvn])
    rstd_from(rs[:], mv[:, 1:2], 1.0, r=[mvn], w=[rsn])
    ts("dve", xo[:], z[:], mv[:, 0:1], rs[:, 0:1], ALU.subtract, ALU.mult, r=zn + [mvn, rsn], w=[xon])
    tt("pool", xo[:], xo[:], lng[:, 0, :], ALU.mult, r=[xon, pfx + "lng"], w=[xon])
    tt("pool", xo[:], xo[:], lng[:, 1, :], ALU.add, r=[xon, pfx + "lng"], w=[xon])


def prep_inputs(inp, b, S, L):
    f = np.float32
    def c(a):
        return np.ascontiguousarray(a)
    NM = L // 2
    wuq = np.asarray(inp["w_uq"])[:L].reshape(L, 384, 8, 96)
    wuq_p = np.concatenate([wuq[..., 0:64].reshape(L, 384, 512), wuq[..., 64:80].reshape(L, 384, 128),
                            wuq[..., 80:96].reshape(L, 384, 128)], axis=-1)
    wukv = np.asarray(inp["w_ukv"])[:L].reshape(L, 256, 8, 128)
    wukv_p = np.concatenate([wukv[..., 0:64].reshape(L, 256, 512), wukv[..., 64:128].reshape(L, 256, 512)], axis=-1)
    lam = np.concatenate([np.asarray(inp[k])[:L] for k in ("lambda_q1", "lambda_k1", "lambda_q2", "lambda_k2")], axis=-1).reshape(L, 1, 256)
    d = {
        "x": c(np.asarray(inp["x"])[b, :S]),
        "p": c(np.asarray(inp["p"])[:L, b, :S]),
        "pos": c(np.asarray(inp["positions"])[b, :S].reshape(1, S).astype(np.int32)),
        "tab": c(np.asarray(inp["rel_bias_table"]).reshape(1, 256)),
        "w_in": c(np.asarray(inp["w_in"])[:L]),
        "bg": c(np.asarray(inp["b_gate"])[:L].reshape(L, 16, 128).transpose(0, 2, 1)),
        "lam": c(lam),
        "subg": c(np.asarray(inp["diff_subln_g"])[:L].reshape(L, 128, 1)),
        "gq": c(np.asarray(inp["mla_q_norm_g"])[:L].reshape(L, 3, 128).transpose(0, 2, 1)),
        "wuq": c(wuq_p),
        "gkv": c(np.asarray(inp["mla_kv_norm_g"])[:L].reshape(L, 2, 128).transpose(0, 2, 1)),
        "wukv": c(wukv_p),
        "wbd": c(np.asarray(inp["w_branch_diff"])[:L]),
        "wbm": c(np.asarray(inp["w_branch_mla"])[:L]),
        "wout": c(np.asarray(inp["w_out"])[:L]),
        "lnm": c(np.stack([np.asarray(inp["ln_mix_g"])[:L], np.asarray(inp["ln_mix_b"])[:L]], axis=1)),
        "dw1": c(np.asarray(inp["dense_w1"])[:(L + 1) // 2]),
        "dw3": c(np.asarray(inp["dense_w3"])[:(L + 1) // 2]),
        "dw2": c(np.asarray(inp["dense_w2"])[:(L + 1) // 2]),
        "rw": c(np.asarray(inp["router_w"])[:max(NM, 1)].transpose(0, 2, 1).reshape(max(NM, 1), 1, 8 * 1024)),
        "ew1": c(np.asarray(inp["expert_w1"])[:max(NM, 1)]),
        "ew3": c(np.asarray(inp["expert_w3"])[:max(NM, 1)]),
        "ew2": c(np.asarray(inp["expert_w2"])[:max(NM, 1)]),
        "wpg": c(np.asarray(inp["w_ple_gate"])[:L]),
        "wpp": c(np.asarray(inp["w_ple_proj"])[:L]),
        "lnf": c(np.stack([np.asarray(inp["ln_ffn_g"])[:L], np.asarray(inp["ln_ffn_b"])[:L]], axis=1)),
    }
    cst = np.zeros((128, 4), f)
    half = 16
    invf = (np.float32(10000.0) ** (-np.arange(half, dtype=f) / np.float32(half))).astype(f)
    cst[:, 0] = np.tile(invf, 8)
    d["cst"] = cst
    return {k: np.ascontiguousarray(v) for k, v in d.items()}


_NC_CACHE = {}


def kernel(**inputs):
    S, L, NCORES = 4096, 4, 8
    if "nc" not in _NC_CACHE:
        _NC_CACHE["nc"] = build(S, L, TG=1024)
    nc = _NC_CACHE["nc"]
    in_maps = [prep_inputs(inputs, b, S, L) for b in range(NCORES)]
    res = run_bass_kernel_spmd(nc, in_maps, core_ids=list(range(NCORES)))
    return np.stack([np.asarray(r["out"], dtype=np.float32) for r in res.results], axis=0)
```

```python
import os
import math
from contextlib import ExitStack
import numpy as np
import concourse.bass as bass
import concourse.mybir as mybir
from concourse.bass_utils import run_bass_kernel_spmd

F32 = mybir.dt.float32
BF16 = mybir.dt.bfloat16
I32 = mybir.dt.int32
AF = mybir.ActivationFunctionType
ALU = mybir.AluOpType
AX = mybir.AxisListType

ENGS = ("pe", "act", "dve", "pool", "sp")
SEM_MAX = 20000
NSLOT = 84


class Op:
    __slots__ = ("eng", "fn", "deps", "dma", "slot", "idx", "sig", "ordn", "barrier")

    def __init__(self, eng, fn, dma):
        self.eng, self.fn, self.dma = eng, fn, dma
        self.deps = []
        self.sig = False
        self.ordn = None
        self.slot = None
        self.barrier = False


class Sched:
    def __init__(self):
        self.ops = {e: [] for e in ENGS}
        self.state = {}
        self.slot_of = {}
        self.slot_cnt = [0] * NSLOT
        self.slot_last = [None] * NSLOT
        self.nslot_used = 0
        self.max_slots = 0

    def add(self, eng, fn, r=(), w=(), dma=None):
        op = Op(eng, fn, dma)
        w = list(w) + [b for b in r if b.startswith('ps')]
        r = [b for b in r if not b.startswith('ps')]
        deps = []
        for b in r:
            st = self.state.setdefault(b, [None, []])
            if st[0] is not None:
                deps.append(st[0])
        for b in w:
            st = self.state.setdefault(b, [None, []])
            if st[0] is not None:
                deps.append(st[0])
            deps.extend(st[1])
        seen = set()
        for d in deps:
            if id(d) in seen:
                continue
            seen.add(id(d))
            if d.dma is None and op.dma is None and d.eng == "pe" and eng == "pe":
                continue
            op.deps.append(d)
            if d.dma is None:
                d.sig = True
        if dma is not None:
            if dma not in self.slot_of:
                assert self.nslot_used < NSLOT, "out of dma semaphore slots"
                self.slot_of[dma] = self.nslot_used
                self.nslot_used += 1
                self.max_slots = max(self.max_slots, self.nslot_used)
            sl = self.slot_of[dma]
            self.slot_cnt[sl] += 1
            assert self.slot_cnt[sl] * 16 < 32000, "dma sem count too large: %s" % dma
            op.slot = sl
            op.ordn = self.slot_cnt[sl]
            self.slot_last[sl] = op
        op.idx = len(self.ops[eng])
        self.ops[eng].append(op)
        for b in r:
            self.state[b][1].append(op)
        for b in w:
            self.state[b] = [op, []]
        return op

    def barrier(self):
        used = self.nslot_used
        c = Op("sp", None, None)
        c.barrier = True
        for e in ENGS:
            if e == "sp":
                continue
            for op in reversed(self.ops[e]):
                if op.dma is None:
                    if not op.barrier:
                        c.deps.append(op)
                        op.sig = True
                    break
        for sl in range(used):
            if self.slot_last[sl] is not None:
                c.deps.append(self.slot_last[sl])
        c.slot = used
        c.sig = True
        c.idx = len(self.ops["sp"])
        self.ops["sp"].append(c)
        for e in ENGS:
            if e == "sp":
                continue
            d = Op(e, None, None)
            d.barrier = True
            d.deps.append(c)
            d.idx = len(self.ops[e])
            self.ops[e].append(d)
        self.state = {}
        self.slot_of = {}
        self.slot_cnt = [0] * NSLOT
        self.slot_last = [None] * NSLOT
        self.nslot_used = 0

    def emit(self, nc):
        from contextlib import ExitStack
        self.barrier()
        nsig = {}
        for e in ENGS:
            n = 0
            for op in self.ops[e]:
                if op.dma is None and op.sig:
                    op.ordn = n
                    n += 1
            nsig[e] = n
        with ExitStack() as es:
            esem = {}
            for e in ENGS:
                k = max(1, (nsig[e] + SEM_MAX - 1) // SEM_MAX)
                esem[e] = [es.enter_context(nc.semaphore("s_%s_%d" % (e, i))) for i in range(k)]
            dsem = [es.enter_context(nc.semaphore("d_%d" % i)) for i in range(self.max_slots)]
            block = es.enter_context(nc.Block())

            def target(d):
                if d.dma is not None:
                    return dsem[d.slot], 16 * d.ordn
                return esem[d.eng][d.ordn // SEM_MAX], d.ordn % SEM_MAX + 1

            def run(e, eng):
                waited = {}
                for op in self.ops[e]:
                    for d in op.deps:
                        s, v = target(d)
                        if waited.get(s.name, 0) >= v:
                            continue
                        waited[s.name] = v
                        eng.wait_ge(s, v)
                    if op.barrier:
                        if e == "sp":
                            for sl in range(op.slot):
                                eng.sem_clear(dsem[sl])
                            ins = eng.nop()
                        else:
                            ins = eng.nop()
                        for k in list(waited.keys()):
                            if k.startswith("d_"):
                                del waited[k]
                    else:
                        ins = op.fn(eng)
                    if op.dma is not None:
                        ins.then_inc(dsem[op.slot], 16)
                    elif op.sig:
                        ins.then_inc(esem[e][op.ordn // SEM_MAX], 1)

            @block.tensor
            def _(eng):
                run("pe", eng)

            @block.scalar
            def _(eng):
                run("act", eng)

            @block.vector
            def _(eng):
                run("dve", eng)

            @block.gpsimd
            def _(eng):
                run("pool", eng)

            @block.sync
            def _(eng):
                run("sp", eng)


D = 1024
NQ = 384
NKV = 256
COLQ, COLK, COLV, COLCQ, COLCKV, COLKR, COLG = 0, 1024, 2048, 3072, 3456, 3712, 3744
INC = 5792
DENSE_FF = 2816
EXP_FF = 3584
NEG = -30000.0
DEPTH_TOTAL = 4
ALPHA = (2.0 * DEPTH_TOTAL) ** 0.25
EPS = 1e-5


def t5_thresholds():
    n = np.arange(0, 4096, dtype=np.int32)
    nf = np.maximum(n, 1).astype(np.float32)
    large = 16 + ((np.log(nf / np.float32(16)) / np.float32(math.log(128 / 16))) * np.float32(16)).astype(np.int32)
    large = np.minimum(large, 31)
    bucket = np.where(n < 16, n, large)
    th = []
    for j in range(1, 32):
        th.append(int(np.argmax(bucket >= j)))
    return th


def build(S, L, TG=1024, dbg=(), upto=None):
    nc = bass.Bass("TRN2", target_bir_lowering=False)
    NT = S // 512
    NB = S // 128
    NG = S // TG
    ND = (L + 1) // 2
    NM = L // 2
    es = ExitStack()
    S_ = Sched()
    add = S_.add

    def din(name, shape, dt=F32):
        return nc.dram_tensor(name, list(shape), dt, kind="ExternalInput").ap()

    def dscr(name, shape, dt):
        kind = "ExternalOutput" if name in dbg else "Internal"
        return nc.dram_tensor(name, list(shape), dt, kind=kind).ap()

    x_in = din("x", [S, D])
    p_in = din("p", [L, S, 256])
    pos_in = din("pos", [1, S], I32)
    tab_in = din("tab", [1, 256])
    w_in = din("w_in", [L, D, INC])
    bg_in = din("bg", [L, 128, 16])
    lam_in = din("lam", [L, 1, 256])
    subg_in = din("subg", [L, 128, 1])
    gq_in = din("gq", [L, 128, 3])
    wuq_in = din("wuq", [L, NQ, 768])
    gkv_in = din("gkv", [L, 128, 2])
    wukv_in = din("wukv", [L, NKV, 1024])
    wbd_in = din("wbd", [L, 1024, D])
    wbm_in = din("wbm", [L, 512, D])
    wout_in = din("wout", [L, D, D])
    lnm_in = din("lnm", [L, 2, D])
    dw1_in = din("dw1", [ND, D, DENSE_FF])
    dw3_in = din("dw3", [ND, D, DENSE_FF])
    dw2_in = din("dw2", [ND, DENSE_FF, D])
    rw_in = din("rw", [max(NM, 1), 1, 8 * D])
    ew1_in = din("ew1", [max(NM, 1), 8, D, EXP_FF])
    ew3_in = din("ew3", [max(NM, 1), 8, D, EXP_FF])
    ew2_in = din("ew2", [max(NM, 1), 8, EXP_FF, D])
    wpg_in = din("wpg", [L, D, D])
    wpp_in = din("wpp", [L, 256, D])
    lnf_in = din("lnf", [L, 2, D])
    cst_in = din("cst", [128, 4])
    out = nc.dram_tensor("out", [S, D], F32, kind="ExternalOutput").ap()

    XT = dscr("XT", [8, 128, S], BF16)
    QD = dscr("QD", [8, 128, S], BF16)
    KD = dscr("KD", [8, 128, S], BF16)
    VD = dscr("VD", [S, 1024], BF16)
    GT = dscr("GT", [16, 128, S], BF16)
    QMN = dscr("QMN", [4, 128, S], BF16)
    KMN = dscr("KMN", [4, 128, S], BF16)
    QR = dscr("QR", [2, 128, S], BF16)
    KR = dscr("KR", [2, 16, S], BF16)
    VM = dscr("VM", [S, 512], BF16)
    OAT = dscr("OAT", [8, 128, S], BF16)
    OBT = dscr("OBT", [8, 64, S], BF16)
    X1 = dscr("X1", [S, D], F32)
    X1T = dscr("X1T", [8, 128, S], BF16)
    EE = dscr("EE", [S, D], F32)
    XR = dscr("XR", [S, D], F32)
    CS = dscr("CS", [2, 128, S], F32)
    BST = dscr("BST", [9, 128, 1024], F32)

    sbc = [0]

    def sb(stack, name, shape, dt):
        sbc[0] += 1
        return stack.enter_context(nc.sbuf_tensor("sb_%s_%d" % (name, sbc[0]), list(shape), dt))

    ident = sb(es, "ident", [128, 128], BF16)
    ones_bf = sb(es, "ones_bf", [128, 128], BF16)
    ones_f = sb(es, "ones_f", [128, 128], F32)
    cst = sb(es, "cst", [128, 4], F32)
    ccol = sb(es, "ccol", [128, 8], F32)
    tabb = sb(es, "tabb", [128, 256], F32)
    gates = sb(es, "gates", [128, NB, 8], F32)
    PSB = []
    PSTT = []
    psn = ["ps%d" % i for i in range(8)]
    psc = [0]

    def alloc_psum(ph, nf, nbf):
        psc[0] += 1
        PSB[:] = [ph.enter_context(nc.psum_tensor("psb%d_%d" % (i, psc[0]), [128, 512], F32)) for i in range(nf)]
        PSTT[:] = [ph.enter_context(nc.psum_tensor("pst%d_%d" % (i, psc[0]), [128, 1024], BF16)) for i in range(nbf)]
    ring = [0]

    def nps():
        i = ring[0] % len(PSB)
        ring[0] += 1
        return PSB[i], psn[i]

    def mm(out_ap, pairs, r, w):
        pairs = list(pairs)

        def fn(e):
            ins = None
            n = len(pairs)
            for i, (l, rh) in enumerate(pairs):
                ins = e.matmul(out_ap, lhsT=l, rhs=rh, start=(i == 0), stop=(i == n - 1))
            return ins
        add("pe", fn, r=r, w=w)

    def mm1(out_ap, l, rh, start, stop, r, w):
        add("pe", lambda e: e.matmul(out_ap, lhsT=l, rhs=rh, start=start, stop=stop), r=r, w=w)

    def dma(eng, out_ap, in_ap, r, w, key):
        add(eng, lambda e: e.dma_start(out=out_ap, in_=in_ap), r=r, w=w, dma=key)

    def act(out_ap, in_ap, func, r, w, bias=None, scale=None):
        kw = {}
        if bias is not None:
            kw["bias"] = bias
        if scale is not None:
            kw["scale"] = scale
        add("act", lambda e: e.activation(out=out_ap, in_=in_ap, func=func, **kw), r=r, w=w)

    def tt(eng, out_ap, a, b, op, r, w):
        add(eng, lambda e: e.tensor_tensor(out=out_ap, in0=a, in1=b, op=op), r=r, w=w)

    def ts(eng, out_ap, a, s1, s2, op0, op1, r, w):
        if op1 is None:
            add(eng, lambda e: e.tensor_scalar(out=out_ap, in0=a, scalar1=s1, scalar2=None, op0=op0), r=r, w=w)
        else:
            add(eng, lambda e: e.tensor_scalar(out=out_ap, in0=a, scalar1=s1, scalar2=s2, op0=op0, op1=op1), r=r, w=w)

    def stt(eng, out_ap, a, sc, b, op0, op1, r, w):
        add(eng, lambda e: e.scalar_tensor_tensor(out=out_ap, in0=a, scalar=sc, in1=b, op0=op0, op1=op1), r=r, w=w)

    def cp(eng, out_ap, in_ap, r, w):
        if eng == "act":
            add("act", lambda e: e.copy(out=out_ap, in_=in_ap), r=r, w=w)
        else:
            add(eng, lambda e: e.tensor_copy(out=out_ap, in_=in_ap), r=r, w=w)

    STGW = 2048
    stg_state = {"tiles": None, "n": 0}
    cast_rot = ("pool", "act", "pool", "dve")

    def wload(dst_ap, src_ap, width, wname, parts=128):
        tiles = stg_state["tiles"]
        k = stg_state["n"]
        stg_state["n"] += 1
        i = k % len(tiles)
        st_ = tiles[i]
        dma("sp", st_[0:parts, 0:width], src_ap, r=[], w=["stg%d" % i], key="stg%d" % i)
        cp(cast_rot[k % 4], dst_ap, st_[0:parts, 0:width], r=["stg%d" % i], w=[wname])

    def rstd_from(out_ap, in_ap, scale, r, w):
        act(out_ap, in_ap, AF.Ln, r=r + ["ccol"], w=w, bias=ccol[:, 0:1], scale=scale)
        act(out_ap, out_ap, AF.Exp, r=w, w=w, scale=-0.5)

    add("pool", lambda e: e.memset(ident[:], 0.0), w=["ident"])
    add("pool", lambda e: e.affine_select(out=ident[:], in_=ident[:], compare_op=ALU.not_equal, fill=1.0,
                                          base=0, pattern=[[-1, 128]], channel_multiplier=1),
        r=["ident"], w=["ident"])
    add("dve", lambda e: e.memset(ones_bf[:], 1.0), w=["ones_bf"])
    add("dve", lambda e: e.memset(ones_f[:], 1.0), w=["ones_f"])
    add("dve", lambda e: e.memset(ccol[:, 0:1], EPS), w=["ccol"])
    add("dve", lambda e: e.memset(ccol[:, 1:2], -math.pi), r=["ccol"], w=["ccol"])
    add("dve", lambda e: e.memset(ccol[:, 2:3], 1.0), r=["ccol"], w=["ccol"])
    add("dve", lambda e: e.memset(ccol[:, 3:4], 0.0), r=["ccol"], w=["ccol"])
    dma("sp", cst[:], cst_in, r=[], w=["cst"], key="cst")
    dma("sp", tabb[:], tab_in.partition_broadcast(128), r=[], w=["tabb"], key="tabb")

    with ExitStack() as ph:
        posi = sb(ph, "posi", [128, S], I32)
        posf = sb(ph, "posf", [128, S], F32)
        ang = sb(ph, "ang", [128, S], F32)
        tmpc = sb(ph, "tmpc", [128, S], F32)
        dma("sp", posi[:], pos_in.partition_broadcast(128), r=[], w=["posi"], key="posi")
        cp("dve", posf[:], posi[:], r=["posi"], w=["posf"])
        ts("dve", ang[:], posf[:], cst[:, 0:1], None, ALU.mult, None, r=["posf", "cst"], w=["ang"])
        C1 = 6.28125
        C2 = 2.0 * math.pi - C1
        PI_IN = 3.1415925
        indc = sb(ph, "indc", [128, S], F32)
        ts("dve", tmpc[:], ang[:], 1.0 / (2.0 * math.pi), None, ALU.mult, None, r=["ang"], w=["tmpc"])
        cp("dve", posi[:], tmpc[:], r=["tmpc", "posf"], w=["posi"])
        cp("dve", posf[:], posi[:], r=["posi", "ang"], w=["posf"])
        stt("dve", tmpc[:], posf[:], -C1, ang[:], ALU.mult, ALU.add, r=["posf", "ang"], w=["tmpc"])
        stt("dve", tmpc[:], posf[:], -C2, tmpc[:], ALU.mult, ALU.add, r=["posf", "tmpc"], w=["tmpc"])

        def wrap(tn, t):
            ts("dve", indc[:], t[:], math.pi, None, ALU.is_gt, None, r=[tn], w=["indc"])
            stt("dve", t[:], indc[:], -2.0 * math.pi, t[:], ALU.mult, ALU.add, r=["indc", tn], w=[tn])
            ts("dve", indc[:], t[:], -math.pi, None, ALU.is_lt, None, r=[tn], w=["indc"])
            stt("dve", t[:], indc[:], 2.0 * math.pi, t[:], ALU.mult, ALU.add, r=["indc", tn], w=[tn])
            ts("dve", t[:], t[:], PI_IN, -PI_IN, ALU.min, ALU.max, r=[tn], w=[tn])
        wrap("tmpc", tmpc)
        ts("dve", ang[:], tmpc[:], 0.5 * math.pi, None, ALU.add, None, r=["tmpc"], w=["ang"])
        wrap("ang", ang)
        act(ang[:], ang[:], AF.Sin, r=["ang"], w=["ang"])
        dma("sp", CS[0], ang[:], r=["ang"], w=[], key="ang")
        act(tmpc[:], tmpc[:], AF.Sin, r=["tmpc"], w=["tmpc"])
        dma("sp", CS[1], tmpc[:], r=["tmpc"], w=[], key="tmpc")
    S_.barrier()
    with ExitStack() as ph:
        nmi = sb(ph, "nmi", [128, 1024], I32)
        nmat = sb(ph, "nmat", [128, 1024], F32)
        ind = sb(ph, "ind", [128, 1024], F32)
        mstrip = sb(ph, "mstrip", [128, 1024], F32)
        dtab = sb(ph, "dtab", [128, 256], F32)
        bst = [sb(ph, "bst%d" % h, [128, 1024], F32) for h in range(8)]
        add("pool", lambda e: e.iota(nmi[:], pattern=[[1, 1024]], base=-384, channel_multiplier=-1), w=["nmi"])
        cp("dve", nmat[:], nmi[:], r=["nmi"], w=["nmat"])
        tt("dve", dtab[:, 8:256], tabb[:, 8:256], tabb[:, 0:248], ALU.subtract, r=["tabb"], w=["dtab"])
        ts("dve", ind[:], nmat[:], 0.0, None, ALU.is_ge, None, r=["nmat"], w=["ind"])
        ts("dve", mstrip[:], ind[:], 1.0, -NEG, ALU.subtract, ALU.mult, r=["ind"], w=["mstrip"])
        for h in range(8):
            ts("pool", bst[h][:], mstrip[:], tabb[:, h:h + 1], None, ALU.add, None, r=["mstrip", "tabb"], w=["bst%d" % h])
        th = t5_thresholds()
        for j in range(1, 32):
            ts("dve", ind[:], nmat[:], float(th[j - 1]), None, ALU.is_ge, None, r=["nmat"], w=["ind"])
            for h in range(8):
                eng = "dve"
                stt(eng, bst[h][:], ind[:], dtab[:, j * 8 + h:j * 8 + h + 1], bst[h][:], ALU.mult, ALU.add,
                    r=["ind", "dtab", "bst%d" % h], w=["bst%d" % h])
        for h in range(8):
            dma("sp", BST[h], bst[h][:], r=["bst%d" % h], w=[], key="bst%d" % h)
        dma("sp", BST[8], mstrip[:], r=["mstrip"], w=[], key="mstrip")
    S_.barrier()
    if upto == 'C':
        S_.emit(nc)
        es.close()
        return nc

    for li in range(L):
        lam_init = 0.8 - 0.6 * math.exp(-0.3 * li)
        x_src = x_in if li == 0 else XR
        x_dst = out if li == L - 1 else XR

        with ExitStack() as ph:
            alloc_psum(ph, 6, 2)
            w1 = sb(ph, "p1w", [128, 8, 3072], BF16)
            xs = [sb(ph, "p1xs%d" % i, [128, 4, 1024], F32) for i in range(2)]
            xb = sb(ph, "p1xb", [128, 4, 1024], BF16)
            xT = [sb(ph, "p1xT%d" % i, [128, 8, 512], BF16) for i in range(2)]
            qd = [sb(ph, "p1qd%d" % i, [128, 8, 512], BF16) for i in range(2)]
            kd = [sb(ph, "p1kd%d" % i, [128, 8, 512], BF16) for i in range(2)]
            vd = [sb(ph, "p1vd%d" % i, [128, 4, 1024], BF16) for i in range(2)]
            stg_state["tiles"] = [sb(ph, "p1stg%d" % i, [128, STGW], F32) for i in range(3)]
            for kb in range(8):
                for c3 in range(0, 3072, STGW):
                    wd = min(STGW, 3072 - c3)
                    wload(w1[:, kb, c3:c3 + wd], w_in[li, kb * 128:(kb + 1) * 128, c3:c3 + wd], wd, "p1w%d_%d" % (kb, c3))
            wn = ["p1w%d_%d" % (kb, c3) for kb in range(8) for c3 in range(0, 3072, STGW)]

            def ldx(t):
                i = t % 2
                dma("sp", xs[i][:], x_src[t * 512:(t + 1) * 512, :].rearrange("(j p) d -> p j d", p=128),
                    r=[], w=["p1xs%d" % i], key="p1xs%d" % i)
            ldx(0)
            for t in range(NT):
                i = t % 2
                if t + 1 < NT:
                    ldx(t + 1)
                import os
                cut = int(os.environ.get("P1CUT", "99"))
                if cut < 1:
                    continue
                for j in range(4):
                    cp("dve" if j % 2 == 0 else "pool", xb[:, j, :], xs[i][:, j, :], r=["p1xs%d" % i], w=["p1xb%d" % j])
                if cut < 2:
                    continue
                for kb in range(8):
                    def tr(e, kb=kb, PSTT=tuple(PSTT)):
                        ins = None
                        for j in range(4):
                            ins = e.transpose(out=PSTT[kb % 2][:, j * 128:(j + 1) * 128],
                                              in_=xb[:, j, kb * 128:(kb + 1) * 128], identity=ident[:])
                        return ins
                    sub_ = os.environ.get("P1SUB", "")
                    if sub_ == "one" and kb > 0:
                        continue
                    add("pe", tr, r=["p1xb%d" % j for j in range(4)] + ["ident"], w=["pst%d" % (kb % 2)])
                    if sub_ == "tr":
                        continue
                    cp("act" if kb % 2 else "dve", xT[i][:, kb, :], PSTT[kb % 2][:, 0:512],
                       r=["pst%d" % (kb % 2)], w=["p1xT%d_%d" % (i, kb)])
                xTn = ["p1xT%d_%d" % (i, kb) for kb in range(8)]
                if cut < 3:
                    continue
                dma("sp", XT[:, :, t * 512:(t + 1) * 512].rearrange("k p s -> p k s"), xT[i][:], r=xTn, w=[], key="p1xT%d" % i)
                if cut < 4:
                    continue
                for (col, dst, dstn, DR, sc) in ((COLQ, qd, "p1qd", QD, 0.125), (COLK, kd, "p1kd", KD, 1.0)):
                    for h in range(8):
                        pt, pn = nps()
                        mm(pt[:], [(w1[:, kb, col + h * 128:col + (h + 1) * 128], xT[i][:, kb, :]) for kb in range(8)],
                           r=wn + xTn, w=[pn])
                        if h % 2 == 0:
                            act(dst[i][:, h, :], pt[:], AF.Copy, r=[pn], w=["%s%d_%d" % (dstn, i, h)], scale=sc)
                        else:
                            ts("dve", dst[i][:, h, :], pt[:], sc, None, ALU.mult, None, r=[pn], w=["%s%d_%d" % (dstn, i, h)])
                    dma("sp", DR[:, :, t * 512:(t + 1) * 512].rearrange("h p s -> p h s"), dst[i][:],
                        r=["%s%d_%d" % (dstn, i, h) for h in range(8)], w=[], key="%s%d" % (dstn, i))
                if cut < 5:
                    continue
                for j in range(4):
                    for hf in range(2):
                        pt, pn = nps()
                        mm(pt[:], [(xT[i][:, kb, j * 128:(j + 1) * 128], w1[:, kb, COLV + hf * 512:COLV + (hf + 1) * 512]) for kb in range(8)],
                           r=wn + xTn, w=[pn])
                        cp("act" if hf else "dve", vd[i][:, j, hf * 512:(hf + 1) * 512], pt[:], r=[pn], w=["p1vd%d_%d_%d" % (i, j, hf)])
                dma("sp", VD[t * 512:(t + 1) * 512, :].rearrange("(j p) e -> p j e", p=128), vd[i][:],
                    r=["p1vd%d_%d_%d" % (i, j, hf) for j in range(4) for hf in range(2)], w=[], key="p1vd%d" % i)
        S_.barrier()
        if upto == 'P1':
            S_.emit(nc)
            es.close()
            return nc

        with ExitStack() as ph:
            alloc_psum(ph, 8, 0)
            w2 = sb(ph, "p2w", [128, 8, INC - 3072], BF16)
            W2O = 3072
            wq_st = sb(ph, "p2wqs", [128, 3, 768], F32)
            wkv_st = sb(ph, "p2wkvs", [128, 2, 1024], F32)
            wq = sb(ph, "p2wq", [128, 3, 768], BF16)
            wkv = sb(ph, "p2wkv", [128, 2, 1024], BF16)
            gq = sb(ph, "p2gq", [128, 3], F32)
            gkv = sb(ph, "p2gkv", [128, 2], F32)
            bg = sb(ph, "p2bg", [128, 16], F32)
            xT = [sb(ph, "p2xT%d" % i, [128, 8, 512], BF16) for i in range(2)]
            cs = [sb(ph, "p2cs%d" % i, [128, 2, 512], F32) for i in range(2)]
            gsb = sb(ph, "p2g", [128, 16, 512], BF16)
            cqb = sb(ph, "p2cqb", [128, 3, 512], BF16)
            sqq = sb(ph, "p2sqq", [128, 3, 512], BF16)
            ckb = sb(ph, "p2ckb", [128, 2, 512], BF16)
            sqk = sb(ph, "p2sqk", [128, 2, 512], BF16)
            rq = sb(ph, "p2rq", [128, 512], F32)
            rk = sb(ph, "p2rk", [128, 512], F32)
            rkc = sb(ph, "p2rkc", [128, 4], F32)
            qn = sb(ph, "p2qn", [128, 4, 512], BF16)
            kn = sb(ph, "p2kn", [128, 4, 512], BF16)
            vm = sb(ph, "p2vm", [128, 4, 512], BF16)
            x1s = sb(ph, "p2x1s", [128, 512], F32)
            x2s = sb(ph, "p2x2s", [128, 512], F32)
            ta = sb(ph, "p2ta", [128, 512], F32)
            tb = sb(ph, "p2tb", [128, 512], F32)
            qr = sb(ph, "p2qr", [128, 2, 512], BF16)
            kr = sb(ph, "p2kr", [16, 2, 512], BF16)
            stg_state["tiles"] = [sb(ph, "p2stg%d" % i, [128, STGW], F32) for i in range(2)]
            W2W = INC - 3072
            for kb in range(8):
                for c3 in range(0, W2W, STGW):
                    wd = min(STGW, W2W - c3)
                    wload(w2[:, kb, c3:c3 + wd], w_in[li, kb * 128:(kb + 1) * 128, 3072 + c3:3072 + c3 + wd], wd, "p2w%d_%d" % (kb, c3))
            wn = ["p2w%d_%d" % (kb, c3) for kb in range(8) for c3 in range(0, W2W, STGW)]
            dma("sp", wq_st[:], wuq_in[li].rearrange("(k p) c -> p k c", p=128), r=[], w=["p2wqs"], key="p2wqs")
            dma("sp", wkv_st[:], wukv_in[li].rearrange("(k p) c -> p k c", p=128), r=[], w=["p2wkvs"], key="p2wkvs")
            dma("sp", gq[:], gq_in[li], r=[], w=["p2gq"], key="p2gq")
            dma("sp", gkv[:], gkv_in[li], r=[], w=["p2gkv"], key="p2gkv")
            dma("sp", bg[:], bg_in[li], r=[], w=["p2bg"], key="p2bg")
            for k in range(3):
                ts("dve", wq[:, k, :], wq_st[:, k, :], gq[:, k:k + 1], None, ALU.mult, None, r=["p2wqs", "p2gq"], w=["p2wq"])
            for k in range(2):
                ts("dve", wkv[:, k, :], wkv_st[:, k, :], gkv[:, k:k + 1], None, ALU.mult, None, r=["p2wkvs", "p2gkv"], w=["p2wkv"])

            def ldt(t):
                i = t % 2
                dma("sp", xT[i][:], XT[:, :, t * 512:(t + 1) * 512].rearrange("k p s -> p k s"), r=[], w=["p2xT%d" % i], key="p2xT%d" % i)
                dma("sp", cs[i][:], CS[:, :, t * 512:(t + 1) * 512].rearrange("c p s -> p c s"), r=[], w=["p2cs%d" % i], key="p2cs%d" % i)
            ldt(0)
            MSC = 96.0 ** -0.5
            for t in range(NT):
                i = t % 2
                if t + 1 < NT:
                    ldt(t + 1)
                xn = ["p2xT%d" % i]
                tsl = slice(t * 512, (t + 1) * 512)
                for gb in range(16):
                    pt, pn = nps()
                    c0 = COLG - W2O + gb * 128
                    mm(pt[:], [(w2[:, kb, c0:c0 + 128], xT[i][:, kb, :]) for kb in range(8)], r=wn + xn, w=[pn])
                    act(gsb[:, gb, :], pt[:], AF.Sigmoid, r=[pn, "p2bg"], w=["p2g%d" % gb], bias=bg[:, gb:gb + 1], scale=1.0)
                dma("sp", GT[:, :, tsl].rearrange("g p s -> p g s"), gsb[:], r=["p2g%d" % gb for gb in range(16)], w=[], key="p2g")
                for b in range(3):
                    pt, pn = nps()
                    c0 = COLCQ - W2O + b * 128
                    mm(pt[:], [(w2[:, kb, c0:c0 + 128], xT[i][:, kb, :]) for kb in range(8)], r=wn + xn, w=[pn])
                    cp("dve", cqb[:, b, :], pt[:], r=[pn], w=["p2cqb%d" % b])
                    act(sqq[:, b, :], pt[:], AF.Square, r=[pn], w=["p2sqq%d" % b])
                pt, pn = nps()
                mm(pt[:], [(ones_bf[:], sqq[:, b, :]) for b in range(3)], r=["ones_bf"] + ["p2sqq%d" % b for b in range(3)], w=[pn])
                rstd_from(rq[:], pt[:], 1.0 / NQ, r=[pn], w=["p2rq"])
                ts("dve", rq[:], rq[:], MSC, None, ALU.mult, None, r=["p2rq"], w=["p2rq"])
                cqn = ["p2cqb%d" % b for b in range(3)]
                for pr in range(4):
                    pt, pn = nps()
                    mm(pt[:], [(wq[:, k, pr * 128:(pr + 1) * 128], cqb[:, k, :]) for k in range(3)], r=["p2wq"] + cqn, w=[pn])
                    tt("dve", qn[:, pr, :], pt[:], rq[:], ALU.mult, r=[pn, "p2rq"], w=["p2qn%d" % pr])
                dma("sp", QMN[:, :, tsl].rearrange("j p s -> p j s"), qn[:], r=["p2qn%d" % pr for pr in range(4)], w=[], key="p2qn")
                pt1, pn1 = nps()
                mm(pt1[:], [(wq[:, k, 512:640], cqb[:, k, :]) for k in range(3)], r=["p2wq"] + cqn, w=[pn1])
                pt2, pn2 = nps()
                mm(pt2[:], [(wq[:, k, 640:768], cqb[:, k, :]) for k in range(3)], r=["p2wq"] + cqn, w=[pn2])
                tt("dve", x1s[:], pt1[:], rq[:], ALU.mult, r=[pn1, "p2rq"], w=["p2x1s"])
                tt("dve", x2s[:], pt2[:], rq[:], ALU.mult, r=[pn2, "p2rq"], w=["p2x2s"])
                csn = "p2cs%d" % i
                tt("dve", ta[:], x1s[:], cs[i][:, 0, :], ALU.mult, r=["p2x1s", csn], w=["p2ta"])
                tt("pool", tb[:], x2s[:], cs[i][:, 1, :], ALU.mult, r=["p2x2s", csn], w=["p2tb"])
                tt("dve", qr[:, 0, :], ta[:], tb[:], ALU.subtract, r=["p2ta", "p2tb"], w=["p2qr0"])
                tt("dve", ta[:], x1s[:], cs[i][:, 1, :], ALU.mult, r=["p2x1s", csn], w=["p2ta"])
                tt("pool", tb[:], x2s[:], cs[i][:, 0, :], ALU.mult, r=["p2x2s", csn], w=["p2tb"])
                tt("dve", qr[:, 1, :], ta[:], tb[:], ALU.add, r=["p2ta", "p2tb"], w=["p2qr1"])
                dma("sp", QR[:, :, tsl].rearrange("c p s -> p c s"), qr[:], r=["p2qr0", "p2qr1"], w=[], key="p2qr")
                for b in range(2):
                    pt, pn = nps()
                    c0 = COLCKV - W2O + b * 128
                    mm(pt[:], [(w2[:, kb, c0:c0 + 128], xT[i][:, kb, :]) for kb in range(8)], r=wn + xn, w=[pn])
                    cp("dve", ckb[:, b, :], pt[:], r=[pn], w=["p2ckb%d" % b])
                    act(sqk[:, b, :], pt[:], AF.Square, r=[pn], w=["p2sqk%d" % b])
                sqn = ["p2sqk%d" % b for b in range(2)]
                pt, pn = nps()
                mm(pt[:], [(ones_bf[:], sqk[:, b, :]) for b in range(2)], r=["ones_bf"] + sqn, w=[pn])
                rstd_from(rk[:], pt[:], 1.0 / NKV, r=[pn], w=["p2rk"])
                ptc, pnc = nps()
                for j in range(4):
                    mm(ptc[:, j:j + 1], [(sqk[:, b, j * 128:(j + 1) * 128], ones_bf[:, 0:1]) for b in range(2)], r=["ones_bf"] + sqn, w=[pnc])
                rstd_from(rkc[:], ptc[:, 0:4], 1.0 / NKV, r=[pnc], w=["p2rkc"])
                ckn = ["p2ckb%d" % b for b in range(2)]
                for pr in range(4):
                    pt, pn = nps()
                    mm(pt[:], [(wkv[:, k, pr * 128:(pr + 1) * 128], ckb[:, k, :]) for k in range(2)], r=["p2wkv"] + ckn, w=[pn])
                    tt("dve", kn[:, pr, :], pt[:], rk[:], ALU.mult, r=[pn, "p2rk"], w=["p2kn%d" % pr])
                dma("sp", KMN[:, :, tsl].rearrange("j p s -> p j s"), kn[:], r=["p2kn%d" % pr for pr in range(4)], w=[], key="p2kn")
                for j in range(4):
                    pt, pn = nps()
                    mm(pt[:], [(ckb[:, k, j * 128:(j + 1) * 128], wkv[:, k, 512:1024]) for k in range(2)], r=["p2wkv"] + ckn, w=[pn])
                    act(vm[:, j, :], pt[:], AF.Copy, r=[pn, "p2rkc"], w=["p2vm%d" % j], scale=rkc[:, j:j + 1])
                dma("sp", VM[tsl, :].rearrange("(j p) e -> p j e", p=128), vm[:], r=["p2vm%d" % j for j in range(4)], w=[], key="p2vm")
                pt1, pn1 = nps()
                c0 = COLKR - W2O
                mm(pt1[0:16, :], [(w2[:, kb, c0:c0 + 16], xT[i][:, kb, :]) for kb in range(8)], r=wn + xn, w=[pn1])
                pt2, pn2 = nps()
                mm(pt2[0:16, :], [(w2[:, kb, c0 + 16:c0 + 32], xT[i][:, kb, :]) for kb in range(8)], r=wn + xn, w=[pn2])
                tt("dve", ta[0:16, :], pt1[0:16, :], cs[i][0:16, 0, :], ALU.mult, r=[pn1, csn], w=["p2ta"])
                tt("dve", tb[0:16, :], pt2[0:16, :], cs[i][0:16, 1, :], ALU.mult, r=[pn2, csn], w=["p2tb"])
                tt("dve", kr[:, 0, :], ta[0:16, :], tb[0:16, :], ALU.subtract, r=["p2ta", "p2tb"], w=["p2kr0"])
                tt("dve", ta[0:16, :], pt1[0:16, :], cs[i][0:16, 1, :], ALU.mult, r=[pn1, csn], w=["p2ta"])
                tt("dve", tb[0:16, :], pt2[0:16, :], cs[i][0:16, 0, :], ALU.mult, r=[pn2, csn], w=["p2tb"])
                tt("dve", kr[:, 1, :], ta[0:16, :], tb[0:16, :], ALU.add, r=["p2ta", "p2tb"], w=["p2kr1"])
                dma("sp", KR[:, :, tsl].rearrange("c p s -> p c s"), kr[:], r=["p2kr0", "p2kr1"], w=[], key="p2kr")
        S_.barrier()
        if upto == 'P2':
            S_.emit(nc)
            es.close()
            return nc

        with ExitStack() as ph:
            alloc_psum(ph, 8, 0)
            bstr = sb(ph, "abst", [128, 9, 1024], F32)
            lamt = sb(ph, "alam", [128, 256], F32)
            lamp = sb(ph, "alamp", [128, 128], F32)
            lamc = sb(ph, "alamc", [128, 4], F32)
            subg = sb(ph, "asubg", [128, 1], F32)
            kdt = [sb(ph, "akd%d" % i, [128, S], BF16) for i in range(2)]
            vdt = [sb(ph, "avd%d" % i, [128, NB, 128], BF16) for i in range(2)]
            vmt = [sb(ph, "avm%d" % i, [128, NB, 65], BF16) for i in range(2)]
            qt = [sb(ph, "aq%d" % i, [128, 512], BF16) for i in range(2)]
            NPT = 6
            pT = [sb(ph, "apT%d" % i, [128, 512], BF16) for i in range(NPT)]
            sbias = [sb(ph, "asb%d" % i, [128, 512], F32) for i in range(4)]
            accS = [sb(ph, "aacc%d" % i, [128, 512], F32) for i in range(4)]
            accB = [sb(ph, "aaccb%d" % i, [128, 512], BF16) for i in range(2)]
            sbc2 = [0]
            rr = sb(ph, "arr", [128, 512], F32)
            rr2 = sb(ph, "arr2", [1, 512], F32)
            Rb = [sb(ph, "aRb%d" % i, [128, 512], F32) for i in range(2)]
            t0 = sb(ph, "at0", [128, 512], F32)
            t1 = sb(ph, "at1", [128, 512], F32)
            osq = sb(ph, "aosq", [128, 512], BF16)
            rms = sb(ph, "arms", [128, 512], F32)
            oo = [sb(ph, "aoo%d" % i, [128, 512], BF16) for i in range(2)]
            dma("sp", bstr[:], BST.rearrange("h p n -> p h n"), r=[], w=["abst"], key="abst")
            dma("sp", lamt[:], lam_in[li].partition_broadcast(128), r=[], w=["alam"], key="alam")
            dma("sp", subg[:], subg_in[li], r=[], w=["asubg"], key="asubg")
            tt("dve", lamp[:, 0:64], lamt[:, 0:64], lamt[:, 64:128], ALU.mult, r=["alam"], w=["alamp"])
            tt("dve", lamp[:, 64:128], lamt[:, 128:192], lamt[:, 192:256], ALU.mult, r=["alam", "alamp"], w=["alamp"])
            add("dve", lambda e: e.reduce_sum(out=lamc[:, 0:1], in_=lamp[:, 0:64], axis=AX.X), r=["alamp"], w=["alamc"])
            add("dve", lambda e: e.reduce_sum(out=lamc[:, 1:2], in_=lamp[:, 64:128], axis=AX.X), r=["alamp", "alamc"], w=["alamc"])
            act(lamc[:, 0:2], lamc[:, 0:2], AF.Exp, r=["alamc"], w=["alamc"])
            tt("dve", lamc[:, 2:3], lamc[:, 1:2], lamc[:, 0:1], ALU.subtract, r=["alamc"], w=["alamc"])
            ts("dve", lamc[:, 2:3], lamc[:, 2:3], -lam_init, None, ALU.add, None, r=["alamc"], w=["alamc"])
            for i in range(2):
                add("pool", lambda e, i=i: e.memset(vmt[i][:, :, 64:65], 1.0), w=["avm1_%d" % i])
            PS_S = [(PSB[i], psn[i]) for i in range(4)]
            NPS = 4
            POS = [[(PSB[4], psn[4]), (PSB[5], psn[5])], [(PSB[6], psn[6]), (PSB[7], psn[7])]]
            sctr = [0]
            pctr = [0]
            pending = []
            tctr = [0]

            def make_post(isd, h, t, PO, am, oi):
                tsl = slice(t * 512, (t + 1) * 512)
                segs = []
                if isd:
                    def seg1():
                        for m in range(2):
                            cp("dve", accB[m][:], accS[am + m][:], r=["aacc%d" % (am + m)], w=["aaccb%d" % m])
                            pts, pns = PS_S[sctr[0] % NPS]
                            sctr[0] += 1
                            mm1(pts[0:1, :], ones_bf[:, 0:1], accB[m][:], True, True, r=["ones_bf", "aaccb%d" % m], w=[pns])
                            rrm = rr if m == 0 else rr2
                            act(rrm[0:1, :], pts[0:1, :], AF.Ln, r=[pns], w=["arr%d" % m])
                            act(rrm[0:1, :], rrm[0:1, :], AF.Exp, r=["arr%d" % m], w=["arr%d" % m], scale=-1.0)

                    def seg2():
                        for m in range(2):
                            rrm = rr if m == 0 else rr2
                            pt, pn = PS_S[sctr[0] % NPS]
                            sctr[0] += 1
                            mm1(pt[:], ones_f[0:1, :], rrm[0:1, :], True, True, r=["ones_f", "arr%d" % m], w=[pn])
                            cp("act", Rb[m][:], pt[:], r=[pn], w=["aRb%d" % m])

                    def seg3():
                        tt("dve", t0[:], PO[0][0][:], Rb[0][:], ALU.mult, r=[PO[0][1], "aRb0"], w=["at0"])
                        tt("dve", t1[:], PO[1][0][:], Rb[1][:], ALU.mult, r=[PO[1][1], "aRb1"], w=["at1"])
                        stt("dve", t0[:], t1[:], lamc[:, 2:3], t0[:], ALU.mult, ALU.add, r=["at0", "at1", "alamc"], w=["at0"])
                        act(osq[:], t0[:], AF.Square, r=["at0"], w=["aosq"])

                    def seg4():
                        pt, pn = PS_S[sctr[0] % NPS]
                        sctr[0] += 1
                        mm1(pt[:], ones_bf[:], osq[:], True, True, r=["ones_bf", "aosq"], w=[pn])
                        rstd_from(rms[:], pt[:], 1.0 / 128.0, r=[pn], w=["arms"])
                        stt("dve", t1[:], t0[:], subg[:, 0:1], rms[:], ALU.mult, ALU.mult, r=["at0", "asubg", "arms"], w=["at1"])
                        ts("dve", oo[oi][:], t1[:], 1.0 - lam_init, None, ALU.mult, None, r=["at1"], w=["aoo%d" % oi])
                        dma("sp", OAT[h, :, tsl], oo[oi][:], r=["aoo%d" % oi], w=[], key="aoo%d" % oi)
                    segs = [seg1, seg2, seg3, seg4]
                else:
                    def seg1():
                        act(rr[64:65, :], PO[0][0][64:65, :], AF.Ln, r=[PO[0][1]], w=["arr0"])
                        act(rr[64:65, :], rr[64:65, :], AF.Exp, r=["arr0"], w=["arr0"], scale=-1.0)
                        pt, pn = PS_S[sctr[0] % NPS]
                        sctr[0] += 1
                        mm1(pt[0:64, :], ones_f[64:65, 0:64], rr[64:65, :], True, True, r=["ones_f", "arr0"], w=[pn])
                        cp("act", Rb[0][0:64, :], pt[0:64, :], r=[pn], w=["aRb0"])

                    def seg2():
                        tt("dve", oo[oi][0:64, :], PO[0][0][0:64, :], Rb[0][0:64, :], ALU.mult, r=[PO[0][1], "aRb0"], w=["aoo%d" % oi])
                        dma("sp", OBT[h, :, tsl], oo[oi][0:64, :], r=["aoo%d" % oi], w=[], key="aoo%d" % oi)
                    segs = [seg1, seg2]
                return segs

            for hh in range(16):
                isd = hh < 8
                h = hh % 8
                i = hh % 2
                if isd:
                    dma("sp", kdt[i][:], KD[h], r=[], w=["akd%d" % i], key="akd%d" % i)
                    dma("sp", vdt[i][:], VD[:, h * 128:(h + 1) * 128].rearrange("(b p) e -> p b e", p=128), r=[], w=["avd%d" % i], key="avd%d" % i)
                else:
                    pr, hp = h // 2, h % 2
                    dma("sp", kdt[i][0:64, :], KMN[pr, hp * 64:(hp + 1) * 64, :], r=[], w=["akd%d" % i], key="akd%d" % i)
                    dma("sp", kdt[i][64:80, :], KR[0], r=[], w=["akdr1_%d" % i], key="akdr1_%d" % i)
                    dma("sp", kdt[i][80:96, :], KR[1], r=[], w=["akdr2_%d" % i], key="akdr2_%d" % i)
                    dma("sp", vmt[i][:, :, 0:64], VM[:, h * 64:(h + 1) * 64].rearrange("(b p) e -> p b e", p=128), r=[], w=["avm%d" % i], key="avm%d" % i)
                kn_ = ["akd%d" % i] + ([] if isd else ["akdr1_%d" % i, "akdr2_%d" % i])
                vn_ = ["avd%d" % i] if isd else ["avm%d" % i, "avm1_%d" % i]
                for t in range(NT):
                    tc = tctr[0]
                    tctr[0] += 1
                    qi = tc % 2
                    PO = POS[tc % 2]
                    am = 2 * (tc % 2)
                    tsl = slice(t * 512, (t + 1) * 512)
                    if isd:
                        dma("sp", qt[qi][:], QD[h, :, tsl], r=[], w=["aq%d" % qi], key="aq%d" % qi)
                        qn_ = ["aq%d" % qi]
                    else:
                        pr, hp = h // 2, h % 2
                        dma("sp", qt[qi][0:64, :], QMN[pr, hp * 64:(hp + 1) * 64, tsl], r=[], w=["aq%d" % qi], key="aq%d" % qi)
                        dma("sp", qt[qi][64:80, :], QR[0, h * 16:(h + 1) * 16, tsl], r=[], w=["aqr1_%d" % qi], key="aqr1_%d" % qi)
                        dma("sp", qt[qi][80:96, :], QR[1, h * 16:(h + 1) * 16, tsl], r=[], w=["aqr2_%d" % qi], key="aqr2_%d" % qi)
                        qn_ = ["aq%d" % qi, "aqr1_%d" % qi, "aqr2_%d" % qi]
                    nkb = 4 * t + 4
                    maps = (0, 1) if isd else (0,)

                    def stage1(kb):
                        delta = 512 * t - 128 * kb
                        c0 = max(0, -delta)
                        near = delta < 256
                        ksl = slice(kb * 128, (kb + 1) * 128)
                        res_ = []
                        for m in maps:
                            pt, pn = PS_S[sctr[0] % NPS]
                            sctr[0] += 1
                            pb = pT[pctr[0] % NPT]
                            pbn = "apT%d" % (pctr[0] % NPT)
                            pctr[0] += 1
                            rows = slice(m * 64, (m + 1) * 64) if isd else slice(0, 96)
                            mm1(pt[:, c0:512], kdt[i][rows, ksl], qt[qi][rows, c0:512], True, True, r=kn_ + qn_, w=[pn])
                            if (isd and near) or ((not isd) and delta <= 0):
                                si = sbc2[0] % 4
                                sbc2[0] += 1
                                sbt = sbias[si]
                                bi = h if isd else 8
                                tt("dve", sbt[:, c0:512], pt[:, c0:512], bstr[:, bi, delta + 384 + c0:delta + 384 + 512], ALU.add,
                                   r=[pn, "abst"], w=["asb%d" % si])
                                act(pb[:, c0:512], sbt[:, c0:512], AF.Exp, r=["asb%d" % si], w=[pbn])
                            elif isd:
                                act(pb[:, c0:512], pt[:, c0:512], AF.Exp, r=[pn, "tabb"], w=[pbn], bias=tabb[:, 248 + h:249 + h], scale=1.0)
                            else:
                                act(pb[:, c0:512], pt[:, c0:512], AF.Exp, r=[pn], w=[pbn])
                            res_.append((pb, pbn, c0))
                        return res_

                    def stage2(kb, res_):
                        first, last = kb == 0, kb == nkb - 1
                        for m, (pb, pbn, c0) in zip(maps, res_):
                            if isd:
                                mm1(PO[m][0][:, c0:512], vdt[i][:, kb, :], pb[:, c0:512], first, last, r=vn_ + [pbn], w=[PO[m][1]])
                                an = "aacc%d" % (am + m)
                                if first:
                                    cp("dve", accS[am + m][:], pb[:], r=[pbn], w=[an])
                                else:
                                    tt("dve", accS[am + m][:, c0:512], accS[am + m][:, c0:512], pb[:, c0:512], ALU.add, r=[pbn, an], w=[an])
                            else:
                                mm1(PO[0][0][0:65, c0:512], vmt[i][:, kb, :], pb[:, c0:512], first, last, r=vn_ + [pbn], w=[PO[0][1]])
                    q_ = [stage1(0), stage1(1)]
                    for kb in range(nkb):
                        if kb + 2 < nkb:
                            q_.append(stage1(kb + 2))
                        if pending:
                            pending.pop(0)()
                        stage2(kb, q_.pop(0))
                    while pending:
                        pending.pop(0)()
                    pending.extend(make_post(isd, h, t, PO, am, tc % 2))
            while pending:
                pending.pop(0)()
        S_.barrier()
        if upto == 'A':
            S_.emit(nc)
            es.close()
            return nc

        with ExitStack() as ph:
            alloc_psum(ph, 6, 2)
            wbd = sb(ph, "mwbd", [128, 8, D], BF16)
            wbm = sb(ph, "mwbm", [64, 8, D], BF16)
            wout = sb(ph, "mwout", [128, 8, D], BF16)
            lng = sb(ph, "mlng", [128, 2, D], F32)
            oa = [sb(ph, "moa%d" % i, [128, 8, 512], BF16) for i in range(2)]
            ob = [sb(ph, "mob%d" % i, [64, 8, 512], BF16) for i in range(2)]
            gs = [sb(ph, "mg%d" % i, [128, 16, 512], BF16) for i in range(2)]
            xs = [sb(ph, "mxs%d" % i, [128, 4, D], F32) for i in range(2)]
            mT = sb(ph, "mmT", [128, 8, 512], BF16)
            tA = [sb(ph, "mtA%d" % i, [128, 512], F32) for i in range(2)]
            tB = [sb(ph, "mtB%d" % i, [128, 512], F32) for i in range(2)]
            zs = [sb(ph, "mzs%d" % i, [128, D], F32) for i in range(2)]
            x1 = [sb(ph, "mx1%d" % i, [128, D], F32) for i in range(2)]
            x1b = sb(ph, "mx1b", [128, D], BF16)
            x1T = sb(ph, "mx1T", [128, 8, 512], BF16)
            st = sb(ph, "mst", [128, 12], F32)
            mv = sb(ph, "mmv", [128, 2], F32)
            rs = sb(ph, "mrs", [128, 1], F32)
            stg_state["tiles"] = [sb(ph, "mstg%d" % i, [128, 1024], F32) for i in range(2)]
            for k in range(8):
                wload(wbd[:, k, :], wbd_in[li, k * 128:(k + 1) * 128, :], 1024, "mwbd")
                wload(wbm[:, k, :], wbm_in[li, k * 64:(k + 1) * 64, :], 1024, "mwbm", parts=64)
                wload(wout[:, k, :], wout_in[li, k * 128:(k + 1) * 128, :], 1024, "mwout")
            dma("sp", lng[:].rearrange("p a d -> p (a d)"), lnm_in[li].rearrange("a d -> (a d)").partition_broadcast(128), r=[], w=["mlng"], key="mlng")

            def ldm(t):
                i = t % 2
                tsl = slice(t * 512, (t + 1) * 512)
                dma("sp", oa[i][:], OAT[:, :, tsl].rearrange("h p s -> p h s"), r=[], w=["moa%d" % i], key="moa%d" % i)
                dma("sp", ob[i][:], OBT[:, :, tsl].rearrange("h p s -> p h s"), r=[], w=["mob%d" % i], key="mob%d" % i)
                dma("sp", gs[i][:], GT[:, :, tsl].rearrange("g p s -> p g s"), r=[], w=["mg%d" % i], key="mg%d" % i)
                dma("sp", xs[i][:], x_src[tsl, :].rearrange("(j p) d -> p j d", p=128), r=[], w=["mxs%d" % i], key="mxs%d" % i)
            ldm(0)
            for t in range(NT):
                i = t % 2
                tsl = slice(t * 512, (t + 1) * 512)
                if t + 1 < NT:
                    ldm(t + 1)
                for db in range(8):
                    dsl = slice(db * 128, (db + 1) * 128)
                    ptA, pnA = nps()
                    mm(ptA[:], [(wbd[:, h, dsl], oa[i][:, h, :]) for h in range(8)], r=["mwbd", "moa%d" % i], w=[pnA])
                    ptB, pnB = nps()
                    mm(ptB[:], [(wbm[:, h, dsl], ob[i][:, h, :]) for h in range(8)], r=["mwbm", "mob%d" % i], w=[pnB])
                    a = db % 2
                    tt("dve", tA[a][:], ptA[:], gs[i][:, db, :], ALU.mult, r=[pnA, "mg%d" % i], w=["mtA%d" % a])
                    tt("dve", tB[a][:], ptB[:], gs[i][:, 8 + db, :], ALU.mult, r=[pnB, "mg%d" % i], w=["mtB%d" % a])
                    tt("pool", mT[:, db, :], tA[a][:], tB[a][:], ALU.add, r=["mtA%d" % a, "mtB%d" % a], w=["mmT%d" % db])
                mTn = ["mmT%d" % db for db in range(8)]
                for j in range(4):
                    a = j % 2
                    for hf in range(2):
                        hsl = slice(hf * 512, (hf + 1) * 512)
                        pt, pn = nps()
                        mm(pt[:], [(mT[:, db, j * 128:(j + 1) * 128], wout[:, db, hsl]) for db in range(8)], r=["mwout"] + mTn, w=[pn])
                        stt("dve", zs[a][:, hsl], xs[i][:, j, hsl], ALPHA, pt[:], ALU.mult, ALU.add, r=["mxs%d" % i, pn], w=["mzs%d_%d" % (a, hf)])
                    zn = ["mzs%d_0" % a, "mzs%d_1" % a]
                    layernorm(add, ts, tt, act, rstd_from, zs[a], x1[a], st, mv, rs, lng, zn, "mx1%d" % a, "m")
                    dma("sp", X1[t * 512 + j * 128:t * 512 + (j + 1) * 128, :], x1[a][:], r=["mx1%d" % a], w=[], key="mx1%d" % a)
                    cp("pool", x1b[:], x1[a][:], r=["mx1%d" % a], w=["mx1b"])
                    for kq in range(2):
                        def tr(e, kq=kq, PSTT=tuple(PSTT)):
                            ins = None
                            for k4 in range(4):
                                kb = kq * 4 + k4
                                ins = e.transpose(out=PSTT[kq][:, k4 * 128:(k4 + 1) * 128],
                                                  in_=x1b[:, kb * 128:(kb + 1) * 128], identity=ident[:])
                            return ins
                        add("pe", tr, r=["mx1b", "ident"], w=["pst%d" % kq])
                        add("act", lambda e, kq=kq, j=j, src=PSTT[kq]: e.copy(out=x1T[:, kq * 4:(kq + 1) * 4, j * 128:(j + 1) * 128],
                                                               in_=src[:, 0:512].rearrange("p (k s) -> p k s", k=4)),
                            r=["pst%d" % kq], w=["mx1T_%d_%d" % (j, kq)])
                dma("sp", X1T[:, :, tsl].rearrange("k p s -> p k s"), x1T[:], r=["mx1T_%d_%d" % (j, kq) for j in range(4) for kq in range(2)], w=[], key="mx1T")
        S_.barrier()
        if upto == 'M':
            S_.emit(nc)
            es.close()
            return nc

        with ExitStack() as ph:
            alloc_psum(ph, 6, 2)
            wpg = sb(ph, "ewpg", [128, 8, D], BF16)
            wpp = sb(ph, "ewpp", [128, 2, D], BF16)
            xT = [sb(ph, "exT%d" % i, [128, 8, 512], BF16) for i in range(2)]
            pp = [sb(ph, "epp%d" % i, [128, 4, 256], F32) for i in range(2)]
            ppb = sb(ph, "eppb", [128, 4, 256], BF16)
            ppT = sb(ph, "eppT", [128, 2, 512], BF16)
            sg = [sb(ph, "esg%d" % i, [128, 512], F32) for i in range(2)]
            ee = [sb(ph, "eee%d" % i, [128, 4, D], F32) for i in range(2)]
            stg_state["tiles"] = [sb(ph, "estg%d" % i, [128, 1024], F32) for i in range(2)]
            for k in range(8):
                wload(wpg[:, k, :], wpg_in[li, k * 128:(k + 1) * 128, :], 1024, "ewpg")
            for k in range(2):
                wload(wpp[:, k, :], wpp_in[li, k * 128:(k + 1) * 128, :], 1024, "ewpp")

            def lde(t):
                i = t % 2
                tsl = slice(t * 512, (t + 1) * 512)
                dma("sp", xT[i][:], X1T[:, :, tsl].rearrange("k p s -> p k s"), r=[], w=["exT%d" % i], key="exT%d" % i)
                dma("sp", pp[i][:], p_in[li, tsl, :].rearrange("(j p) c -> p j c", p=128), r=[], w=["epp%d" % i], key="epp%d" % i)
            lde(0)
            for t in range(NT):
                i = t % 2
                tsl = slice(t * 512, (t + 1) * 512)
                if t + 1 < NT:
                    lde(t + 1)
                cp("pool", ppb[:], pp[i][:], r=["epp%d" % i], w=["eppb"])
                for kq in range(2):
                    def tr(e, kq=kq, PSTT=tuple(PSTT)):
                        ins = None
                        for j in range(4):
                            ins = e.transpose(out=PSTT[kq][:, j * 128:(j + 1) * 128],
                                              in_=ppb[:, j, kq * 128:(kq + 1) * 128], identity=ident[:])
                        return ins
                    add("pe", tr, r=["eppb", "ident"], w=["pst%d" % kq])
                    cp("act", ppT[:, kq, :], PSTT[kq][:, 0:512], r=["pst%d" % kq], w=["eppT%d" % kq])
                for j in range(4):
                    for hf in range(2):
                        hsl = slice(hf * 512, (hf + 1) * 512)
                        a = hf
                        ptA, pnA = nps()
                        mm(ptA[:], [(xT[i][:, kb, j * 128:(j + 1) * 128], wpg[:, kb, hsl]) for kb in range(8)], r=["ewpg", "exT%d" % i], w=[pnA])
                        ptB, pnB = nps()
                        mm(ptB[:], [(ppT[:, kq, j * 128:(j + 1) * 128], wpp[:, kq, hsl]) for kq in range(2)], r=["ewpp", "eppT0", "eppT1"], w=[pnB])
                        act(sg[a][:], ptA[:], AF.Sigmoid, r=[pnA], w=["esg%d" % a])
                        tt("dve", ee[i][:, j, hsl], sg[a][:], ptB[:], ALU.mult, r=["esg%d" % a, pnB], w=["eee%d_%d_%d" % (i, j, hf)])
                dma("sp", EE[tsl, :].rearrange("(j p) d -> p j d", p=128), ee[i][:],
                    r=["eee%d_%d_%d" % (i, j, hf) for j in range(4) for hf in range(2)], w=[], key="eee%d" % i)
        S_.barrier()
        if upto == 'E':
            S_.emit(nc)
            es.close()
            return nc

        moe = (li % 2 == 1)
        lj = li // 2
        if moe:
            with ExitStack() as ph2:
                rwb = sb(ph2, "frw", [128, 8, D], F32)
                rx1 = [sb(ph2, "rx1%d" % i, [128, D], F32) for i in range(2)]
                lg = sb(ph2, "flg", [128, 8], F32)
                junk = sb(ph2, "fjunk", [128, D], F32)
                mx8 = sb(ph2, "fmx8", [128, 8], F32)
                msk = sb(ph2, "fmsk", [128, 8], F32)
                ex = sb(ph2, "fex", [128, 8], F32)
                den = sb(ph2, "fden", [128, 2], F32)
                dma("sp", rwb[:].rearrange("p a d -> p (a d)"), rw_in[lj].partition_broadcast(128), r=[], w=["frw"], key="frw")
                for sbi in range(NB):
                    a = sbi % 2
                    dma("sp", rx1[a][:], X1[sbi * 128:(sbi + 1) * 128, :], r=[], w=["rx1%d" % a], key="rx1%d" % a)
                    for ex_ in range(8):
                        add("dve", lambda e, a=a, ex_=ex_: e.scalar_tensor_tensor(out=junk[:], in0=rx1[a][:], scalar=1.0, in1=rwb[:, ex_, :], op0=ALU.mult, op1=ALU.mult, accum_out=lg[:, ex_:ex_ + 1]),
                            r=["rx1%d" % a, "frw", "flg", "fjunk"], w=["flg", "fjunk"])
                    add("dve", lambda e: e.max(out=mx8[:], in_=lg[:]), r=["flg"], w=["fmx8"])
                    ts("dve", msk[:], lg[:], mx8[:, 1:2], None, ALU.is_ge, None, r=["flg", "fmx8"], w=["fmsk"])
                    ts("dve", ex[:], lg[:], mx8[:, 0:1], None, ALU.subtract, None, r=["flg", "fmx8"], w=["fex"])
                    act(ex[:], ex[:], AF.Exp, r=["fex"], w=["fex"])
                    tt("dve", ex[:], ex[:], msk[:], ALU.mult, r=["fex", "fmsk"], w=["fex"])
                    add("dve", lambda e: e.reduce_sum(out=den[:, 0:1], in_=ex[:], axis=AX.X), r=["fex"], w=["fden"])
                    add("dve", lambda e: e.reciprocal(out=den[:, 1:2], in_=den[:, 0:1]), r=["fden"], w=["fden"])
                    ts("dve", gates[:, sbi, :], ex[:], den[:, 1:2], None, ALU.mult, None, r=["fex", "fden"], w=["gates"])
            S_.barrier()

        with ExitStack() as ph:
            alloc_psum(ph, 8, 0)
            TGl = TG if moe else min(S, 2 * TG)
            NGl = S // TGl
            NSUB = TGl // 128
            NCB = 7 if moe else 4
            Y = sb(ph, "fY", [128, NSUB, D], F32)
            xT = sb(ph, "fxT", [128, 8, TGl], BF16)
            wa = [sb(ph, "fwa%d" % i, [128, 8, NCB * 128], BF16) for i in range(2)]
            wb = [sb(ph, "fwb%d" % i, [128, 8, NCB * 128], BF16) for i in range(2)]
            wc = [sb(ph, "fwc%d" % i, [128, NCB, D], BF16) for i in range(2)]
            GTt = [sb(ph, "fG%d" % i, [128, NCB, 512], BF16) for i in range(2)]
            sl = [sb(ph, "fsl%d" % i, [128, 512], F32) for i in range(2)]
            x1 = [sb(ph, "fx1%d" % i_, [128, D], F32) for i_ in range(2)]
            zz = [sb(ph, "fzz%d" % i_, [128, D], F32) for i_ in range(2)]
            xo = [sb(ph, "fxo%d" % i_, [128, D], F32) for i_ in range(2)]
            lng = sb(ph, "flng", [128, 2, D], F32)
            st = sb(ph, "fst", [128, 12], F32)
            mv = sb(ph, "fmv", [128, 2], F32)
            rs = sb(ph, "frs", [128, 1], F32)
            stg_state["tiles"] = [sb(ph, "fstg%d" % i, [128, 1024], F32) for i in range(3)]
            dma("sp", lng[:].rearrange("p a d -> p (a d)"), lnf_in[li].rearrange("a d -> (a d)").partition_broadcast(128), r=[], w=["flng"], key="flng")
            FF = EXP_FF if moe else DENSE_FF
            nblk = FF // 128
            chunks = []
            b0 = 0
            while b0 < nblk:
                nb_ = min(NCB, nblk - b0)
                if nblk - b0 - nb_ in (1, 2) and nb_ > 3:
                    nb_ -= 2
                chunks.append((b0, nb_))
                b0 += nb_
            nexp = 8 if moe else 1
            seq = [(g, ex_, c) for g in range(NGl) for ex_ in range(nexp) for c in range(len(chunks))]

            def wjobs(k):
                g, ex_, c = seq[k]
                i = k % 2
                b0, nb_ = chunks[c]
                if moe:
                    s1, s3, s2 = ew1_in[lj, ex_], ew3_in[lj, ex_], ew2_in[lj, ex_]
                else:
                    s1, s3, s2 = dw1_in[lj], dw3_in[lj], dw2_in[lj]
                jobs = []
                wdt = nb_ * 128
                for kb in range(8):
                    jobs.append(lambda kb=kb: wload(wa[i][:, kb, 0:wdt], s1[kb * 128:(kb + 1) * 128, b0 * 128:b0 * 128 + wdt], wdt, "fwa%d_%d" % (i, kb)))
                    jobs.append(lambda kb=kb: wload(wb[i][:, kb, 0:wdt], s3[kb * 128:(kb + 1) * 128, b0 * 128:b0 * 128 + wdt], wdt, "fwb%d_%d" % (i, kb)))
                for jb in range(nb_):
                    jobs.append(lambda jb=jb: wload(wc[i][:, jb, :], s2[(b0 + jb) * 128:(b0 + jb + 1) * 128, :], 1024, "fwc%d_%d" % (i, jb)))
                return jobs
            for jfn in wjobs(0):
                jfn()
            gctr = [0]
            for k, (g, ex_, c) in enumerate(seq):
                i = k % 2
                b0, nb_ = chunks[c]
                gsl = slice(g * TGl, (g + 1) * TGl)
                if ex_ == 0 and c == 0 and g == 0:
                    dma("sp", xT[:], X1T[:, :, gsl].rearrange("k p s -> p k s"), r=[], w=["fxT"], key="fxT")
                    dma("sp", Y[:], EE[gsl, :].rearrange("(j p) d -> p j d", p=128), r=[], w=["fY%d" % s_ for s_ in range(NSUB)], key="fY")
                pend = wjobs(k + 1) if k + 1 < len(seq) else []
                pend.reverse()

                def pump(n=1):
                    for _ in range(n):
                        if pend:
                            pend.pop()()
                wan = ["fwa%d_%d" % (i, kb) for kb in range(8)]
                wbn = ["fwb%d_%d" % (i, kb) for kb in range(8)]
                for tl in range(TGl // 512):
                    gi = gctr[0] % 2
                    gctr[0] += 1
                    tsl = slice(tl * 512, (tl + 1) * 512)
                    for jb in range(nb_):
                        fsl = slice(jb * 128, (jb + 1) * 128)
                        pump(1)
                        pt1, pn1 = nps()
                        mm(pt1[:], [(wa[i][:, kb, fsl], xT[:, kb, tsl]) for kb in range(8)], r=wan + ["fxT"], w=[pn1])
                        pt3, pn3 = nps()
                        mm(pt3[:], [(wb[i][:, kb, fsl], xT[:, kb, tsl]) for kb in range(8)], r=wbn + ["fxT"], w=[pn3])
                        a = jb % 2
                        act(sl[a][:], pt1[:], AF.Silu, r=[pn1], w=["fsl%d" % a])
                        tt("dve", GTt[gi][:, jb, :], sl[a][:], pt3[:], ALU.mult, r=["fsl%d" % a, pn3], w=["fG%d_%d" % (gi, jb)])
                    Gn = ["fG%d_%d" % (gi, jb) for jb in range(nb_)]
                    wcn = ["fwc%d_%d" % (i, jb) for jb in range(nb_)]
                    for j4 in range(4):
                        sub = tl * 4 + j4
                        sbi = g * NSUB + sub
                        for hf in range(2):
                            hsl = slice(hf * 512, (hf + 1) * 512)
                            pump(1)
                            pt, pn = nps()
                            mm(pt[:], [(GTt[gi][:, jb, j4 * 128:(j4 + 1) * 128], wc[i][:, jb, hsl]) for jb in range(nb_)], r=wcn + Gn, w=[pn])
                            gcol = gates[:, sbi, ex_:ex_ + 1] if moe else ccol[:, 2:3]
                            stt("dve", Y[:, sub, hsl], pt[:], gcol, Y[:, sub, hsl], ALU.mult, ALU.add,
                                r=[pn, "fY%d" % sub, "gates", "ccol"], w=["fY%d" % sub])
                pump(100)
                if ex_ == nexp - 1 and c == len(chunks) - 1:
                    nxt_g = g + 1 < NGl
                    if nxt_g:
                        ngsl = slice((g + 1) * TGl, (g + 2) * TGl)
                        dma("sp", xT[:], X1T[:, :, ngsl].rearrange("k p s -> p k s"), r=[], w=["fxT"], key="fxT")
                    for sub in range(NSUB):
                        a = sub % 2
                        r0 = g * TGl + sub * 128
                        dma("sp", x1[a][:], X1[r0:r0 + 128, :], r=[], w=["fx1%d" % a], key="fx1%d" % a)
                        stt("dve", zz[a][:], x1[a][:], ALPHA, Y[:, sub, :], ALU.mult, ALU.add, r=["fx1%d" % a, "fY%d" % sub], w=["fzz%d" % a])
                        if nxt_g:
                            n0 = (g + 1) * TGl + sub * 128
                            dma("sp", Y[:, sub, :], EE[n0:n0 + 128, :], r=[], w=["fY%d" % sub], key="fYs%d" % sub)
                        layernorm(add, ts, tt, act, rstd_from, zz[a], xo[a], st, mv, rs, lng, ["fzz%d" % a], "fxo%d" % a, "f")
                        dma("sp", x_dst[r0:r0 + 128, :], xo[a][:], r=["fxo%d" % a], w=[], key="fxo%d" % a)
        S_.barrier()

    S_.emit(nc)
    es.close()
    return nc


def layernorm(add, ts, tt, act, rstd_from, z, xo, st, mv, rs, lng, zn, xon, pfx):
    stn, mvn, rsn = pfx + "st", pfx + "mv", pfx + "rs"
    add("dve", lambda e: e.bn_stats(out=st[:, 0:6], in_=z[:, 0:512]), r=zn, w=[stn])
    add("dve", lambda e: e.bn_stats(out=st[:, 6:12], in_=z[:, 512:1024]), r=zn + [stn], w=[stn])
    add("dve", lambda e: e.bn_aggr(out=mv[:], in_=st[:]), r=[stn], w=[mvn])
    rstd_from(rs[:], mv[:, 1:2], 1.0, r=[mvn], w=[rsn])
    ts("dve", xo[:], z[:], mv[:, 0:1], rs[:, 0:1], ALU.subtract, ALU.mult, r=zn + [mvn, rsn], w=[xon])
    tt("dve", xo[:], xo[:], lng[:, 0, :], ALU.mult, r=[xon, pfx + "lng"], w=[xon])
    tt("pool", xo[:], xo[:], lng[:, 1, :], ALU.add, r=[xon, pfx + "lng"], w=[xon])


def prep_inputs(inp, b, S, L):
    f = np.float32
    def c(a):
        return np.ascontiguousarray(a)
    NM = L // 2
    wuq = np.asarray(inp["w_uq"])[:L].reshape(L, 384, 8, 96)
    wuq_p = np.concatenate([wuq[..., 0:64].reshape(L, 384, 512), wuq[..., 64:80].reshape(L, 384, 128),
                            wuq[..., 80:96].reshape(L, 384, 128)], axis=-1)
    wukv = np.asarray(inp["w_ukv"])[:L].reshape(L, 256, 8, 128)
    wukv_p = np.concatenate([wukv[..., 0:64].reshape(L, 256, 512), wukv[..., 64:128].reshape(L, 256, 512)], axis=-1)
    lam = np.concatenate([np.asarray(inp[k])[:L] for k in ("lambda_q1", "lambda_k1", "lambda_q2", "lambda_k2")], axis=-1).reshape(L, 1, 256)
    d = {
        "x": c(np.asarray(inp["x"])[b, :S]),
        "p": c(np.asarray(inp["p"])[:L, b, :S]),
        "pos": c(np.asarray(inp["positions"])[b, :S].reshape(1, S).astype(np.int32)),
        "tab": c(np.asarray(inp["rel_bias_table"]).reshape(1, 256)),
        "w_in": c(np.asarray(inp["w_in"])[:L]),
        "bg": c(np.asarray(inp["b_gate"])[:L].reshape(L, 16, 128).transpose(0, 2, 1)),
        "lam": c(lam),
        "subg": c(np.asarray(inp["diff_subln_g"])[:L].reshape(L, 128, 1)),
        "gq": c(np.asarray(inp["mla_q_norm_g"])[:L].reshape(L, 3, 128).transpose(0, 2, 1)),
        "wuq": c(wuq_p),
        "gkv": c(np.asarray(inp["mla_kv_norm_g"])[:L].reshape(L, 2, 128).transpose(0, 2, 1)),
        "wukv": c(wukv_p),
        "wbd": c(np.asarray(inp["w_branch_diff"])[:L]),
        "wbm": c(np.asarray(inp["w_branch_mla"])[:L]),
        "wout": c(np.asarray(inp["w_out"])[:L]),
        "lnm": c(np.stack([np.asarray(inp["ln_mix_g"])[:L], np.asarray(inp["ln_mix_b"])[:L]], axis=1)),
        "dw1": c(np.asarray(inp["dense_w1"])[:(L + 1) // 2]),
        "dw3": c(np.asarray(inp["dense_w3"])[:(L + 1) // 2]),
        "dw2": c(np.asarray(inp["dense_w2"])[:(L + 1) // 2]),
        "rw": c(np.asarray(inp["router_w"])[:max(NM, 1)].transpose(0, 2, 1).reshape(max(NM, 1), 1, 8 * 1024)),
        "ew1": c(np.asarray(inp["expert_w1"])[:max(NM, 1)]),
        "ew3": c(np.asarray(inp["expert_w3"])[:max(NM, 1)]),
        "ew2": c(np.asarray(inp["expert_w2"])[:max(NM, 1)]),
        "wpg": c(np.asarray(inp["w_ple_gate"])[:L]),
        "wpp": c(np.asarray(inp["w_ple_proj"])[:L]),
        "lnf": c(np.stack([np.asarray(inp["ln_ffn_g"])[:L], np.asarray(inp["ln_ffn_b"])[:L]], axis=1)),
    }
    cst = np.zeros((128, 4), f)
    half = 16
    invf = (np.float32(10000.0) ** (-np.arange(half, dtype=f) / np.float32(half))).astype(f)
    cst[:, 0] = np.tile(invf, 8)
    d["cst"] = cst
    return {k: np.ascontiguousarray(v) for k, v in d.items()}


_NC_CACHE = {}


def kernel(**inputs):
    S, L, NCORES = 4096, 4, 8
    if "nc" not in _NC_CACHE:
        _NC_CACHE["nc"] = build(S, L, TG=1024)
    nc = _NC_CACHE["nc"]
    in_maps = [prep_inputs(inputs, b, S, L) for b in range(NCORES)]
    res = run_bass_kernel_spmd(nc, in_maps, core_ids=list(range(NCORES)))
    return np.stack([np.asarray(r["out"], dtype=np.float32) for r in res.results], axis=0)
```

```python
import os
import math
from contextlib import ExitStack
import numpy as np
import concourse.bass as bass
import concourse.mybir as mybir
from concourse.bass_utils import run_bass_kernel_spmd

F32 = mybir.dt.float32
BF16 = mybir.dt.bfloat16
I32 = mybir.dt.int32
AF = mybir.ActivationFunctionType
ALU = mybir.AluOpType
AX = mybir.AxisListType

ENGS = ("pe", "act", "dve", "pool", "sp")
SEM_MAX = 20000
NSLOT = 84


class Op:
    __slots__ = ("eng", "fn", "deps", "dma", "slot", "idx", "sig", "ordn", "barrier")

    def __init__(self, eng, fn, dma):
        self.eng, self.fn, self.dma = eng, fn, dma
        self.deps = []
        self.sig = False
        self.ordn = None
        self.slot = None
        self.barrier = False


class Sched:
    def __init__(self):
        self.ops = {e: [] for e in ENGS}
        self.state = {}
        self.slot_of = {}
        self.slot_cnt = [0] * NSLOT
        self.slot_last = [None] * NSLOT
        self.nslot_used = 0
        self.max_slots = 0

    def add(self, eng, fn, r=(), w=(), dma=None):
        op = Op(eng, fn, dma)
        w = list(w) + [b for b in r if b.startswith('ps')]
        r = [b for b in r if not b.startswith('ps')]
        deps = []
        for b in r:
            st = self.state.setdefault(b, [None, []])
            if st[0] is not None:
                deps.append(st[0])
        for b in w:
            st = self.state.setdefault(b, [None, []])
            if st[0] is not None:
                deps.append(st[0])
            deps.extend(st[1])
        seen = set()
        for d in deps:
            if id(d) in seen:
                continue
            seen.add(id(d))
            if d.dma is None and op.dma is None and d.eng == "pe" and eng == "pe":
                continue
            op.deps.append(d)
            if d.dma is None:
                d.sig = True
        if dma is not None:
            if dma not in self.slot_of:
                assert self.nslot_used < NSLOT, "out of dma semaphore slots"
                self.slot_of[dma] = self.nslot_used
                self.nslot_used += 1
                self.max_slots = max(self.max_slots, self.nslot_used)
            sl = self.slot_of[dma]
            self.slot_cnt[sl] += 1
            assert self.slot_cnt[sl] * 16 < 32000, "dma sem count too large: %s" % dma
            op.slot = sl
            op.ordn = self.slot_cnt[sl]
            self.slot_last[sl] = op
        op.idx = len(self.ops[eng])
        self.ops[eng].append(op)
        for b in r:
            self.state[b][1].append(op)
        for b in w:
            self.state[b] = [op, []]
        return op

    def barrier(self):
        used = self.nslot_used
        c = Op("sp", None, None)
        c.barrier = True
        for e in ENGS:
            if e == "sp":
                continue
            for op in reversed(self.ops[e]):
                if op.dma is None:
                    if not op.barrier:
                        c.deps.append(op)
                        op.sig = True
                    break
        for sl in range(used):
            if self.slot_last[sl] is not None:
                c.deps.append(self.slot_last[sl])
        c.slot = used
        c.sig = True
        c.idx = len(self.ops["sp"])
        self.ops["sp"].append(c)
        for e in ENGS:
            if e == "sp":
                continue
            d = Op(e, None, None)
            d.barrier = True
            d.deps.append(c)
            d.idx = len(self.ops[e])
            self.ops[e].append(d)
        self.state = {}
        self.slot_of = {}
        self.slot_cnt = [0] * NSLOT
        self.slot_last = [None] * NSLOT
        self.nslot_used = 0

    def emit(self, nc):
        from contextlib import ExitStack
        self.barrier()
        nsig = {}
        for e in ENGS:
            n = 0
            for op in self.ops[e]:
                if op.dma is None and op.sig:
                    op.ordn = n
                    n += 1
            nsig[e] = n
        with ExitStack() as es:
            esem = {}
            for e in ENGS:
                k = max(1, (nsig[e] + SEM_MAX - 1) // SEM_MAX)
                esem[e] = [es.enter_context(nc.semaphore("s_%s_%d" % (e, i))) for i in range(k)]
            dsem = [es.enter_context(nc.semaphore("d_%d" % i)) for i in range(self.max_slots)]
            block = es.enter_context(nc.Block())

            def target(d):
                if d.dma is not None:
                    return dsem[d.slot], 16 * d.ordn
                return esem[d.eng][d.ordn // SEM_MAX], d.ordn % SEM_MAX + 1

            def run(e, eng):
                waited = {}
                for op in self.ops[e]:
                    for d in op.deps:
                        s, v = target(d)
                        if waited.get(s.name, 0) >= v:
                            continue
                        waited[s.name] = v
                        eng.wait_ge(s, v)
                    if op.barrier:
                        if e == "sp":
                            for sl in range(op.slot):
                                eng.sem_clear(dsem[sl])
                            ins = eng.nop()
                        else:
                            ins = eng.nop()
                        for k in list(waited.keys()):
                            if k.startswith("d_"):
                                del waited[k]
                    else:
                        ins = op.fn(eng)
                    if op.dma is not None:
                        ins.then_inc(dsem[op.slot], 16)
                    elif op.sig:
                        ins.then_inc(esem[e][op.ordn // SEM_MAX], 1)

            @block.tensor
            def _(eng):
                run("pe", eng)

            @block.scalar
            def _(eng):
                run("act", eng)

            @block.vector
            def _(eng):
                run("dve", eng)

            @block.gpsimd
            def _(eng):
                run("pool", eng)

            @block.sync
            def _(eng):
                run("sp", eng)


D = 1024
NQ = 384
NKV = 256
COLQ, COLK, COLV, COLCQ, COLCKV, COLKR, COLG = 0, 1024, 2048, 3072, 3456, 3712, 3744
INC = 5792
DENSE_FF = 2816
EXP_FF = 3584
NEG = -30000.0
DEPTH_TOTAL = 4
ALPHA = (2.0 * DEPTH_TOTAL) ** 0.25
EPS = 1e-5


def t5_thresholds():
    n = np.arange(0, 4096, dtype=np.int32)
    nf = np.maximum(n, 1).astype(np.float32)
    large = 16 + ((np.log(nf / np.float32(16)) / np.float32(math.log(128 / 16))) * np.float32(16)).astype(np.int32)
    large = np.minimum(large, 31)
    bucket = np.where(n < 16, n, large)
    th = []
    for j in range(1, 32):
        th.append(int(np.argmax(bucket >= j)))
    return th


def build(S, L, TG=1024, dbg=(), upto=None):
    nc = bass.Bass("TRN2", target_bir_lowering=False)
    NT = S // 512
    NB = S // 128
    NG = S // TG
    ND = (L + 1) // 2
    NM = L // 2
    es = ExitStack()
    S_ = Sched()
    add = S_.add

    def din(name, shape, dt=F32):
        return nc.dram_tensor(name, list(shape), dt, kind="ExternalInput").ap()

    def dscr(name, shape, dt):
        kind = "ExternalOutput" if name in dbg else "Internal"
        return nc.dram_tensor(name, list(shape), dt, kind=kind).ap()

    x_in = din("x", [S, D])
    p_in = din("p", [L, S, 256])
    pos_in = din("pos", [1, S], I32)
    tab_in = din("tab", [1, 256])
    w_in = din("w_in", [L, D, INC])
    bg_in = din("bg", [L, 128, 16])
    lam_in = din("lam", [L, 1, 256])
    subg_in = din("subg", [L, 128, 1])
    gq_in = din("gq", [L, 128, 3])
    wuq_in = din("wuq", [L, NQ, 768])
    gkv_in = din("gkv", [L, 128, 2])
    wukv_in = din("wukv", [L, NKV, 1024])
    wbd_in = din("wbd", [L, 1024, D])
    wbm_in = din("wbm", [L, 512, D])
    wout_in = din("wout", [L, D, D])
    lnm_in = din("lnm", [L, 2, D])
    dw1_in = din("dw1", [ND, D, DENSE_FF])
    dw3_in = din("dw3", [ND, D, DENSE_FF])
    dw2_in = din("dw2", [ND, DENSE_FF, D])
    rw_in = din("rw", [max(NM, 1), 1, 8 * D])
    ew1_in = din("ew1", [max(NM, 1), 8, D, EXP_FF])
    ew3_in = din("ew3", [max(NM, 1), 8, D, EXP_FF])
    ew2_in = din("ew2", [max(NM, 1), 8, EXP_FF, D])
    wpg_in = din("wpg", [L, D, D])
    wpp_in = din("wpp", [L, 256, D])
    lnf_in = din("lnf", [L, 2, D])
    cst_in = din("cst", [128, 4])
    out = nc.dram_tensor("out", [S, D], F32, kind="ExternalOutput").ap()

    XT = dscr("XT", [8, 128, S], BF16)
    QD = dscr("QD", [8, 128, S], BF16)
    KD = dscr("KD", [8, 128, S], BF16)
    VD = dscr("VD", [S, 1024], BF16)
    GT = dscr("GT", [16, 128, S], BF16)
    QMN = dscr("QMN", [4, 128, S], BF16)
    KMN = dscr("KMN", [4, 128, S], BF16)
    QR = dscr("QR", [2, 128, S], BF16)
    KR = dscr("KR", [2, 16, S], BF16)
    VM = dscr("VM", [S, 512], BF16)
    OAT = dscr("OAT", [8, 128, S], BF16)
    OBT = dscr("OBT", [8, 64, S], BF16)
    X1 = dscr("X1", [S, D], F32)
    X1T = dscr("X1T", [8, 128, S], BF16)
    EE = dscr("EE", [S, D], F32)
    XR = dscr("XR", [S, D], F32)
    CS = dscr("CS", [2, 128, S], F32)
    BST = dscr("BST", [9, 128, 1024], F32)

    sbc = [0]

    def sb(stack, name, shape, dt):
        sbc[0] += 1
        return stack.enter_context(nc.sbuf_tensor("sb_%s_%d" % (name, sbc[0]), list(shape), dt))

    ident = sb(es, "ident", [128, 128], BF16)
    ones_bf = sb(es, "ones_bf", [128, 128], BF16)
    ones_f = sb(es, "ones_f", [128, 128], F32)
    cst = sb(es, "cst", [128, 4], F32)
    ccol = sb(es, "ccol", [128, 8], F32)
    tabb = sb(es, "tabb", [128, 256], F32)
    gates = sb(es, "gates", [128, NB, 8], F32)
    PSB = []
    PSTT = []
    psn = ["ps%d" % i for i in range(8)]
    psc = [0]

    def alloc_psum(ph, nf, nbf):
        psc[0] += 1
        PSB[:] = [ph.enter_context(nc.psum_tensor("psb%d_%d" % (i, psc[0]), [128, 512], F32)) for i in range(nf)]
        PSTT[:] = [ph.enter_context(nc.psum_tensor("pst%d_%d" % (i, psc[0]), [128, 1024], BF16)) for i in range(nbf)]
    ring = [0]

    def nps():
        i = ring[0] % len(PSB)
        ring[0] += 1
        return PSB[i], psn[i]

    def mm(out_ap, pairs, r, w):
        pairs = list(pairs)

        def fn(e):
            ins = None
            n = len(pairs)
            for i, (l, rh) in enumerate(pairs):
                ins = e.matmul(out_ap, lhsT=l, rhs=rh, start=(i == 0), stop=(i == n - 1))
            return ins
        add("pe", fn, r=r, w=w)

    def mm1(out_ap, l, rh, start, stop, r, w):
        add("pe", lambda e: e.matmul(out_ap, lhsT=l, rhs=rh, start=start, stop=stop), r=r, w=w)

    def dma(eng, out_ap, in_ap, r, w, key):
        add(eng, lambda e: e.dma_start(out=out_ap, in_=in_ap), r=r, w=w, dma=key)

    def act(out_ap, in_ap, func, r, w, bias=None, scale=None):
        kw = {}
        if bias is not None:
            kw["bias"] = bias
        if scale is not None:
            kw["scale"] = scale
        add("act", lambda e: e.activation(out=out_ap, in_=in_ap, func=func, **kw), r=r, w=w)

    def tt(eng, out_ap, a, b, op, r, w):
        add(eng, lambda e: e.tensor_tensor(out=out_ap, in0=a, in1=b, op=op), r=r, w=w)

    def ts(eng, out_ap, a, s1, s2, op0, op1, r, w):
        if op1 is None:
            add(eng, lambda e: e.tensor_scalar(out=out_ap, in0=a, scalar1=s1, scalar2=None, op0=op0), r=r, w=w)
        else:
            add(eng, lambda e: e.tensor_scalar(out=out_ap, in0=a, scalar1=s1, scalar2=s2, op0=op0, op1=op1), r=r, w=w)

    def stt(eng, out_ap, a, sc, b, op0, op1, r, w):
        add(eng, lambda e: e.scalar_tensor_tensor(out=out_ap, in0=a, scalar=sc, in1=b, op0=op0, op1=op1), r=r, w=w)

    def cp(eng, out_ap, in_ap, r, w):
        if eng == "act":
            add("act", lambda e: e.copy(out=out_ap, in_=in_ap), r=r, w=w)
        else:
            add(eng, lambda e: e.tensor_copy(out=out_ap, in_=in_ap), r=r, w=w)

    STGW = 2048
    stg_state = {"tiles": None, "n": 0}
    cast_rot = ("pool", "act", "pool", "dve")

    def wload(dst_ap, src_ap, width, wname, parts=128):
        tiles = stg_state["tiles"]
        k = stg_state["n"]
        stg_state["n"] += 1
        i = k % len(tiles)
        st_ = tiles[i]
        dma("sp", st_[0:parts, 0:width], src_ap, r=[], w=["stg%d" % i], key="stg%d" % i)
        cp(cast_rot[k % 4], dst_ap, st_[0:parts, 0:width], r=["stg%d" % i], w=[wname])

    def rstd_from(out_ap, in_ap, scale, r, w):
        act(out_ap, in_ap, AF.Ln, r=r + ["ccol"], w=w, bias=ccol[:, 0:1], scale=scale)
        act(out_ap, out_ap, AF.Exp, r=w, w=w, scale=-0.5)

    add("pool", lambda e: e.memset(ident[:], 0.0), w=["ident"])
    add("pool", lambda e: e.affine_select(out=ident[:], in_=ident[:], compare_op=ALU.not_equal, fill=1.0,
                                          base=0, pattern=[[-1, 128]], channel_multiplier=1),
        r=["ident"], w=["ident"])
    add("dve", lambda e: e.memset(ones_bf[:], 1.0), w=["ones_bf"])
    add("dve", lambda e: e.memset(ones_f[:], 1.0), w=["ones_f"])
    add("dve", lambda e: e.memset(ccol[:, 0:1], EPS), w=["ccol"])
    add("dve", lambda e: e.memset(ccol[:, 1:2], -math.pi), r=["ccol"], w=["ccol"])
    add("dve", lambda e: e.memset(ccol[:, 2:3], 1.0), r=["ccol"], w=["ccol"])
    add("dve", lambda e: e.memset(ccol[:, 3:4], 0.0), r=["ccol"], w=["ccol"])
    dma("sp", cst[:], cst_in, r=[], w=["cst"], key="cst")
    dma("sp", tabb[:], tab_in.partition_broadcast(128), r=[], w=["tabb"], key="tabb")

    with ExitStack() as ph:
        posi = sb(ph, "posi", [128, S], I32)
        posf = sb(ph, "posf", [128, S], F32)
        ang = sb(ph, "ang", [128, S], F32)
        tmpc = sb(ph, "tmpc", [128, S], F32)
        dma("sp", posi[:], pos_in.partition_broadcast(128), r=[], w=["posi"], key="posi")
        cp("dve", posf[:], posi[:], r=["posi"], w=["posf"])
        ts("dve", ang[:], posf[:], cst[:, 0:1], None, ALU.mult, None, r=["posf", "cst"], w=["ang"])
        C1 = 6.28125
        C2 = 2.0 * math.pi - C1
        PI_IN = 3.1415925
        indc = sb(ph, "indc", [128, S], F32)
        ts("dve", tmpc[:], ang[:], 1.0 / (2.0 * math.pi), None, ALU.mult, None, r=["ang"], w=["tmpc"])
        cp("dve", posi[:], tmpc[:], r=["tmpc", "posf"], w=["posi"])
        cp("dve", posf[:], posi[:], r=["posi", "ang"], w=["posf"])
        stt("dve", tmpc[:], posf[:], -C1, ang[:], ALU.mult, ALU.add, r=["posf", "ang"], w=["tmpc"])
        stt("dve", tmpc[:], posf[:], -C2, tmpc[:], ALU.mult, ALU.add, r=["posf", "tmpc"], w=["tmpc"])

        def wrap(tn, t):
            ts("dve", indc[:], t[:], math.pi, None, ALU.is_gt, None, r=[tn], w=["indc"])
            stt("dve", t[:], indc[:], -2.0 * math.pi, t[:], ALU.mult, ALU.add, r=["indc", tn], w=[tn])
            ts("dve", indc[:], t[:], -math.pi, None, ALU.is_lt, None, r=[tn], w=["indc"])
            stt("dve", t[:], indc[:], 2.0 * math.pi, t[:], ALU.mult, ALU.add, r=["indc", tn], w=[tn])
            ts("dve", t[:], t[:], PI_IN, -PI_IN, ALU.min, ALU.max, r=[tn], w=[tn])
        wrap("tmpc", tmpc)
        ts("dve", ang[:], tmpc[:], 0.5 * math.pi, None, ALU.add, None, r=["tmpc"], w=["ang"])
        wrap("ang", ang)
        act(ang[:], ang[:], AF.Sin, r=["ang"], w=["ang"])
        dma("sp", CS[0], ang[:], r=["ang"], w=[], key="ang")
        act(tmpc[:], tmpc[:], AF.Sin, r=["tmpc"], w=["tmpc"])
        dma("sp", CS[1], tmpc[:], r=["tmpc"], w=[], key="tmpc")
    S_.barrier()
    with ExitStack() as ph:
        nmi = sb(ph, "nmi", [128, 1024], I32)
        nmat = sb(ph, "nmat", [128, 1024], F32)
        ind = sb(ph, "ind", [128, 1024], F32)
        mstrip = sb(ph, "mstrip", [128, 1024], F32)
        dtab = sb(ph, "dtab", [128, 256], F32)
        bst = [sb(ph, "bst%d" % h, [128, 1024], F32) for h in range(8)]
        add("pool", lambda e: e.iota(nmi[:], pattern=[[1, 1024]], base=-384, channel_multiplier=-1), w=["nmi"])
        cp("dve", nmat[:], nmi[:], r=["nmi"], w=["nmat"])
        tt("dve", dtab[:, 8:256], tabb[:, 8:256], tabb[:, 0:248], ALU.subtract, r=["tabb"], w=["dtab"])
        ts("dve", ind[:], nmat[:], 0.0, None, ALU.is_ge, None, r=["nmat"], w=["ind"])
        ts("dve", mstrip[:], ind[:], 1.0, -NEG, ALU.subtract, ALU.mult, r=["ind"], w=["mstrip"])
        for h in range(8):
            ts("pool", bst[h][:], mstrip[:], tabb[:, h:h + 1], None, ALU.add, None, r=["mstrip", "tabb"], w=["bst%d" % h])
        th = t5_thresholds()
        for j in range(1, 32):
            ts("dve", ind[:], nmat[:], float(th[j - 1]), None, ALU.is_ge, None, r=["nmat"], w=["ind"])
            for h in range(8):
                eng = "dve"
                stt(eng, bst[h][:], ind[:], dtab[:, j * 8 + h:j * 8 + h + 1], bst[h][:], ALU.mult, ALU.add,
                    r=["ind", "dtab", "bst%d" % h], w=["bst%d" % h])
        for h in range(8):
            dma("sp", BST[h], bst[h][:], r=["bst%d" % h], w=[], key="bst%d" % h)
        dma("sp", BST[8], mstrip[:], r=["mstrip"], w=[], key="mstrip")
    S_.barrier()
    if upto == 'C':
        S_.emit(nc)
        es.close()
        return nc

    for li in range(L):
        lam_init = 0.8 - 0.6 * math.exp(-0.3 * li)
        x_src = x_in if li == 0 else XR
        x_dst = out if li == L - 1 else XR

        with ExitStack() as ph:
            alloc_psum(ph, 6, 2)
            w1 = sb(ph, "p1w", [128, 8, 3072], BF16)
            xs = [sb(ph, "p1xs%d" % i, [128, 4, 1024], F32) for i in range(2)]
            xb = sb(ph, "p1xb", [128, 4, 1024], BF16)
            xT = [sb(ph, "p1xT%d" % i, [128, 8, 512], BF16) for i in range(2)]
            qd = [sb(ph, "p1qd%d" % i, [128, 8, 512], BF16) for i in range(2)]
            kd = [sb(ph, "p1kd%d" % i, [128, 8, 512], BF16) for i in range(2)]
            vd = [sb(ph, "p1vd%d" % i, [128, 4, 1024], BF16) for i in range(2)]
            stg_state["tiles"] = [sb(ph, "p1stg%d" % i, [128, STGW], F32) for i in range(3)]
            for kb in range(8):
                for c3 in range(0, 3072, STGW):
                    wd = min(STGW, 3072 - c3)
                    wload(w1[:, kb, c3:c3 + wd], w_in[li, kb * 128:(kb + 1) * 128, c3:c3 + wd], wd, "p1w%d_%d" % (kb, c3))
            wn = ["p1w%d_%d" % (kb, c3) for kb in range(8) for c3 in range(0, 3072, STGW)]

            def ldx(t):
                i = t % 2
                dma("sp", xs[i][:], x_src[t * 512:(t + 1) * 512, :].rearrange("(j p) d -> p j d", p=128),
                    r=[], w=["p1xs%d" % i], key="p1xs%d" % i)
            ldx(0)
            for t in range(NT):
                i = t % 2
                if t + 1 < NT:
                    ldx(t + 1)
                import os
                cut = int(os.environ.get("P1CUT", "99"))
                if cut < 1:
                    continue
                for j in range(4):
                    cp("dve" if j % 2 == 0 else "pool", xb[:, j, :], xs[i][:, j, :], r=["p1xs%d" % i], w=["p1xb%d" % j])
                if cut < 2:
                    continue
                for kb in range(8):
                    def tr(e, kb=kb, PSTT=tuple(PSTT)):
                        ins = None
                        for j in range(4):
                            ins = e.transpose(out=PSTT[kb % 2][:, j * 128:(j + 1) * 128],
                                              in_=xb[:, j, kb * 128:(kb + 1) * 128], identity=ident[:])
                        return ins
                    sub_ = os.environ.get("P1SUB", "")
                    if sub_ == "one" and kb > 0:
                        continue
                    add("pe", tr, r=["p1xb%d" % j for j in range(4)] + ["ident"], w=["pst%d" % (kb % 2)])
                    if sub_ == "tr":
                        continue
                    cp("act" if kb % 2 else "dve", xT[i][:, kb, :], PSTT[kb % 2][:, 0:512],
                       r=["pst%d" % (kb % 2)], w=["p1xT%d_%d" % (i, kb)])
                xTn = ["p1xT%d_%d" % (i, kb) for kb in range(8)]
                if cut < 3:
                    continue
                dma("sp", XT[:, :, t * 512:(t + 1) * 512].rearrange("k p s -> p k s"), xT[i][:], r=xTn, w=[], key="p1xT%d" % i)
                if cut < 4:
                    continue
                for (col, dst, dstn, DR, sc) in ((COLQ, qd, "p1qd", QD, 0.125), (COLK, kd, "p1kd", KD, 1.0)):
                    for h in range(8):
                        pt, pn = nps()
                        mm(pt[:], [(w1[:, kb, col + h * 128:col + (h + 1) * 128], xT[i][:, kb, :]) for kb in range(8)],
                           r=wn + xTn, w=[pn])
                        if h % 2 == 0:
                            act(dst[i][:, h, :], pt[:], AF.Copy, r=[pn], w=["%s%d_%d" % (dstn, i, h)], scale=sc)
                        else:
                            ts("dve", dst[i][:, h, :], pt[:], sc, None, ALU.mult, None, r=[pn], w=["%s%d_%d" % (dstn, i, h)])
                    dma("sp", DR[:, :, t * 512:(t + 1) * 512].rearrange("h p s -> p h s"), dst[i][:],
                        r=["%s%d_%d" % (dstn, i, h) for h in range(8)], w=[], key="%s%d" % (dstn, i))
                if cut < 5:
                    continue
                for j in range(4):
                    for hf in range(2):
                        pt, pn = nps()
                        mm(pt[:], [(xT[i][:, kb, j * 128:(j + 1) * 128], w1[:, kb, COLV + hf * 512:COLV + (hf + 1) * 512]) for kb in range(8)],
                           r=wn + xTn, w=[pn])
                        cp("act" if hf else "dve", vd[i][:, j, hf * 512:(hf + 1) * 512], pt[:], r=[pn], w=["p1vd%d_%d_%d" % (i, j, hf)])
                dma("sp", VD[t * 512:(t + 1) * 512, :].rearrange("(j p) e -> p j e", p=128), vd[i][:],
                    r=["p1vd%d_%d_%d" % (i, j, hf) for j in range(4) for hf in range(2)], w=[], key="p1vd%d" % i)
        S_.barrier()
        if upto == 'P1':
            S_.emit(nc)
            es.close()
            return nc

        with ExitStack() as ph:
            alloc_psum(ph, 8, 0)
            w2 = sb(ph, "p2w", [128, 8, INC - 3072], BF16)
            W2O = 3072
            wq_st = sb(ph, "p2wqs", [128, 3, 768], F32)
            wkv_st = sb(ph, "p2wkvs", [128, 2, 1024], F32)
            wq = sb(ph, "p2wq", [128, 3, 768], BF16)
            wkv = sb(ph, "p2wkv", [128, 2, 1024], BF16)
            gq = sb(ph, "p2gq", [128, 3], F32)
            gkv = sb(ph, "p2gkv", [128, 2], F32)
            bg = sb(ph, "p2bg", [128, 16], F32)
            xT = [sb(ph, "p2xT%d" % i, [128, 8, 512], BF16) for i in range(2)]
            cs = [sb(ph, "p2cs%d" % i, [128, 2, 512], F32) for i in range(2)]
            gsb = sb(ph, "p2g", [128, 16, 512], BF16)
            cqb = sb(ph, "p2cqb", [128, 3, 512], BF16)
            sqq = sb(ph, "p2sqq", [128, 3, 512], BF16)
            ckb = sb(ph, "p2ckb", [128, 2, 512], BF16)
            sqk = sb(ph, "p2sqk", [128, 2, 512], BF16)
            rq = sb(ph, "p2rq", [128, 512], F32)
            rk = sb(ph, "p2rk", [128, 512], F32)
            rkc = sb(ph, "p2rkc", [128, 4], F32)
            qn = sb(ph, "p2qn", [128, 4, 512], BF16)
            kn = sb(ph, "p2kn", [128, 4, 512], BF16)
            vm = sb(ph, "p2vm", [128, 4, 512], BF16)
            x1s = sb(ph, "p2x1s", [128, 512], F32)
            x2s = sb(ph, "p2x2s", [128, 512], F32)
            ta = sb(ph, "p2ta", [128, 512], F32)
            tb = sb(ph, "p2tb", [128, 512], F32)
            qr = sb(ph, "p2qr", [128, 2, 512], BF16)
            kr = sb(ph, "p2kr", [16, 2, 512], BF16)
            stg_state["tiles"] = [sb(ph, "p2stg%d" % i, [128, STGW], F32) for i in range(2)]
            W2W = INC - 3072
            for kb in range(8):
                for c3 in range(0, W2W, STGW):
                    wd = min(STGW, W2W - c3)
                    wload(w2[:, kb, c3:c3 + wd], w_in[li, kb * 128:(kb + 1) * 128, 3072 + c3:3072 + c3 + wd], wd, "p2w%d_%d" % (kb, c3))
            wn = ["p2w%d_%d" % (kb, c3) for kb in range(8) for c3 in range(0, W2W, STGW)]
            dma("sp", wq_st[:], wuq_in[li].rearrange("(k p) c -> p k c", p=128), r=[], w=["p2wqs"], key="p2wqs")
            dma("sp", wkv_st[:], wukv_in[li].rearrange("(k p) c -> p k c", p=128), r=[], w=["p2wkvs"], key="p2wkvs")
            dma("sp", gq[:], gq_in[li], r=[], w=["p2gq"], key="p2gq")
            dma("sp", gkv[:], gkv_in[li], r=[], w=["p2gkv"], key="p2gkv")
            dma("sp", bg[:], bg_in[li], r=[], w=["p2bg"], key="p2bg")
            for k in range(3):
                ts("dve", wq[:, k, :], wq_st[:, k, :], gq[:, k:k + 1], None, ALU.mult, None, r=["p2wqs", "p2gq"], w=["p2wq"])
            for k in range(2):
                ts("dve", wkv[:, k, :], wkv_st[:, k, :], gkv[:, k:k + 1], None, ALU.mult, None, r=["p2wkvs", "p2gkv"], w=["p2wkv"])

            def ldt(t):
                i = t % 2
                dma("sp", xT[i][:], XT[:, :, t * 512:(t + 1) * 512].rearrange("k p s -> p k s"), r=[], w=["p2xT%d" % i], key="p2xT%d" % i)
                dma("sp", cs[i][:], CS[:, :, t * 512:(t + 1) * 512].rearrange("c p s -> p c s"), r=[], w=["p2cs%d" % i], key="p2cs%d" % i)
            ldt(0)
            MSC = 96.0 ** -0.5
            for t in range(NT):
                i = t % 2
                if t + 1 < NT:
                    ldt(t + 1)
                xn = ["p2xT%d" % i]
                tsl = slice(t * 512, (t + 1) * 512)
                for gb in range(16):
                    pt, pn = nps()
                    c0 = COLG - W2O + gb * 128
                    mm(pt[:], [(w2[:, kb, c0:c0 + 128], xT[i][:, kb, :]) for kb in range(8)], r=wn + xn, w=[pn])
                    act(gsb[:, gb, :], pt[:], AF.Sigmoid, r=[pn, "p2bg"], w=["p2g%d" % gb], bias=bg[:, gb:gb + 1], scale=1.0)
                dma("sp", GT[:, :, tsl].rearrange("g p s -> p g s"), gsb[:], r=["p2g%d" % gb for gb in range(16)], w=[], key="p2g")
                for b in range(3):
                    pt, pn = nps()
                    c0 = COLCQ - W2O + b * 128
                    mm(pt[:], [(w2[:, kb, c0:c0 + 128], xT[i][:, kb, :]) for kb in range(8)], r=wn + xn, w=[pn])
                    cp("dve", cqb[:, b, :], pt[:], r=[pn], w=["p2cqb%d" % b])
                    act(sqq[:, b, :], pt[:], AF.Square, r=[pn], w=["p2sqq%d" % b])
                pt, pn = nps()
                mm(pt[:], [(ones_bf[:], sqq[:, b, :]) for b in range(3)], r=["ones_bf"] + ["p2sqq%d" % b for b in range(3)], w=[pn])
                rstd_from(rq[:], pt[:], 1.0 / NQ, r=[pn], w=["p2rq"])
                ts("dve", rq[:], rq[:], MSC, None, ALU.mult, None, r=["p2rq"], w=["p2rq"])
                cqn = ["p2cqb%d" % b for b in range(3)]
                for pr in range(4):
                    pt, pn = nps()
                    mm(pt[:], [(wq[:, k, pr * 128:(pr + 1) * 128], cqb[:, k, :]) for k in range(3)], r=["p2wq"] + cqn, w=[pn])
                    tt("dve", qn[:, pr, :], pt[:], rq[:], ALU.mult, r=[pn, "p2rq"], w=["p2qn%d" % pr])
                dma("sp", QMN[:, :, tsl].rearrange("j p s -> p j s"), qn[:], r=["p2qn%d" % pr for pr in range(4)], w=[], key="p2qn")
                pt1, pn1 = nps()
                mm(pt1[:], [(wq[:, k, 512:640], cqb[:, k, :]) for k in range(3)], r=["p2wq"] + cqn, w=[pn1])
                pt2, pn2 = nps()
                mm(pt2[:], [(wq[:, k, 640:768], cqb[:, k, :]) for k in range(3)], r=["p2wq"] + cqn, w=[pn2])
                tt("dve", x1s[:], pt1[:], rq[:], ALU.mult, r=[pn1, "p2rq"], w=["p2x1s"])
                tt("dve", x2s[:], pt2[:], rq[:], ALU.mult, r=[pn2, "p2rq"], w=["p2x2s"])
                csn = "p2cs%d" % i
                tt("dve", ta[:], x1s[:], cs[i][:, 0, :], ALU.mult, r=["p2x1s", csn], w=["p2ta"])
                tt("pool", tb[:], x2s[:], cs[i][:, 1, :], ALU.mult, r=["p2x2s", csn], w=["p2tb"])
                tt("dve", qr[:, 0, :], ta[:], tb[:], ALU.subtract, r=["p2ta", "p2tb"], w=["p2qr0"])
                tt("dve", ta[:], x1s[:], cs[i][:, 1, :], ALU.mult, r=["p2x1s", csn], w=["p2ta"])
                tt("pool", tb[:], x2s[:], cs[i][:, 0, :], ALU.mult, r=["p2x2s", csn], w=["p2tb"])
                tt("dve", qr[:, 1, :], ta[:], tb[:], ALU.add, r=["p2ta", "p2tb"], w=["p2qr1"])
                dma("sp", QR[:, :, tsl].rearrange("c p s -> p c s"), qr[:], r=["p2qr0", "p2qr1"], w=[], key="p2qr")
                for b in range(2):
                    pt, pn = nps()
                    c0 = COLCKV - W2O + b * 128
                    mm(pt[:], [(w2[:, kb, c0:c0 + 128], xT[i][:, kb, :]) for kb in range(8)], r=wn + xn, w=[pn])
                    cp("dve", ckb[:, b, :], pt[:], r=[pn], w=["p2ckb%d" % b])
                    act(sqk[:, b, :], pt[:], AF.Square, r=[pn], w=["p2sqk%d" % b])
                sqn = ["p2sqk%d" % b for b in range(2)]
                pt, pn = nps()
                mm(pt[:], [(ones_bf[:], sqk[:, b, :]) for b in range(2)], r=["ones_bf"] + sqn, w=[pn])
                rstd_from(rk[:], pt[:], 1.0 / NKV, r=[pn], w=["p2rk"])
                ptc, pnc = nps()
                for j in range(4):
                    mm(ptc[:, j:j + 1], [(sqk[:, b, j * 128:(j + 1) * 128], ones_bf[:, 0:1]) for b in range(2)], r=["ones_bf"] + sqn, w=[pnc])
                rstd_from(rkc[:], ptc[:, 0:4], 1.0 / NKV, r=[pnc], w=["p2rkc"])
                ckn = ["p2ckb%d" % b for b in range(2)]
                for pr in range(4):
                    pt, pn = nps()
                    mm(pt[:], [(wkv[:, k, pr * 128:(pr + 1) * 128], ckb[:, k, :]) for k in range(2)], r=["p2wkv"] + ckn, w=[pn])
                    tt("dve", kn[:, pr, :], pt[:], rk[:], ALU.mult, r=[pn, "p2rk"], w=["p2kn%d" % pr])
                dma("sp", KMN[:, :, tsl].rearrange("j p s -> p j s"), kn[:], r=["p2kn%d" % pr for pr in range(4)], w=[], key="p2kn")
                for j in range(4):
                    pt, pn = nps()
                    mm(pt[:], [(ckb[:, k, j * 128:(j + 1) * 128], wkv[:, k, 512:1024]) for k in range(2)], r=["p2wkv"] + ckn, w=[pn])
                    act(vm[:, j, :], pt[:], AF.Copy, r=[pn, "p2rkc"], w=["p2vm%d" % j], scale=rkc[:, j:j + 1])
                dma("sp", VM[tsl, :].rearrange("(j p) e -> p j e", p=128), vm[:], r=["p2vm%d" % j for j in range(4)], w=[], key="p2vm")
                pt1, pn1 = nps()
                c0 = COLKR - W2O
                mm(pt1[0:16, :], [(w2[:, kb, c0:c0 + 16], xT[i][:, kb, :]) for kb in range(8)], r=wn + xn, w=[pn1])
                pt2, pn2 = nps()
                mm(pt2[0:16, :], [(w2[:, kb, c0 + 16:c0 + 32], xT[i][:, kb, :]) for kb in range(8)], r=wn + xn, w=[pn2])
                tt("dve", ta[0:16, :], pt1[0:16, :], cs[i][0:16, 0, :], ALU.mult, r=[pn1, csn], w=["p2ta"])
                tt("dve", tb[0:16, :], pt2[0:16, :], cs[i][0:16, 1, :], ALU.mult, r=[pn2, csn], w=["p2tb"])
                tt("dve", kr[:, 0, :], ta[0:16, :], tb[0:16, :], ALU.subtract, r=["p2ta", "p2tb"], w=["p2kr0"])
                tt("dve", ta[0:16, :], pt1[0:16, :], cs[i][0:16, 1, :], ALU.mult, r=[pn1, csn], w=["p2ta"])
                tt("dve", tb[0:16, :], pt2[0:16, :], cs[i][0:16, 0, :], ALU.mult, r=[pn2, csn], w=["p2tb"])
                tt("dve", kr[:, 1, :], ta[0:16, :], tb[0:16, :], ALU.add, r=["p2ta", "p2tb"], w=["p2kr1"])
                dma("sp", KR[:, :, tsl].rearrange("c p s -> p c s"), kr[:], r=["p2kr0", "p2kr1"], w=[], key="p2kr")
        S_.barrier()
        if upto == 'P2':
            S_.emit(nc)
            es.close()
            return nc

        with ExitStack() as ph:
            alloc_psum(ph, 8, 0)
            bstr = sb(ph, "abst", [128, 9, 1024], F32)
            lamt = sb(ph, "alam", [128, 256], F32)
            lamp = sb(ph, "alamp", [128, 128], F32)
            lamc = sb(ph, "alamc", [128, 4], F32)
            subg = sb(ph, "asubg", [128, 1], F32)
            kdt = [sb(ph, "akd%d" % i, [128, S], BF16) for i in range(2)]
            vdt = [sb(ph, "avd%d" % i, [128, NB, 128], BF16) for i in range(2)]
            vmt = [sb(ph, "avm%d" % i, [128, NB, 65], BF16) for i in range(2)]
            qt = [sb(ph, "aq%d" % i, [128, 512], BF16) for i in range(2)]
            NPT = 6
            pT = [sb(ph, "apT%d" % i, [128, 512], BF16) for i in range(NPT)]
            sbias = [sb(ph, "asb%d" % i, [128, 512], F32) for i in range(4)]
            accS = [sb(ph, "aacc%d" % i, [128, 512], F32) for i in range(4)]
            accB = [sb(ph, "aaccb%d" % i, [128, 512], BF16) for i in range(2)]
            sbc2 = [0]
            rr = sb(ph, "arr", [128, 512], F32)
            rr2 = sb(ph, "arr2", [1, 512], F32)
            Rb = [sb(ph, "aRb%d" % i, [128, 512], F32) for i in range(2)]
            t0 = sb(ph, "at0", [128, 512], F32)
            t1 = sb(ph, "at1", [128, 512], F32)
            osq = sb(ph, "aosq", [128, 512], BF16)
            rms = sb(ph, "arms", [128, 512], F32)
            oo = [sb(ph, "aoo%d" % i, [128, 512], BF16) for i in range(2)]
            dma("sp", bstr[:], BST.rearrange("h p n -> p h n"), r=[], w=["abst"], key="abst")
            dma("sp", lamt[:], lam_in[li].partition_broadcast(128), r=[], w=["alam"], key="alam")
            dma("sp", subg[:], subg_in[li], r=[], w=["asubg"], key="asubg")
            tt("dve", lamp[:, 0:64], lamt[:, 0:64], lamt[:, 64:128], ALU.mult, r=["alam"], w=["alamp"])
            tt("dve", lamp[:, 64:128], lamt[:, 128:192], lamt[:, 192:256], ALU.mult, r=["alam", "alamp"], w=["alamp"])
            add("dve", lambda e: e.reduce_sum(out=lamc[:, 0:1], in_=lamp[:, 0:64], axis=AX.X), r=["alamp"], w=["alamc"])
            add("dve", lambda e: e.reduce_sum(out=lamc[:, 1:2], in_=lamp[:, 64:128], axis=AX.X), r=["alamp", "alamc"], w=["alamc"])
            act(lamc[:, 0:2], lamc[:, 0:2], AF.Exp, r=["alamc"], w=["alamc"])
            tt("dve", lamc[:, 2:3], lamc[:, 1:2], lamc[:, 0:1], ALU.subtract, r=["alamc"], w=["alamc"])
            ts("dve", lamc[:, 2:3], lamc[:, 2:3], -lam_init, None, ALU.add, None, r=["alamc"], w=["alamc"])
            for i in range(2):
                add("pool", lambda e, i=i: e.memset(vmt[i][:, :, 64:65], 1.0), w=["avm1_%d" % i])
            PS_S = [(PSB[i], psn[i]) for i in range(4)]
            NPS = 4
            POS = [[(PSB[4], psn[4]), (PSB[5], psn[5])], [(PSB[6], psn[6]), (PSB[7], psn[7])]]
            sctr = [0]
            pctr = [0]
            pending = []
            tctr = [0]

            def make_post(isd, h, t, PO, am, oi):
                tsl = slice(t * 512, (t + 1) * 512)
                segs = []
                if isd:
                    def seg1():
                        for m in range(2):
                            cp("dve", accB[m][:], accS[am + m][:], r=["aacc%d" % (am + m)], w=["aaccb%d" % m])
                            pts, pns = PS_S[sctr[0] % NPS]
                            sctr[0] += 1
                            mm1(pts[0:1, :], ones_bf[:, 0:1], accB[m][:], True, True, r=["ones_bf", "aaccb%d" % m], w=[pns])
                            rrm = rr if m == 0 else rr2
                            act(rrm[0:1, :], pts[0:1, :], AF.Ln, r=[pns], w=["arr%d" % m])
                            act(rrm[0:1, :], rrm[0:1, :], AF.Exp, r=["arr%d" % m], w=["arr%d" % m], scale=-1.0)

                    def seg2():
                        for m in range(2):
                            rrm = rr if m == 0 else rr2
                            pt, pn = PS_S[sctr[0] % NPS]
                            sctr[0] += 1
                            mm1(pt[:], ones_f[0:1, :], rrm[0:1, :], True, True, r=["ones_f", "arr%d" % m], w=[pn])
                            cp("act", Rb[m][:], pt[:], r=[pn], w=["aRb%d" % m])

                    def seg3():
                        tt("dve", t0[:], PO[0][0][:], Rb[0][:], ALU.mult, r=[PO[0][1], "aRb0"], w=["at0"])
                        tt("dve", t1[:], PO[1][0][:], Rb[1][:], ALU.mult, r=[PO[1][1], "aRb1"], w=["at1"])
                        stt("dve", t0[:], t1[:], lamc[:, 2:3], t0[:], ALU.mult, ALU.add, r=["at0", "at1", "alamc"], w=["at0"])
                        act(osq[:], t0[:], AF.Square, r=["at0"], w=["aosq"])

                    def seg4():
                        pt, pn = PS_S[sctr[0] % NPS]
                        sctr[0] += 1
                        mm1(pt[:], ones_bf[:], osq[:], True, True, r=["ones_bf", "aosq"], w=[pn])
                        rstd_from(rms[:], pt[:], 1.0 / 128.0, r=[pn], w=["arms"])
                        stt("dve", t1[:], t0[:], subg[:, 0:1], rms[:], ALU.mult, ALU.mult, r=["at0", "asubg", "arms"], w=["at1"])
                        ts("dve", oo[oi][:], t1[:], 1.0 - lam_init, None, ALU.mult, None, r=["at1"], w=["aoo%d" % oi])
                        dma("sp", OAT[h, :, tsl], oo[oi][:], r=["aoo%d" % oi], w=[], key="aoo%d" % oi)
                    segs = [seg1, seg2, seg3, seg4]
                else:
                    def seg1():
                        act(rr[64:65, :], PO[0][0][64:65, :], AF.Ln, r=[PO[0][1]], w=["arr0"])
                        act(rr[64:65, :], rr[64:65, :], AF.Exp, r=["arr0"], w=["arr0"], scale=-1.0)
                        pt, pn = PS_S[sctr[0] % NPS]
                        sctr[0] += 1
                        mm1(pt[0:64, :], ones_f[64:65, 0:64], rr[64:65, :], True, True, r=["ones_f", "arr0"], w=[pn])
                        cp("act", Rb[0][0:64, :], pt[0:64, :], r=[pn], w=["aRb0"])

                    def seg2():
                        tt("dve", oo[oi][0:64, :], PO[0][0][0:64, :], Rb[0][0:64, :], ALU.mult, r=[PO[0][1], "aRb0"], w=["aoo%d" % oi])
                        dma("sp", OBT[h, :, tsl], oo[oi][0:64, :], r=["aoo%d" % oi], w=[], key="aoo%d" % oi)
                    segs = [seg1, seg2]
                return segs

            for hh in range(16):
                isd = hh < 8
                h = hh % 8
                i = hh % 2
                if isd:
                    dma("sp", kdt[i][:], KD[h], r=[], w=["akd%d" % i], key="akd%d" % i)
                    dma("sp", vdt[i][:], VD[:, h * 128:(h + 1) * 128].rearrange("(b p) e -> p b e", p=128), r=[], w=["avd%d" % i], key="avd%d" % i)
                else:
                    pr, hp = h // 2, h % 2
                    dma("sp", kdt[i][0:64, :], KMN[pr, hp * 64:(hp + 1) * 64, :], r=[], w=["akd%d" % i], key="akd%d" % i)
                    dma("sp", kdt[i][64:80, :], KR[0], r=[], w=["akdr1_%d" % i], key="akdr1_%d" % i)
                    dma("sp", kdt[i][80:96, :], KR[1], r=[], w=["akdr2_%d" % i], key="akdr2_%d" % i)
                    dma("sp", vmt[i][:, :, 0:64], VM[:, h * 64:(h + 1) * 64].rearrange("(b p) e -> p b e", p=128), r=[], w=["avm%d" % i], key="avm%d" % i)
                kn_ = ["akd%d" % i] + ([] if isd else ["akdr1_%d" % i, "akdr2_%d" % i])
                vn_ = ["avd%d" % i] if isd else ["avm%d" % i, "avm1_%d" % i]
                for t in range(NT):
                    tc = tctr[0]
                    tctr[0] += 1
                    qi = tc % 2
                    PO = POS[tc % 2]
                    am = 2 * (tc % 2)
                    tsl = slice(t * 512, (t + 1) * 512)
                    if isd:
                        dma("sp", qt[qi][:], QD[h, :, tsl], r=[], w=["aq%d" % qi], key="aq%d" % qi)
                        qn_ = ["aq%d" % qi]
                    else:
                        pr, hp = h // 2, h % 2
                        dma("sp", qt[qi][0:64, :], QMN[pr, hp * 64:(hp + 1) * 64, tsl], r=[], w=["aq%d" % qi], key="aq%d" % qi)
                        dma("sp", qt[qi][64:80, :], QR[0, h * 16:(h + 1) * 16, tsl], r=[], w=["aqr1_%d" % qi], key="aqr1_%d" % qi)
                        dma("sp", qt[qi][80:96, :], QR[1, h * 16:(h + 1) * 16, tsl], r=[], w=["aqr2_%d" % qi], key="aqr2_%d" % qi)
                        qn_ = ["aq%d" % qi, "aqr1_%d" % qi, "aqr2_%d" % qi]
                    nkb = 4 * t + 4
                    maps = (0, 1) if isd else (0,)

                    def stage1(kb):
                        delta = 512 * t - 128 * kb
                        c0 = max(0, -delta)
                        near = delta < 256
                        ksl = slice(kb * 128, (kb + 1) * 128)
                        res_ = []
                        for m in maps:
                            pt, pn = PS_S[sctr[0] % NPS]
                            sctr[0] += 1
                            pb = pT[pctr[0] % NPT]
                            pbn = "apT%d" % (pctr[0] % NPT)
                            pctr[0] += 1
                            rows = slice(m * 64, (m + 1) * 64) if isd else slice(0, 96)
                            mm1(pt[:, c0:512], kdt[i][rows, ksl], qt[qi][rows, c0:512], True, True, r=kn_ + qn_, w=[pn])
                            if (isd and near) or ((not isd) and delta <= 0):
                                si = sbc2[0] % 4
                                sbc2[0] += 1
                                sbt = sbias[si]
                                bi = h if isd else 8
                                tt("dve", sbt[:, c0:512], pt[:, c0:512], bstr[:, bi, delta + 384 + c0:delta + 384 + 512], ALU.add,
                                   r=[pn, "abst"], w=["asb%d" % si])
                                act(pb[:, c0:512], sbt[:, c0:512], AF.Exp, r=["asb%d" % si], w=[pbn])
                            elif isd:
                                act(pb[:, c0:512], pt[:, c0:512], AF.Exp, r=[pn, "tabb"], w=[pbn], bias=tabb[:, 248 + h:249 + h], scale=1.0)
                            else:
                                act(pb[:, c0:512], pt[:, c0:512], AF.Exp, r=[pn], w=[pbn])
                            res_.append((pb, pbn, c0))
                        return res_

                    def stage2(kb, res_):
                        first, last = kb == 0, kb == nkb - 1
                        for m, (pb, pbn, c0) in zip(maps, res_):
                            if isd:
                                mm1(PO[m][0][:, c0:512], vdt[i][:, kb, :], pb[:, c0:512], first, last, r=vn_ + [pbn], w=[PO[m][1]])
                                an = "aacc%d" % (am + m)
                                if first:
                                    cp("dve", accS[am + m][:], pb[:], r=[pbn], w=[an])
                                else:
                                    tt("dve", accS[am + m][:, c0:512], accS[am + m][:, c0:512], pb[:, c0:512], ALU.add, r=[pbn, an], w=[an])
                            else:
                                mm1(PO[0][0][0:65, c0:512], vmt[i][:, kb, :], pb[:, c0:512], first, last, r=vn_ + [pbn], w=[PO[0][1]])
                    q_ = [stage1(0), stage1(1)]
                    for kb in range(nkb):
                        if kb + 2 < nkb:
                            q_.append(stage1(kb + 2))
                        if pending:
                            pending.pop(0)()
                        stage2(kb, q_.pop(0))
                    while pending:
                        pending.pop(0)()
                    pending.extend(make_post(isd, h, t, PO, am, tc % 2))
            while pending:
                pending.pop(0)()
        S_.barrier()
        if upto == 'A':
            S_.emit(nc)
            es.close()
            return nc

        with ExitStack() as ph:
            alloc_psum(ph, 6, 2)
            wbd = sb(ph, "mwbd", [128, 8, D], BF16)
            wbm = sb(ph, "mwbm", [64, 8, D], BF16)
            wout = sb(ph, "mwout", [128, 8, D], BF16)
            lng = sb(ph, "mlng", [128, 2, D], F32)
            oa = [sb(ph, "moa%d" % i, [128, 8, 512], BF16) for i in range(2)]
            ob = [sb(ph, "mob%d" % i, [64, 8, 512], BF16) for i in range(2)]
            gs = [sb(ph, "mg%d" % i, [128, 16, 512], BF16) for i in range(2)]
            xs = [sb(ph, "mxs%d" % i, [128, 4, D], F32) for i in range(2)]
            mT = sb(ph, "mmT", [128, 8, 512], BF16)
            tA = [sb(ph, "mtA%d" % i, [128, 512], F32) for i in range(2)]
            tB = [sb(ph, "mtB%d" % i, [128, 512], F32) for i in range(2)]
            zs = [sb(ph, "mzs%d" % i, [128, D], F32) for i in range(2)]
            x1 = [sb(ph, "mx1%d" % i, [128, D], F32) for i in range(2)]
            x1b = sb(ph, "mx1b", [128, D], BF16)
            x1T = sb(ph, "mx1T", [128, 8, 512], BF16)
            st = sb(ph, "mst", [128, 12], F32)
            mv = sb(ph, "mmv", [128, 2], F32)
            rs = sb(ph, "mrs", [128, 1], F32)
            stg_state["tiles"] = [sb(ph, "mstg%d" % i, [128, 1024], F32) for i in range(2)]
            for k in range(8):
                wload(wbd[:, k, :], wbd_in[li, k * 128:(k + 1) * 128, :], 1024, "mwbd")
                wload(wbm[:, k, :], wbm_in[li, k * 64:(k + 1) * 64, :], 1024, "mwbm", parts=64)
                wload(wout[:, k, :], wout_in[li, k * 128:(k + 1) * 128, :], 1024, "mwout")
            dma("sp", lng[:].rearrange("p a d -> p (a d)"), lnm_in[li].rearrange("a d -> (a d)").partition_broadcast(128), r=[], w=["mlng"], key="mlng")

            def ldm(t):
                i = t % 2
                tsl = slice(t * 512, (t + 1) * 512)
                dma("sp", oa[i][:], OAT[:, :, tsl].rearrange("h p s -> p h s"), r=[], w=["moa%d" % i], key="moa%d" % i)
                dma("sp", ob[i][:], OBT[:, :, tsl].rearrange("h p s -> p h s"), r=[], w=["mob%d" % i], key="mob%d" % i)
                dma("sp", gs[i][:], GT[:, :, tsl].rearrange("g p s -> p g s"), r=[], w=["mg%d" % i], key="mg%d" % i)
                dma("sp", xs[i][:], x_src[tsl, :].rearrange("(j p) d -> p j d", p=128), r=[], w=["mxs%d" % i], key="mxs%d" % i)
            ldm(0)
            for t in range(NT):
                i = t % 2
                tsl = slice(t * 512, (t + 1) * 512)
                if t + 1 < NT:
                    ldm(t + 1)
                for db in range(8):
                    dsl = slice(db * 128, (db + 1) * 128)
                    ptA, pnA = nps()
                    mm(ptA[:], [(wbd[:, h, dsl], oa[i][:, h, :]) for h in range(8)], r=["mwbd", "moa%d" % i], w=[pnA])
                    ptB, pnB = nps()
                    mm(ptB[:], [(wbm[:, h, dsl], ob[i][:, h, :]) for h in range(8)], r=["mwbm", "mob%d" % i], w=[pnB])
                    a = db % 2
                    tt("dve", tA[a][:], ptA[:], gs[i][:, db, :], ALU.mult, r=[pnA, "mg%d" % i], w=["mtA%d" % a])
                    tt("dve", tB[a][:], ptB[:], gs[i][:, 8 + db, :], ALU.mult, r=[pnB, "mg%d" % i], w=["mtB%d" % a])
                    tt("dve", mT[:, db, :], tA[a][:], tB[a][:], ALU.add, r=["mtA%d" % a, "mtB%d" % a], w=["mmT%d" % db])
                mTn = ["mmT%d" % db for db in range(8)]
                for j in range(4):
                    a = j % 2
                    for hf in range(2):
                        hsl = slice(hf * 512, (hf + 1) * 512)
                        pt, pn = nps()
                        mm(pt[:], [(mT[:, db, j * 128:(j + 1) * 128], wout[:, db, hsl]) for db in range(8)], r=["mwout"] + mTn, w=[pn])
                        stt("dve", zs[a][:, hsl], xs[i][:, j, hsl], ALPHA, pt[:], ALU.mult, ALU.add, r=["mxs%d" % i, pn], w=["mzs%d_%d" % (a, hf)])
                    zn = ["mzs%d_0" % a, "mzs%d_1" % a]
                    layernorm(add, ts, tt, act, rstd_from, zs[a], x1[a], st, mv, rs, lng, zn, "mx1%d" % a, "m")
                    dma("sp", X1[t * 512 + j * 128:t * 512 + (j + 1) * 128, :], x1[a][:], r=["mx1%d" % a], w=[], key="mx1%d" % a)
                    cp("act", x1b[:], x1[a][:], r=["mx1%d" % a], w=["mx1b"])
                    for kq in range(2):
                        def tr(e, kq=kq, PSTT=tuple(PSTT)):
                            ins = None
                            for k4 in range(4):
                                kb = kq * 4 + k4
                                ins = e.transpose(out=PSTT[kq][:, k4 * 128:(k4 + 1) * 128],
                                                  in_=x1b[:, kb * 128:(kb + 1) * 128], identity=ident[:])
                            return ins
                        add("pe", tr, r=["mx1b", "ident"], w=["pst%d" % kq])
                        add("act", lambda e, kq=kq, j=j, src=PSTT[kq]: e.copy(out=x1T[:, kq * 4:(kq + 1) * 4, j * 128:(j + 1) * 128],
                                                               in_=src[:, 0:512].rearrange("p (k s) -> p k s", k=4)),
                            r=["pst%d" % kq], w=["mx1T_%d_%d" % (j, kq)])
                dma("sp", X1T[:, :, tsl].rearrange("k p s -> p k s"), x1T[:], r=["mx1T_%d_%d" % (j, kq) for j in range(4) for kq in range(2)], w=[], key="mx1T")
        S_.barrier()
        if upto == 'M':
            S_.emit(nc)
            es.close()
            return nc

        with ExitStack() as ph:
            alloc_psum(ph, 6, 2)
            wpg = sb(ph, "ewpg", [128, 8, D], BF16)
            wpp = sb(ph, "ewpp", [128, 2, D], BF16)
            xT = [sb(ph, "exT%d" % i, [128, 8, 512], BF16) for i in range(2)]
            pp = [sb(ph, "epp%d" % i, [128, 4, 256], F32) for i in range(2)]
            ppb = sb(ph, "eppb", [128, 4, 256], BF16)
            ppT = sb(ph, "eppT", [128, 2, 512], BF16)
            sg = [sb(ph, "esg%d" % i, [128, 512], F32) for i in range(2)]
            ee = [sb(ph, "eee%d" % i, [128, 4, D], F32) for i in range(2)]
            stg_state["tiles"] = [sb(ph, "estg%d" % i, [128, 1024], F32) for i in range(2)]
            for k in range(8):
                wload(wpg[:, k, :], wpg_in[li, k * 128:(k + 1) * 128, :], 1024, "ewpg")
            for k in range(2):
                wload(wpp[:, k, :], wpp_in[li, k * 128:(k + 1) * 128, :], 1024, "ewpp")

            def lde(t):
                i = t % 2
                tsl = slice(t * 512, (t + 1) * 512)
                dma("sp", xT[i][:], X1T[:, :, tsl].rearrange("k p s -> p k s"), r=[], w=["exT%d" % i], key="exT%d" % i)
                dma("sp", pp[i][:], p_in[li, tsl, :].rearrange("(j p) c -> p j c", p=128), r=[], w=["epp%d" % i], key="epp%d" % i)
            lde(0)
            for t in range(NT):
                i = t % 2
                tsl = slice(t * 512, (t + 1) * 512)
                if t + 1 < NT:
                    lde(t + 1)
                cp("pool", ppb[:], pp[i][:], r=["epp%d" % i], w=["eppb"])
                for kq in range(2):
                    def tr(e, kq=kq, PSTT=tuple(PSTT)):
                        ins = None
                        for j in range(4):
                            ins = e.transpose(out=PSTT[kq][:, j * 128:(j + 1) * 128],
                                              in_=ppb[:, j, kq * 128:(kq + 1) * 128], identity=ident[:])
                        return ins
                    add("pe", tr, r=["eppb", "ident"], w=["pst%d" % kq])
                    cp("act", ppT[:, kq, :], PSTT[kq][:, 0:512], r=["pst%d" % kq], w=["eppT%d" % kq])
                for j in range(4):
                    for hf in range(2):
                        hsl = slice(hf * 512, (hf + 1) * 512)
                        a = hf
                        ptA, pnA = nps()
                        mm(ptA[:], [(xT[i][:, kb, j * 128:(j + 1) * 128], wpg[:, kb, hsl]) for kb in range(8)], r=["ewpg", "exT%d" % i], w=[pnA])
                        ptB, pnB = nps()
                        mm(ptB[:], [(ppT[:, kq, j * 128:(j + 1) * 128], wpp[:, kq, hsl]) for kq in range(2)], r=["ewpp", "eppT0", "eppT1"], w=[pnB])
                        act(sg[a][:], ptA[:], AF.Sigmoid, r=[pnA], w=["esg%d" % a])
                        tt("dve", ee[i][:, j, hsl], sg[a][:], ptB[:], ALU.mult, r=["esg%d" % a, pnB], w=["eee%d_%d_%d" % (i, j, hf)])
                dma("sp", EE[tsl, :].rearrange("(j p) d -> p j d", p=128), ee[i][:],
                    r=["eee%d_%d_%d" % (i, j, hf) for j in range(4) for hf in range(2)], w=[], key="eee%d" % i)
        S_.barrier()
        if upto == 'E':
            S_.emit(nc)
            es.close()
            return nc

        moe = (li % 2 == 1)
        lj = li // 2
        if moe:
            with ExitStack() as ph2:
                rwb = sb(ph2, "frw", [128, 8, D], F32)
                rx1 = [sb(ph2, "rx1%d" % i, [128, D], F32) for i in range(2)]
                lg = sb(ph2, "flg", [128, 8], F32)
                junk = sb(ph2, "fjunk", [128, D], F32)
                mx8 = sb(ph2, "fmx8", [128, 8], F32)
                msk = sb(ph2, "fmsk", [128, 8], F32)
                ex = sb(ph2, "fex", [128, 8], F32)
                den = sb(ph2, "fden", [128, 2], F32)
                dma("sp", rwb[:].rearrange("p a d -> p (a d)"), rw_in[lj].partition_broadcast(128), r=[], w=["frw"], key="frw")
                for sbi in range(NB):
                    a = sbi % 2
                    dma("sp", rx1[a][:], X1[sbi * 128:(sbi + 1) * 128, :], r=[], w=["rx1%d" % a], key="rx1%d" % a)
                    for ex_ in range(8):
                        add("dve", lambda e, a=a, ex_=ex_: e.scalar_tensor_tensor(out=junk[:], in0=rx1[a][:], scalar=1.0, in1=rwb[:, ex_, :], op0=ALU.mult, op1=ALU.mult, accum_out=lg[:, ex_:ex_ + 1]),
                            r=["rx1%d" % a, "frw", "flg", "fjunk"], w=["flg", "fjunk"])
                    add("dve", lambda e: e.max(out=mx8[:], in_=lg[:]), r=["flg"], w=["fmx8"])
                    ts("dve", msk[:], lg[:], mx8[:, 1:2], None, ALU.is_ge, None, r=["flg", "fmx8"], w=["fmsk"])
                    ts("dve", ex[:], lg[:], mx8[:, 0:1], None, ALU.subtract, None, r=["flg", "fmx8"], w=["fex"])
                    act(ex[:], ex[:], AF.Exp, r=["fex"], w=["fex"])
                    tt("dve", ex[:], ex[:], msk[:], ALU.mult, r=["fex", "fmsk"], w=["fex"])
                    add("dve", lambda e: e.reduce_sum(out=den[:, 0:1], in_=ex[:], axis=AX.X), r=["fex"], w=["fden"])
                    add("dve", lambda e: e.reciprocal(out=den[:, 1:2], in_=den[:, 0:1]), r=["fden"], w=["fden"])
                    ts("dve", gates[:, sbi, :], ex[:], den[:, 1:2], None, ALU.mult, None, r=["fex", "fden"], w=["gates"])
            S_.barrier()

        with ExitStack() as ph:
            alloc_psum(ph, 8, 0)
            TGl = TG if moe else min(S, 2 * TG)
            NGl = S // TGl
            NSUB = TGl // 128
            NCB = 7 if moe else 4
            Y = sb(ph, "fY", [128, NSUB, D], F32)
            xT = sb(ph, "fxT", [128, 8, TGl], BF16)
            wa = [sb(ph, "fwa%d" % i, [128, 8, NCB * 128], BF16) for i in range(2)]
            wb = [sb(ph, "fwb%d" % i, [128, 8, NCB * 128], BF16) for i in range(2)]
            wc = [sb(ph, "fwc%d" % i, [128, NCB, D], BF16) for i in range(2)]
            GTt = [sb(ph, "fG%d" % i, [128, NCB, 512], BF16) for i in range(2)]
            sl = [sb(ph, "fsl%d" % i, [128, 512], F32) for i in range(2)]
            x1 = [sb(ph, "fx1%d" % i_, [128, D], F32) for i_ in range(2)]
            zz = [sb(ph, "fzz%d" % i_, [128, D], F32) for i_ in range(2)]
            xo = [sb(ph, "fxo%d" % i_, [128, D], F32) for i_ in range(2)]
            lng = sb(ph, "flng", [128, 2, D], F32)
            st = sb(ph, "fst", [128, 12], F32)
            mv = sb(ph, "fmv", [128, 2], F32)
            rs = sb(ph, "frs", [128, 1], F32)
            stg_state["tiles"] = [sb(ph, "fstg%d" % i, [128, 1024], F32) for i in range(3)]
            dma("sp", lng[:].rearrange("p a d -> p (a d)"), lnf_in[li].rearrange("a d -> (a d)").partition_broadcast(128), r=[], w=["flng"], key="flng")
            FF = EXP_FF if moe else DENSE_FF
            nblk = FF // 128
            chunks = []
            b0 = 0
            while b0 < nblk:
                nb_ = min(NCB, nblk - b0)
                if nblk - b0 - nb_ in (1, 2) and nb_ > 3:
                    nb_ -= 2
                chunks.append((b0, nb_))
                b0 += nb_
            nexp = 8 if moe else 1
            seq = [(g, ex_, c) for g in range(NGl) for ex_ in range(nexp) for c in range(len(chunks))]

            def wjobs(k):
                g, ex_, c = seq[k]
                i = k % 2
                b0, nb_ = chunks[c]
                if moe:
                    s1, s3, s2 = ew1_in[lj, ex_], ew3_in[lj, ex_], ew2_in[lj, ex_]
                else:
                    s1, s3, s2 = dw1_in[lj], dw3_in[lj], dw2_in[lj]
                jobs = []
                wdt = nb_ * 128
                for kb in range(8):
                    jobs.append(lambda kb=kb: wload(wa[i][:, kb, 0:wdt], s1[kb * 128:(kb + 1) * 128, b0 * 128:b0 * 128 + wdt], wdt, "fwa%d_%d" % (i, kb)))
                    jobs.append(lambda kb=kb: wload(wb[i][:, kb, 0:wdt], s3[kb * 128:(kb + 1) * 128, b0 * 128:b0 * 128 + wdt], wdt, "fwb%d_%d" % (i, kb)))
                for jb in range(nb_):
                    jobs.append(lambda jb=jb: wload(wc[i][:, jb, :], s2[(b0 + jb) * 128:(b0 + jb + 1) * 128, :], 1024, "fwc%d_%d" % (i, jb)))
                return jobs
            for jfn in wjobs(0):
                jfn()
            gctr = [0]
            for k, (g, ex_, c) in enumerate(seq):
                i = k % 2
                b0, nb_ = chunks[c]
                gsl = slice(g * TGl, (g + 1) * TGl)
                if ex_ == 0 and c == 0 and g == 0:
                    dma("sp", xT[:], X1T[:, :, gsl].rearrange("k p s -> p k s"), r=[], w=["fxT"], key="fxT")
                    dma("sp", Y[:], EE[gsl, :].rearrange("(j p) d -> p j d", p=128), r=[], w=["fY%d" % s_ for s_ in range(NSUB)], key="fY")
                pend = wjobs(k + 1) if k + 1 < len(seq) else []
                pend.reverse()

                def pump(n=1):
                    for _ in range(n):
                        if pend:
                            pend.pop()()
                wan = ["fwa%d_%d" % (i, kb) for kb in range(8)]
                wbn = ["fwb%d_%d" % (i, kb) for kb in range(8)]
                for tl in range(TGl // 512):
                    gi = gctr[0] % 2
                    gctr[0] += 1
                    tsl = slice(tl * 512, (tl + 1) * 512)
                    for jb in range(nb_):
                        fsl = slice(jb * 128, (jb + 1) * 128)
                        pump(1)
                        pt1, pn1 = nps()
                        mm(pt1[:], [(wa[i][:, kb, fsl], xT[:, kb, tsl]) for kb in range(8)], r=wan + ["fxT"], w=[pn1])
                        pt3, pn3 = nps()
                        mm(pt3[:], [(wb[i][:, kb, fsl], xT[:, kb, tsl]) for kb in range(8)], r=wbn + ["fxT"], w=[pn3])
                        a = jb % 2
                        act(sl[a][:], pt1[:], AF.Silu, r=[pn1], w=["fsl%d" % a])
                        tt("dve", GTt[gi][:, jb, :], sl[a][:], pt3[:], ALU.mult, r=["fsl%d" % a, pn3], w=["fG%d_%d" % (gi, jb)])
                    Gn = ["fG%d_%d" % (gi, jb) for jb in range(nb_)]
                    wcn = ["fwc%d_%d" % (i, jb) for jb in range(nb_)]
                    for j4 in range(4):
                        sub = tl * 4 + j4
                        sbi = g * NSUB + sub
                        for hf in range(2):
                            hsl = slice(hf * 512, (hf + 1) * 512)
                            pump(1)
                            pt, pn = nps()
                            mm(pt[:], [(GTt[gi][:, jb, j4 * 128:(j4 + 1) * 128], wc[i][:, jb, hsl]) for jb in range(nb_)], r=wcn + Gn, w=[pn])
                            gcol = gates[:, sbi, ex_:ex_ + 1] if moe else ccol[:, 2:3]
                            stt("dve", Y[:, sub, hsl], pt[:], gcol, Y[:, sub, hsl], ALU.mult, ALU.add,
                                r=[pn, "fY%d" % sub, "gates", "ccol"], w=["fY%d" % sub])
                pump(100)
                if ex_ == nexp - 1 and c == len(chunks) - 1:
                    nxt_g = g + 1 < NGl
                    if nxt_g:
                        ngsl = slice((g + 1) * TGl, (g + 2) * TGl)
                        dma("sp", xT[:], X1T[:, :, ngsl].rearrange("k p s -> p k s"), r=[], w=["fxT"], key="fxT")
                    for sub in range(NSUB):
                        a = sub % 2
                        r0 = g * TGl + sub * 128
                        dma("sp", x1[a][:], X1[r0:r0 + 128, :], r=[], w=["fx1%d" % a], key="fx1%d" % a)
                        stt("dve", zz[a][:], x1[a][:], ALPHA, Y[:, sub, :], ALU.mult, ALU.add, r=["fx1%d" % a, "fY%d" % sub], w=["fzz%d" % a])
                        if nxt_g:
                            n0 = (g + 1) * TGl + sub * 128
                            dma("sp", Y[:, sub, :], EE[n0:n0 + 128, :], r=[], w=["fY%d" % sub], key="fYs%d" % sub)
                        layernorm(add, ts, tt, act, rstd_from, zz[a], xo[a], st, mv, rs, lng, ["fzz%d" % a], "fxo%d" % a, "f")
                        dma("sp", x_dst[r0:r0 + 128, :], xo[a][:], r=["fxo%d" % a], w=[], key="fxo%d" % a)
        S_.barrier()

    S_.emit(nc)
    es.close()
    return nc


def layernorm(add, ts, tt, act, rstd_from, z, xo, st, mv, rs, lng, zn, xon, pfx):
    stn, mvn, rsn = pfx + "st", pfx + "mv", pfx + "rs"
    add("dve", lambda e: e.bn_stats(out=st[:, 0:6], in_=z[:, 0:512]), r=zn, w=[stn])
    add("dve", lambda e: e.bn_stats(out=st[:, 6:12], in_=z[:, 512:1024]), r=zn + [stn], w=[stn])
    add("dve", lambda e: e.bn_aggr(out=mv[:], in_=st[:]), r=[stn], w=[mvn])
    rstd_from(rs[:], mv[:, 1:2], 1.0, r=[mvn], w=[rsn])
    ts("dve", xo[:], z[:], mv[:, 0:1], rs[:, 0:1], ALU.subtract, ALU.mult, r=zn + [mvn, rsn], w=[xon])
    tt("dve", xo[:], xo[:], lng[:, 0, :], ALU.mult, r=[xon, pfx + "lng"], w=[xon])
    tt("dve", xo[:], xo[:], lng[:, 1, :], ALU.add, r=[xon, pfx + "lng"], w=[xon])


def prep_inputs(inp, b, S, L):
    f = np.float32
    def c(a):
        return np.ascontiguousarray(a)
    NM = L // 2
    wuq = np.asarray(inp["w_uq"])[:L].reshape(L, 384, 8, 96)
    wuq_p = np.concatenate([wuq[..., 0:64].reshape(L, 384, 512), wuq[..., 64:80].reshape(L, 384, 128),
                            wuq[..., 80:96].reshape(L, 384, 128)], axis=-1)
    wukv = np.asarray(inp["w_ukv"])[:L].reshape(L, 256, 8, 128)
    wukv_p = np.concatenate([wukv[..., 0:64].reshape(L, 256, 512), wukv[..., 64:128].reshape(L, 256, 512)], axis=-1)
    lam = np.concatenate([np.asarray(inp[k])[:L] for k in ("lambda_q1", "lambda_k1", "lambda_q2", "lambda_k2")], axis=-1).reshape(L, 1, 256)
    d = {
        "x": c(np.asarray(inp["x"])[b, :S]),
        "p": c(np.asarray(inp["p"])[:L, b, :S]),
        "pos": c(np.asarray(inp["positions"])[b, :S].reshape(1, S).astype(np.int32)),
        "tab": c(np.asarray(inp["rel_bias_table"]).reshape(1, 256)),
        "w_in": c(np.asarray(inp["w_in"])[:L]),
        "bg": c(np.asarray(inp["b_gate"])[:L].reshape(L, 16, 128).transpose(0, 2, 1)),
        "lam": c(lam),
        "subg": c(np.asarray(inp["diff_subln_g"])[:L].reshape(L, 128, 1)),
        "gq": c(np.asarray(inp["mla_q_norm_g"])[:L].reshape(L, 3, 128).transpose(0, 2, 1)),
        "wuq": c(wuq_p),
        "gkv": c(np.asarray(inp["mla_kv_norm_g"])[:L].reshape(L, 2, 128).transpose(0, 2, 1)),
        "wukv": c(wukv_p),
        "wbd": c(np.asarray(inp["w_branch_diff"])[:L]),
        "wbm": c(np.asarray(inp["w_branch_mla"])[:L]),
        "wout": c(np.asarray(inp["w_out"])[:L]),
        "lnm": c(np.stack([np.asarray(inp["ln_mix_g"])[:L], np.asarray(inp["ln_mix_b"])[:L]], axis=1)),
        "dw1": c(np.asarray(inp["dense_w1"])[:(L + 1) // 2]),
        "dw3": c(np.asarray(inp["dense_w3"])[:(L + 1) // 2]),
        "dw2": c(np.asarray(inp["dense_w2"])[:(L + 1) // 2]),
        "rw": c(np.asarray(inp["router_w"])[:max(NM, 1)].transpose(0, 2, 1).reshape(max(NM, 1), 1, 8 * 1024)),
        "ew1": c(np.asarray(inp["expert_w1"])[:max(NM, 1)]),
        "ew3": c(np.asarray(inp["expert_w3"])[:max(NM, 1)]),
        "ew2": c(np.asarray(inp["expert_w2"])[:max(NM, 1)]),
        "wpg": c(np.asarray(inp["w_ple_gate"])[:L]),
        "wpp": c(np.asarray(inp["w_ple_proj"])[:L]),
        "lnf": c(np.stack([np.asarray(inp["ln_ffn_g"])[:L], np.asarray(inp["ln_ffn_b"])[:L]], axis=1)),
    }
    cst = np.zeros((128, 4), f)
    half = 16
    invf = (np.float32(10000.0) ** (-np.arange(half, dtype=f) / np.float32(half))).astype(f)
    cst[:, 0] = np.tile(invf, 8)
    d["cst"] = cst
    return {k: np.ascontiguousarray(v) for k, v in d.items()}


_NC_CACHE = {}


def kernel(**inputs):
    S, L, NCORES = 4096, 4, 8
    if "nc" not in _NC_CACHE:
        _NC_CACHE["nc"] = build(S, L, TG=1024)
    nc = _NC_CACHE["nc"]
    in_maps = [prep_inputs(inputs, b, S, L) for b in range(NCORES)]
    res = run_bass_kernel_spmd(nc, in_maps, core_ids=list(range(NCORES)))
    return np.stack([np.asarray(r["out"], dtype=np.float32) for r in res.results], axis=0)
```
